# Optimizing a Trainium2 kernel written in Bass

```python
import math
import jax, jax.numpy as jnp
from jax import lax
import numpy as np

D_MODEL = 1024
BATCH = 32
SEQ = 2048
DEPTH = 1

CHUNK = 64
MIX_WIDTH = D_MODEL
ATT_WIDTH = MIX_WIDTH // 2
GMLP_WIDTH = MIX_WIDTH - ATT_WIDTH
ATT_HEAD_DIM = 64
ATT_VDIM = 2 * ATT_HEAD_DIM
ATT_HEADS = ATT_WIDTH // ATT_VDIM
Q_BLOCK = 128
GMLP_BLOCK = 128
GMLP_GROUPS = 4
GMLP_GROUP_DIM = GMLP_WIDTH // GMLP_GROUPS
IN_WIDTH = 3 * ATT_WIDTH + 2 * GMLP_WIDTH
PEER_HEADS = 8
PEER_KEYS = 128
PEER_EXPERTS = PEER_KEYS * PEER_KEYS
PEER_TOPK = 16
PEER_QDIM = 256
PEER_HALF = PEER_QDIM // 2
PEER_TOKEN_BLOCK = 128
EPS = 1e-6

kernel_name = 'hybrid_diffattn_gmlp_peer_block'


def rmsnorm(x, g):
    xf = x.astype(jnp.float32)
    y = xf * lax.rsqrt(jnp.mean(xf * xf, axis=-1, keepdims=True) + EPS)
    return (y * g.astype(jnp.float32)).astype(x.dtype)


def alibi_slopes(n):
    return 2.0 ** (-8.0 * jnp.arange(1, n + 1, dtype=jnp.float32) / n)


def diff_attention(q, k, v, lam, slopes):
    B, S = q.shape[0], q.shape[1]
    nb = S // Q_BLOCK
    k_pos = jnp.arange(S)
    qb = (q * (ATT_HEAD_DIM ** -0.5)).reshape(B, nb, Q_BLOCK, ATT_HEADS, 2, ATT_HEAD_DIM)
    qb = qb.transpose(1, 0, 2, 3, 4, 5)

    def block(args):
        q_blk, i = args
        q_pos = i * Q_BLOCK + jnp.arange(Q_BLOCK)
        allowed = (k_pos[None, :] // CHUNK) <= (q_pos[:, None] // CHUNK)
        dist = jnp.abs(q_pos[:, None] - k_pos[None, :]).astype(jnp.float32)
        bias = -slopes[:, None, None] * dist[None]
        s = jnp.einsum('bqhmd,bkhmd->bhmqk', q_blk, k).astype(jnp.float32)
        s = jnp.where(allowed, s + bias[None, :, None], -jnp.inf)
        p = jax.nn.softmax(s, axis=-1)
        a = p[:, :, 0] - lam * p[:, :, 1]
        return jnp.einsum('bhqk,bkhe->bqhe', a.astype(v.dtype), v)

    o = lax.map(block, (qb, jnp.arange(nb)))
    return o.transpose(1, 0, 2, 3, 4).reshape(B, S, ATT_HEADS, ATT_VDIM)


def spatial_gating(u, gv, w_s, b_s, g_v, g_out):
    B, S = u.shape[0], u.shape[1]
    nc = S // GMLP_BLOCK
    shp = (B, nc, GMLP_BLOCK, GMLP_GROUPS, GMLP_GROUP_DIM)
    gshp = (GMLP_GROUPS, GMLP_GROUP_DIM)
    gv = rmsnorm(gv.reshape(shp), g_v.reshape(gshp))
    mask = jnp.tril(jnp.ones((GMLP_BLOCK, GMLP_BLOCK), dtype=bool))
    w = jnp.where(mask[None], w_s, jnp.zeros_like(w_s))
    gate = jnp.einsum('gts,bnsgc->bntgc', w.astype(gv.dtype), gv) + b_s.T[None, None, :, :, None]
    y = rmsnorm(u.reshape(shp) * gate, g_out.reshape(gshp))
    return y.reshape(B, S, GMLP_WIDTH)


def peer(xn, w_q, sub_keys, w_u, w_v):
    B, S, D = xn.shape
    T = B * S
    xt = xn.reshape(T // PEER_TOKEN_BLOCK, PEER_TOKEN_BLOCK, D)
    K = PEER_TOPK

    def block(xb):
        q = (xb @ w_q).reshape(PEER_TOKEN_BLOCK, PEER_HEADS, 2, PEER_HALF)
        s = jnp.einsum('thpd,hpnd->thpn', q, sub_keys).astype(jnp.float32)
        vals, idx = lax.top_k(s, K)
        cand = vals[:, :, 0, :, None] + vals[:, :, 1, None, :]
        best, flat = lax.top_k(cand.reshape(PEER_TOKEN_BLOCK, PEER_HEADS, K * K), K)
        i1 = jnp.take_along_axis(idx[:, :, 0], flat // K, axis=-1)
        i2 = jnp.take_along_axis(idx[:, :, 1], flat % K, axis=-1)
        expert = i1 * PEER_KEYS + i2
        g = jax.nn.softmax(best, axis=-1)
        h = jnp.einsum('thkd,td->thk', w_u[expert], xb)
        a = (g * jax.nn.gelu(h.astype(jnp.float32))).astype(xb.dtype)
        return jnp.einsum('thk,thkd->td', a, w_v[expert])

    return lax.map(block, xt).reshape(B, S, D)


def setup_inputs(seed: int = 0) -> dict:
    key = jax.random.key(seed)
    ks = jax.random.split(key, 20)
    f32 = jnp.float32
    nrm = lambda k, shp, sc: jax.random.normal(k, shp, f32) * sc
    return {
        'x': nrm(ks[0], (BATCH, SEQ, D_MODEL), 1.0),
        'w_in': nrm(ks[1], (DEPTH, D_MODEL, IN_WIDTH), D_MODEL ** -0.5),
        'lam_q1': nrm(ks[2], (DEPTH, ATT_HEAD_DIM), 0.1),
        'lam_k1': nrm(ks[3], (DEPTH, ATT_HEAD_DIM), 0.1),
        'lam_q2': nrm(ks[4], (DEPTH, ATT_HEAD_DIM), 0.1),
        'lam_k2': nrm(ks[5], (DEPTH, ATT_HEAD_DIM), 0.1),
        'g_subln': 1.0 + nrm(ks[6], (DEPTH, ATT_VDIM), 0.02),
        'w_s': nrm(ks[7], (DEPTH, GMLP_GROUPS, GMLP_BLOCK, GMLP_BLOCK), GMLP_BLOCK ** -0.5),
        'b_s': 1.0 + nrm(ks[8], (DEPTH, GMLP_GROUPS, GMLP_BLOCK), 0.02),
        'g_gv': 1.0 + nrm(ks[9], (DEPTH, GMLP_WIDTH), 0.02),
        'g_gout': 1.0 + nrm(ks[10], (DEPTH, GMLP_WIDTH), 0.02),
        'w_out': nrm(ks[11], (DEPTH, MIX_WIDTH, D_MODEL), MIX_WIDTH ** -0.5),
        'g_mix': 1.0 + nrm(ks[12], (DEPTH, D_MODEL), 0.02),
        'g_ffn': 1.0 + nrm(ks[13], (DEPTH, D_MODEL), 0.02),
        'peer_wq': nrm(ks[14], (DEPTH, D_MODEL, PEER_HEADS * PEER_QDIM), D_MODEL ** -0.5),
        'peer_keys': nrm(ks[15], (DEPTH, PEER_HEADS, 2, PEER_KEYS, PEER_HALF), PEER_HALF ** -0.5),
        'peer_wu': nrm(ks[16], (DEPTH, PEER_EXPERTS, D_MODEL), D_MODEL ** -0.5),
        'peer_wv': nrm(ks[17], (DEPTH, PEER_EXPERTS, D_MODEL), PEER_HEADS ** -0.5),
        'g_final': 1.0 + nrm(ks[18], (D_MODEL,), 0.02),
    }


def reference(x, w_in, lam_q1, lam_k1, lam_q2, lam_k2, g_subln, w_s, b_s, g_gv, g_gout,
              w_out, g_mix, g_ffn, peer_wq, peer_keys, peer_wu, peer_wv, g_final):
    B, S, _ = x.shape
    slopes = alibi_slopes(ATT_HEADS)
    splits = [ATT_WIDTH, 2 * ATT_WIDTH, 3 * ATT_WIDTH, 3 * ATT_WIDTH + GMLP_WIDTH]
    for l in range(DEPTH):
        h = rmsnorm(x, g_mix[l])
        p = h @ w_in[l]
        q, k, v, u, gv = jnp.split(p, splits, axis=-1)
        q = q.reshape(B, S, ATT_HEADS, 2, ATT_HEAD_DIM)
        k = k.reshape(B, S, ATT_HEADS, 2, ATT_HEAD_DIM)
        v = v.reshape(B, S, ATT_HEADS, ATT_VDIM)
        lam_init = 0.8 - 0.6 * math.exp(-0.3 * l)
        lam = (jnp.exp(jnp.sum(lam_q1[l] * lam_k1[l]).astype(jnp.float32))
               - jnp.exp(jnp.sum(lam_q2[l] * lam_k2[l]).astype(jnp.float32)) + lam_init)
        o = diff_attention(q, k, v, lam, slopes)
        o = rmsnorm(o, g_subln[l]) * (1.0 - lam_init)
        y = spatial_gating(jax.nn.gelu(u), jax.nn.gelu(gv), w_s[l], b_s[l], g_gv[l], g_gout[l])
        mix = jnp.concatenate([o.reshape(B, S, ATT_WIDTH), y], axis=-1)
        x = x + mix @ w_out[l]
        x = x + peer(rmsnorm(x, g_ffn[l]), peer_wq[l], peer_keys[l], peer_wu[l], peer_wv[l])
    return rmsnorm(x, g_final)
```

```python
import numpy as np
from contextlib import ExitStack
import concourse.bass as bass
import concourse.mybir as mybir
from concourse.bass_utils import run_bass_kernel_spmd

F32 = mybir.dt.float32
BF16 = mybir.dt.bfloat16
U32 = mybir.dt.uint32
AF = mybir.ActivationFunctionType
ALU = mybir.AluOpType
AX = mybir.AxisListType

ENGS = ["sync", "scalar", "vector", "gpsimd", "tensor"]
EPS = 1e-6
NCORES = 8
D = 1024
S = 2048
NT = S // 128
NEG = -30000.0


class Prog:
    def __init__(self, nc):
        self.nc = nc
        self.ops = []
        self.last_w = {}
        self.readers = {}
        self.dma_sem_count = {}
        self.epoch = 0
        self.key_epoch = {}
        self.fence_ops = []
        self.last_eng_op = {}
        self.last_dma_op = {}

    def fence(self):
        self.epoch += 1
        self.fence_ops = list(self.last_eng_op.values()) + list(self.last_dma_op.values())

    def _add(self, eng, fn, r, w, dma_sem=None):
        idx = len(self.ops)
        deps = set()
        for k in list(r) + list(w):
            if k.startswith("ws:") and self.key_epoch.get(k) != self.epoch:
                self.key_epoch[k] = self.epoch
                self.last_w.pop(k, None)
                self.readers.pop(k, None)
                deps.update(self.fence_ops)
        raw = set()
        for k in r:
            lw = self.last_w.get(k)
            if lw is not None:
                deps.add(lw)
                raw.add(lw)
        for k in w:
            lw = self.last_w.get(k)
            if lw is not None:
                deps.add(lw)
            deps.update(self.readers.get(k, ()))
        real = set()
        for d in deps:
            od = self.ops[d]
            if od["dma_sem"] is None and od["eng"] == eng and d not in raw:
                continue
            real.add(d)
        op = dict(eng=eng, fn=fn, deps=real, dma_sem=dma_sem, idx=idx)
        if dma_sem is not None:
            c = self.dma_sem_count.get(dma_sem, 0) + 16
            self.dma_sem_count[dma_sem] = c
            op["val"] = c
            self.last_dma_op[dma_sem] = idx
        else:
            self.last_eng_op[eng] = idx
        self.ops.append(op)
        for k in r:
            self.readers.setdefault(k, []).append(idx)
        for k in w:
            self.last_w[k] = idx
            self.readers[k] = []
        return idx

    def op(self, eng, fn, r=(), w=()):
        return self._add(eng, fn, r, w)

    def dma(self, eng, out, in_, r=(), w=(), sem="dma", **kw):
        def fn(e, out=out, in_=in_, kw=kw):
            return e.dma_start(out=out, in_=in_, **kw)
        return self._add(eng, fn, r, w, dma_sem=sem)

    def emit(self, final_wait_ops=()):
        nc = self.nc
        ops = self.ops
        needs_inc = [False] * len(ops)
        for o in ops:
            for d in o["deps"]:
                needs_inc[d] = True
        for d in final_wait_ops:
            needs_inc[d] = True
        cnt = {e: 0 for e in ENGS}
        for o in ops:
            if o["dma_sem"] is None:
                o["sem"] = "eng_" + o["eng"]
                if needs_inc[o["idx"]]:
                    cnt[o["eng"]] += 1
                    o["val"] = cnt[o["eng"]]
                else:
                    o["val"] = None
            else:
                o["sem"] = "dma_" + str(o["dma_sem"])
        semnames = ["eng_" + e for e in ENGS] + ["dma_" + str(k) for k in self.dma_sem_count]
        with ExitStack() as st:
            sems = {n: st.enter_context(nc.semaphore(n)) for n in semnames}
            block = st.enter_context(nc.Block())
            per_eng = {e: [o for o in ops if o["eng"] == e] for e in ENGS}

            def make(ename):
                def body(eng):
                    waited = {}
                    for o in per_eng[ename]:
                        for d in sorted(o["deps"]):
                            od = ops[d]
                            s, v = od["sem"], od["val"]
                            if waited.get(s, 0) >= v:
                                continue
                            eng.wait_ge(sems[s], v)
                            waited[s] = v
                        ins = o["fn"](eng)
                        if o["dma_sem"] is not None:
                            ins.then_inc(sems[o["sem"]], 16)
                        elif o["val"] is not None:
                            ins.then_inc(sems[o["sem"]], 1)
                    if ename == "sync":
                        for d in final_wait_ops:
                            od = ops[d]
                            if waited.get(od["sem"], 0) >= od["val"]:
                                continue
                            eng.wait_ge(sems[od["sem"]], od["val"])
                            waited[od["sem"]] = od["val"]
                return body

            block.sync(make("sync"))
            block.scalar(make("scalar"))
            block.vector(make("vector"))
            block.gpsimd(make("gpsimd"))
            block.tensor(make("tensor"))


class Carver:
    def __init__(self, base):
        self.base = base
        self.off = 0
        self.cap = base.shape[1]

    def take(self, shape, dtype):
        esz = 2 if dtype == BF16 else 4
        n = int(np.prod(shape)) * esz // 2
        n_al = (n + 15) // 16 * 16
        assert self.off + n_al <= self.cap, ("workspace overflow", self.off, n_al, self.cap)
        v = self.base[:, self.off:self.off + n]
        self.off += n_al
        if dtype != BF16:
            v = v.bitcast(dtype)
        if len(shape) == 2:
            v = v.rearrange("p (a b) -> p a b", a=shape[0])
        elif len(shape) == 3:
            v = v.rearrange("p (a b c) -> p a b c", a=shape[0], b=shape[1])
        return v


def build_nc(nseq, stop_after=None):
    import os
    stop_after = os.environ.get('KSTOP', stop_after)
    NTOK = nseq * S
    nc = bass.Bass("TRN2", target_bir_lowering=False)
    dt = lambda n, s, d=F32, kind="ExternalInput": nc.dram_tensor(n, s, d, kind=kind).ap()
    x = dt("x", [NTOK, D])
    w_in = dt("w_in", [D, 2560])
    lamv = dt("lamv", [4, 64])
    g_subln = dt("g_subln", [128])
    w_s = dt("w_s", [4, 128, 128])
    b_s = dt("b_s", [4, 128])
    g_gv = dt("g_gv", [512])
    g_gout = dt("g_gout", [512])
    w_out = dt("w_out", [D, D])
    g_mix = dt("g_mix", [D])
    g_ffn = dt("g_ffn", [D])
    peer_wq = dt("peer_wq", [D, 2048])
    peer_keys = dt("peer_keys", [16, 128, 128])
    peer_wu = dt("peer_wu", [16384, D])
    peer_wv = dt("peer_wv", [16384, D])
    g_final = dt("g_final", [D])
    out = dt("out", [NTOK, D], F32, "ExternalOutput")
    wuT_d = dt("wuT_d", [128, 128, 1024], BF16, "Internal")
    wvb_d = dt("wvb_d", [128, 128, 1024], BF16, "Internal")
    wqb_d = dt("wqb_d", [16, 128, 8, 128], BF16, "Internal")
    x1s = dt("x1s", [NTOK, D], F32, "Internal")

    WS_ELEMS = (212800 - 21600) // 2 // 16 * 16
    with ExitStack() as st:
        sb = lambda n, s, d: st.enter_context(nc.sbuf_tensor(n, s, d))
        ws = sb("ws", [128, WS_ELEMS], BF16)
        iotf = sb("iotf", [128, 128], F32)
        iorow = sb("iorow", [128, 128], F32)
        iob = sb("iob", [128, 128], BF16)
        identb = sb("identb", [128, 128], BF16)
        identf = sb("identf", [128, 128], F32)
        trilm = sb("trilm", [128, 128], F32)
        MdT = sb("MdT", [128, 4, 128], F32)
        bcol = sb("bcol", [128, 4, 16], F32)
        lamt = sb("lamt", [128, 4, 64], F32)
        lamw = sb("lamw", [128, 8], F32)
        ggv = sb("ggv", [128, 512], F32)
        ggo = sb("ggo", [128, 512], F32)
        gsub = sb("gsub", [128, 128], F32)
        gfin = sb("gfin", [128, 1024], F32)
        gmc = sb("gmc", [128, 8], F32)
        gfc = sb("gfc", [128, 8], F32)
        gfcb = sb("gfcb", [128, 8], BF16)
        bsT = sb("bsT", [128, 4], F32)
        wsT = sb("wsT", [128, 4, 128], BF16)
        keysT = sb("keysT", [128, 16, 128], BF16)
        stat = sb("stat", [128, 64], F32)
        pb = [st.enter_context(nc.psum_tensor(f"ws:pb{i}", [128, 512], F32)) for i in range(8)]
        pbb = [p[:].bitcast(BF16) for p in pb]

        P = Prog(nc)
        V = lambda name, r, w, **kw: P.op("vector", lambda e: getattr(e, name)(**kw), r, w)
        A = lambda r, w, **kw: P.op("scalar", lambda e: e.activation(**kw), r, w)
        G = lambda name, r, w, **kw: P.op("gpsimd", lambda e: getattr(e, name)(**kw), r, w)
        MM = lambda r, w, **kw: P.op("tensor", lambda e: e.matmul(**kw), r, w)
        TR = lambda r, w, **kw: P.op("tensor", lambda e: e.transpose(**kw), r, w)

        statn = [0]

        def stcol(n=1):
            c = statn[0] % 12 * 4
            statn[0] += 1
            return stat[:, c:c + n], f"stat{c}"

        def rstd_from_ss(ss_ap, ss_key, n, inv_n, out_ap, out_key):
            t1, k1 = stcol(n)
            V("tensor_scalar", [ss_key], [k1], out=t1, in0=ss_ap, scalar1=inv_n, scalar2=EPS,
              op0=ALU.mult, op1=ALU.add)
            t2, k2 = stcol(n)
            A([k1], [k2], out=t2, in_=t1, func=AF.Sqrt)
            V("reciprocal", [k2], [out_key], out=out_ap, in_=t2)


        def stop_here(tag, src_ap, rkeys):
            if stop_after != tag:
                return False
            o = P.dma("sync", out[0:128, 0:src_ap.shape[1]], src_ap, r=rkeys, sem="stopout")
            print("STOP at", tag, "n ops", len(P.ops))
            P.emit(final_wait_ops=[o])
            return True
        G("iota", [], ["iotf"], out=iotf[:], pattern=[[1, 128]], base=0, channel_multiplier=-1,
          allow_small_or_imprecise_dtypes=True)
        G("iota", [], ["iorow"], out=iorow[:], pattern=[[1, 128]], base=0, channel_multiplier=0,
          allow_small_or_imprecise_dtypes=True)
        V("tensor_copy", ["iorow"], ["iob"], out=iob[:], in_=iorow[:])
        V("tensor_single_scalar", ["iotf"], ["identb"], out=identb[:], in_=iotf[:], scalar=0.0, op=ALU.is_equal)
        V("tensor_single_scalar", ["iotf"], ["identf"], out=identf[:], in_=iotf[:], scalar=0.0, op=ALU.is_equal)
        V("tensor_single_scalar", ["iotf"], ["trilm"], out=trilm[:], in_=iotf[:], scalar=0.0, op=ALU.is_le)
        absd = MdT[:, 3, :]
        A(["iotf"], ["MdT3"], out=absd, in_=iotf[:], func=AF.Abs)
        V("tensor_tensor", ["MdT3", "iorow"], ["MdT3"], out=absd, in0=iorow[:], in1=absd, op=ALU.subtract)
        slopes = [2.0 ** (-2.0 * (h + 1)) for h in range(4)]
        for h in range(4):
            V("tensor_scalar", ["MdT3"], [f"MdT{h}"], out=MdT[:, h, :], in0=absd, scalar1=slopes[h], scalar2=None,
              op0=ALU.mult)
        for h in range(4):
            V("memset", [], [f"MdT{h}"], ap=MdT[64:128, h, 0:64], constant=NEG)
        G("iota", [], ["bcol3"], out=bcol[:, 3, :], pattern=[[-128, 16]], base=0, channel_multiplier=1,
          allow_small_or_imprecise_dtypes=True)
        for h in range(4):
            V("tensor_scalar", ["bcol3"], [f"bcol{h}"], out=bcol[:, h, :], in0=bcol[:, 3, :], scalar1=slopes[h],
              scalar2=None, op0=ALU.mult)
        P.dma("sync", lamt[:].rearrange("p a b -> p (a b)"), lamv.rearrange("a b -> (a b)").partition_broadcast(128),
              w=["lamt"], sem="c0")
        V("tensor_tensor", ["lamt"], ["lamp"], out=lamt[:, 0, :], in0=lamt[:, 0, :], in1=lamt[:, 1, :], op=ALU.mult)
        V("tensor_tensor", ["lamt"], ["lamp2"], out=lamt[:, 2, :], in0=lamt[:, 2, :], in1=lamt[:, 3, :], op=ALU.mult)
        V("tensor_reduce", ["lamp"], ["lw0"], out=lamw[:, 0:1], in_=lamt[:, 0, :], axis=AX.X, op=ALU.add)
        V("tensor_reduce", ["lamp2"], ["lw1"], out=lamw[:, 1:2], in_=lamt[:, 2, :], axis=AX.X, op=ALU.add)
        A(["lw0", "lw1"], ["lw23"], out=lamw[:, 2:4], in_=lamw[:, 0:2], func=AF.Exp)
        V("tensor_tensor", ["lw23"], ["lw4"], out=lamw[:, 4:5], in0=lamw[:, 3:4], in1=lamw[:, 2:3], op=ALU.subtract)
        V("tensor_scalar", ["lw4"], ["neglam"], out=lamw[:, 5:6], in0=lamw[:, 4:5], scalar1=-0.2, scalar2=None,
          op0=ALU.add)
        neglam = lamw[:, 5:6]
        P.dma("sync", ggv[:], g_gv.partition_broadcast(128), w=["ggv"], sem="c1")
        P.dma("sync", ggo[:], g_gout.partition_broadcast(128), w=["ggo"], sem="c2")
        P.dma("sync", gsub[:], g_subln.partition_broadcast(128), w=["gsubraw"], sem="c3")
        V("tensor_scalar", ["gsubraw"], ["gsub"], out=gsub[:], in0=gsub[:], scalar1=0.8, scalar2=None, op0=ALU.mult)
        P.dma("sync", gfin[:], g_final.partition_broadcast(128), w=["gfin"], sem="c4")
        P.dma("sync", gmc[:], g_mix.rearrange("(c p) -> p c", p=128), w=["gmc"], sem="c5",
              allow_slow_non_contiguous=True)
        P.dma("sync", gfc[:], g_ffn.rearrange("(c p) -> p c", p=128), w=["gfc"], sem="c6",
              allow_slow_non_contiguous=True)
        V("tensor_copy", ["gfc"], ["gfcb"], out=gfcb[:], in_=gfc[:])
        P.dma("sync", bsT[:], b_s.rearrange("g t -> t g"), w=["bsT"], sem="c7", allow_slow_non_contiguous=True)

        if stop_here('const', gfin[:], ['gfin','ggv','ggo','gsub','gmc','gfc','gfcb','bsT','neglam']):
            return nc
        cv = Carver(ws[:])
        f32s = [cv.take([1024], F32) for _ in range(4)]
        b16s = [cv.take([1024], BF16) for _ in range(4)]
        b16t = [cv.take([8, 128], BF16) for _ in range(2)]
        ri = [0]

        def ring(n):
            i = ri[0] % n
            ri[0] += 1
            return i

        for g in range(4):
            i = ring(4)
            P.dma("sync", f32s[i][:, 0:128], w_s[g], w=[f"ws:f32s{i}"], sem=f"f32s{i}")
            V("tensor_tensor", [f"ws:f32s{i}", "trilm"], [f"ws:f32s{i}"], out=f32s[i][:, 0:128], in0=f32s[i][:, 0:128],
              in1=trilm[:], op=ALU.mult)
            TR([f"ws:f32s{i}", "identf"], ["ws:pb6"], out=pb[6][:, 0:128], in_=f32s[i][:, 0:128], identity=identf[:])
            V("tensor_copy", ["ws:pb6"], [f"wsT{g}"], out=wsT[:, g, :], in_=pb[6][:, 0:128])
        for hp in range(16):
            i = ring(4)
            P.dma("sync", f32s[i][:, 0:128], peer_keys[hp], w=[f"ws:f32s{i}"], sem=f"f32s{i}")
            TR([f"ws:f32s{i}", "identf"], ["ws:pb6"], out=pb[6][:, 0:128], in_=f32s[i][:, 0:128], identity=identf[:])
            V("tensor_copy", ["ws:pb6"], [f"keysT{hp}"], out=keysT[:, hp, :], in_=pb[6][:, 0:128])
        for c in range(8):
            for half in range(2):
                i = ring(4)
                P.dma("sync", f32s[i][:], peer_wq[c * 128:(c + 1) * 128, half * 1024:(half + 1) * 1024],
                      w=[f"ws:f32s{i}"], sem=f"f32s{i}")
                V("tensor_scalar", [f"ws:f32s{i}", "gfc"], [f"ws:b16s{i}"], out=b16s[i][:], in0=f32s[i][:],
                  scalar1=gfc[:, c:c + 1], scalar2=None, op0=ALU.mult)
                P.dma("gpsimd", wqb_d[half * 8:(half + 1) * 8, :, c, :].rearrange("h p n -> p h n"),
                      b16s[i][:].rearrange("p (h n) -> p h n", h=8), r=[f"ws:b16s{i}"], w=["wqb_d"],
                      sem=f"b16w{i}")
        if stop_here('p0a', gfin[:], ['gfin','wqb_d'] + [f'keysT{i}' for i in range(16)]):
            return nc
        n_et = 128
        for et in range(n_et):
            i = ring(4)
            P.dma("sync", f32s[i][:], peer_wu[et * 128:(et + 1) * 128, :], w=[f"ws:f32s{i}"], sem=f"f32s{i}")
            A([f"ws:f32s{i}"], [f"ws:b16s{i}"], out=b16s[i][:], in_=f32s[i][:], func=AF.Copy)
            pbk = 6 + (et % 2)
            for c in range(8):
                TR([f"ws:b16s{i}", "identb"], [f"ws:pb{pbk}"], out=pbb[pbk][:, c * 128:(c + 1) * 128],
                   in_=b16s[i][:, c * 128:(c + 1) * 128], identity=identb[:])
            j = et % 2
            V("tensor_tensor", [f"ws:pb{pbk}", "gfcb"], [f"ws:b16t{j}"], out=b16t[j][:],
              in0=pbb[pbk][:].rearrange("p (c e) -> p c e", c=8), in1=gfcb[:].unsqueeze(2).to_broadcast([128, 8, 128]),
              op=ALU.mult)
            P.dma("gpsimd", wuT_d[et], b16t[j][:].rearrange("p c e -> p (c e)"), r=[f"ws:b16t{j}"], w=[f"wuT{et}"],
                  sem=f"b16tw{j}")
            i = ring(4)
            P.dma("sync", f32s[i][:], peer_wv[et * 128:(et + 1) * 128, :], w=[f"ws:f32s{i}"], sem=f"f32s{i}")
            V("tensor_copy", [f"ws:f32s{i}"], [f"ws:b16s{i}"], out=b16s[i][:], in_=f32s[i][:])
            P.dma("gpsimd", wvb_d[et], b16s[i][:], r=[f"ws:b16s{i}"], w=[f"wvb{et}"], sem=f"b16w{i}")

        if stop_here('p0', gfin[:], ['gfin'] + [f'wuT{i}' for i in range(128)] + [f'wvb{i}' for i in range(128)]):
            return nc
        P.fence()
        cv = Carver(ws[:])
        winT = cv.take([8, 2560], BF16)
        woutT = cv.take([8, 1024], BF16)
        hT = cv.take([8, S], BF16)
        qkT = [[cv.take([S], BF16) for _ in range(2)] for _ in range(2)]
        Vaug = cv.take([NT, 4, 130], BF16)
        mix = cv.take([NT, 1024], BF16)
        xin = [cv.take([1024], F32) for _ in range(2)]
        hb = [cv.take([1024], BF16) for _ in range(2)]
        ug = [cv.take([512], F32) for _ in range(2)]
        gvg = [cv.take([512], F32) for _ in range(2)]
        gvn = [cv.take([512], BF16) for _ in range(2)]
        t5a = [cv.take([512], F32) for _ in range(2)]
        pT = [cv.take([128], BF16) for _ in range(4)]
        dtmp = [cv.take([128], F32) for _ in range(2)]
        osb = [cv.take([128], F32) for _ in range(2)]
        mixT = [cv.take([8, 128], BF16) for _ in range(2)]
        print("phase1 ws used", cv.off, "of", cv.cap)

        ri[0] = 0
        for c in range(8):
            for (a, b) in ((0, 1024), (1024, 2048), (2048, 2560)):
                i = ring(2)
                P.dma("sync", xin[i][:, 0:b - a], w_in[c * 128:(c + 1) * 128, a:b], w=[f"ws:xin{i}"], sem=f"xin{i}")
                V("tensor_scalar", [f"ws:xin{i}", "gmc"], [f"ws:winT{c}"], out=winT[:, c, a:b], in0=xin[i][:, 0:b - a],
                  scalar1=gmc[:, c:c + 1], scalar2=None, op0=ALU.mult)
        for c in range(8):
            i = ring(2)
            P.dma("sync", xin[i][:], w_out[c * 128:(c + 1) * 128, :], w=[f"ws:xin{i}"], sem=f"xin{i}")
            A([f"ws:xin{i}"], [f"ws:woutT{c}"], out=woutT[:, c, :], in_=xin[i][:], func=AF.Copy)
        winK = [f"ws:winT{c}" for c in range(8)]
        woutK = [f"ws:woutT{c}" for c in range(8)]
        V("memset", [], ["ws:vones"], ap=Vaug[:, :, :, 128:130], constant=1.0)

        for seq in range(nseq):
            r0 = seq * S
            for tt in range(NT):
                i = tt % 2
                P.dma("sync", xin[i][:], x[r0 + tt * 128:r0 + (tt + 1) * 128, :], w=[f"ws:xin{i}"], sem=f"xin{i}")
                ss, ssk = stcol()
                A([f"ws:xin{i}"], [f"ws:hb{i}", ssk], out=hb[i][:], in_=xin[i][:], func=AF.Square, accum_out=ss)
                rs, rsk = stcol()
                rstd_from_ss(ss, ssk, 1, 1.0 / D, rs, rsk)
                V("tensor_scalar", [f"ws:xin{i}", rsk], [f"ws:hb{i}"], out=hb[i][:], in0=xin[i][:], scalar1=rs,
                  scalar2=None, op0=ALU.mult)
                pbk = 6 + i
                for c in range(8):
                    TR([f"ws:hb{i}", "identb"], [f"ws:pb{pbk}"], out=pbb[pbk][:, c * 128:(c + 1) * 128],
                       in_=hb[i][:, c * 128:(c + 1) * 128], identity=identb[:])
                A([f"ws:pb{pbk}"], [f"ws:hT{tt}"], out=hT[:, :, tt * 128:(tt + 1) * 128],
                  in_=pbb[pbk][:].rearrange("p (c t) -> p c t", c=8), func=AF.Copy)
            for tt in range(NT):
                i = tt % 2
                for n in range(3):
                    for c in range(8):
                        MM([f"ws:hT{tt}", winK[c]], [f"ws:pb{n}"], out=pb[n][:, :], lhsT=hT[:, c, tt * 128:(tt + 1) * 128],
                           rhs=winT[:, c, 1024 + n * 512:1024 + (n + 1) * 512], start=(c == 0), stop=(c == 7))
                V("tensor_copy", ["ws:pb0"], [f"ws:V{tt}"], out=Vaug[:, tt, :, 0:128],
                  in_=pb[0][:, :].rearrange("p (h e) -> p h e", h=4))
                A(["ws:pb1"], [f"ws:ug{i}"], out=ug[i][:], in_=pb[1][:, :], func=AF.Gelu_apprx_tanh)
                A(["ws:pb2"], [f"ws:gvg{i}"], out=gvg[i][:], in_=pb[2][:, :], func=AF.Gelu_apprx_tanh)
                V("tensor_tensor", [f"ws:gvg{i}"], [f"ws:t5a{i}"], out=t5a[i][:], in0=gvg[i][:], in1=gvg[i][:], op=ALU.mult)
                s4, s4k = stcol(4)
                V("tensor_reduce", [f"ws:t5a{i}"], [s4k], out=s4, in_=t5a[i][:].rearrange("p (g c) -> p g c", g=4),
                  axis=AX.X, op=ALU.add)
                r4, r4k = stcol(4)
                rstd_from_ss(s4, s4k, 4, 1.0 / 128, r4, r4k)
                V("tensor_tensor", [f"ws:gvg{i}", r4k], [f"ws:t5a{i}"], out=t5a[i][:].rearrange("p (g c) -> p g c", g=4),
                  in0=gvg[i][:].rearrange("p (g c) -> p g c", g=4), in1=r4.unsqueeze(2).to_broadcast([128, 4, 128]),
                  op=ALU.mult)
                V("tensor_tensor", [f"ws:t5a{i}", "ggv"], [f"ws:gvn{i}"], out=gvn[i][:], in0=t5a[i][:], in1=ggv[:], op=ALU.mult)
                for g in range(4):
                    MM([f"ws:gvn{i}", f"wsT{g}"], ["ws:pb3"], out=pb[3][:, g * 128:(g + 1) * 128], lhsT=wsT[:, g, :],
                       rhs=gvn[i][:, g * 128:(g + 1) * 128], start=True, stop=True)
                for g in range(4):
                    V("scalar_tensor_tensor", ["ws:pb3", "bsT", f"ws:ug{i}"], [f"ws:t5a{i}"],
                      out=t5a[i][:, g * 128:(g + 1) * 128], in0=pb[3][:, g * 128:(g + 1) * 128], scalar=bsT[:, g:g + 1],
                      in1=ug[i][:, g * 128:(g + 1) * 128], op0=ALU.add, op1=ALU.mult)
                V("tensor_tensor", [f"ws:t5a{i}"], [f"ws:gvg{i}"], out=gvg[i][:], in0=t5a[i][:], in1=t5a[i][:], op=ALU.mult)
                s4, s4k = stcol(4)
                V("tensor_reduce", [f"ws:gvg{i}"], [s4k], out=s4, in_=gvg[i][:].rearrange("p (g c) -> p g c", g=4),
                  axis=AX.X, op=ALU.add)
                r4, r4k = stcol(4)
                rstd_from_ss(s4, s4k, 4, 1.0 / 128, r4, r4k)
                V("tensor_tensor", [f"ws:t5a{i}", r4k], [f"ws:gvg{i}"], out=gvg[i][:].rearrange("p (g c) -> p g c", g=4),
                  in0=t5a[i][:].rearrange("p (g c) -> p g c", g=4), in1=r4.unsqueeze(2).to_broadcast([128, 4, 128]),
                  op=ALU.mult)
                V("tensor_tensor", [f"ws:gvg{i}", "ggo"], [f"ws:mixg{tt}"], out=mix[:, tt, 512:1024], in0=gvg[i][:],
                  in1=ggo[:], op=ALU.mult)
            pair = 0
            for h in range(4):
                sl = h % 2
                for which in range(2):
                    for tg in range(4):
                        pbk = 6 + (tg % 2)
                        for c in range(8):
                            MM([f"ws:hT{t}" for t in range(tg * 4, tg * 4 + 4)] + [winK[c]], [f"ws:pb{pbk}"],
                               out=pb[pbk][:, :], lhsT=winT[:, c, which * 512 + h * 128:which * 512 + (h + 1) * 128],
                               rhs=hT[:, c, tg * 512:(tg + 1) * 512], start=(c == 0), stop=(c == 7))
                        if tg % 2 == 0:
                            A([f"ws:pb{pbk}"], [f"ws:qk{sl}{which}_{tg}"], out=qkT[sl][which][:, tg * 512:(tg + 1) * 512],
                              in_=pb[pbk][:, :], func=AF.Copy)
                        else:
                            V("tensor_copy", [f"ws:pb{pbk}"], [f"ws:qk{sl}{which}_{tg}"],
                              out=qkT[sl][which][:, tg * 512:(tg + 1) * 512], in_=pb[pbk][:, :])
                qT_, kT_ = qkT[sl][0], qkT[sl][1]
                for qt in range(NT):
                    obs = []
                    for m in range(2):
                        ob = 2 + (qt % 2) * 2 + m
                        obs.append(ob)
                        for j in range(qt + 1):
                            sslot = pair % 8
                            pair += 1
                            sk = f"ws:ps{sslot}"
                            sview = pb[sslot // 4][:, (sslot % 4) * 128:(sslot % 4 + 1) * 128]
                            MM([f"ws:qk{sl}0_{qt // 4}", f"ws:qk{sl}1_{j // 4}"], [sk], out=sview,
                               lhsT=kT_[m * 64:(m + 1) * 64, j * 128:(j + 1) * 128],
                               rhs=qT_[m * 64:(m + 1) * 64, qt * 128:(qt + 1) * 128], start=True, stop=True)
                            ps_ = sslot % 4
                            if j < qt:
                                A([sk, f"bcol{h}"], [f"ws:pT{ps_}"], out=pT[ps_][:], in_=sview, func=AF.Exp,
                                  bias=bcol[:, h, qt - j:qt - j + 1], scale=0.125)
                            else:
                                dsl = m
                                V("scalar_tensor_tensor", [sk, f"MdT{h}"], [f"ws:dtmp{dsl}"], out=dtmp[dsl][:],
                                  in0=sview, scalar=0.125, in1=MdT[:, h, :], op0=ALU.mult, op1=ALU.add)
                                A([f"ws:dtmp{dsl}"], [f"ws:pT{ps_}"], out=pT[ps_][:], in_=dtmp[dsl][:], func=AF.Exp)
                            MM([f"ws:pT{ps_}", f"ws:V{j}", "ws:vones"], [f"ws:pb{ob}"], out=pb[ob][:, 0:129],
                               lhsT=pT[ps_][:], rhs=Vaug[:, j, h, 0:129], start=(j == 0), stop=(j == qt))
                    o1, o2 = obs
                    osl = qt % 2
                    rz, rzk = stcol(2)
                    V("reciprocal", [f"ws:pb{o1}"], [rzk], out=rz[:, 0:1], in_=pb[o1][:, 128:129])
                    rz2, rz2k = stcol(2)
                    V("reciprocal", [f"ws:pb{o2}"], [rz2k], out=rz2[:, 0:1], in_=pb[o2][:, 128:129])
                    V("tensor_tensor", [rz2k, "neglam"], [rz2k], out=rz2[:, 1:2], in0=rz2[:, 0:1], in1=neglam,
                      op=ALU.mult)
                    V("tensor_scalar", [f"ws:pb{o1}", rzk], [f"ws:osb{osl}"], out=osb[osl][:], in0=pb[o1][:, 0:128],
                      scalar1=rz[:, 0:1], scalar2=None, op0=ALU.mult)
                    V("scalar_tensor_tensor", [f"ws:pb{o2}", rz2k, f"ws:osb{osl}"], [f"ws:osb{osl}"],
                      out=osb[osl][:], in0=pb[o2][:, 0:128], scalar=rz2[:, 1:2], in1=osb[osl][:], op0=ALU.mult,
                      op1=ALU.add)
                    sso, ssok = stcol()
                    A([f"ws:osb{osl}"], [f"ws:dtmp{osl}", ssok], out=dtmp[osl][:], in_=osb[osl][:], func=AF.Square,
                      accum_out=sso)
                    ro, rok = stcol()
                    rstd_from_ss(sso, ssok, 1, 1.0 / 128, ro, rok)
                    V("scalar_tensor_tensor", [f"ws:osb{osl}", rok, "gsub"], [f"ws:mixa{qt}_{h}"],
                      out=mix[:, qt, h * 128:(h + 1) * 128], in0=osb[osl][:], scalar=ro, in1=gsub[:], op0=ALU.mult,
                      op1=ALU.mult)
            for tt in range(NT):
                i = tt % 2
                pbk = 6 + i
                mk = [f"ws:mixa{tt}_{h}" for h in range(4)] + [f"ws:mixg{tt}"]
                for c in range(8):
                    TR(mk + ["identb"], [f"ws:pb{pbk}"], out=pbb[pbk][:, c * 128:(c + 1) * 128],
                       in_=mix[:, tt, c * 128:(c + 1) * 128], identity=identb[:])
                A([f"ws:pb{pbk}"], [f"ws:mixT{i}"], out=mixT[i][:], in_=pbb[pbk][:].rearrange("p (c t) -> p c t", c=8),
                  func=AF.Copy)
                P.dma("sync", xin[i][:], x[r0 + tt * 128:r0 + (tt + 1) * 128, :], w=[f"ws:xin{i}"], sem=f"xin{i}")
                for n in range(2):
                    for c in range(8):
                        MM([f"ws:mixT{i}", woutK[c]], [f"ws:pb{n}"], out=pb[n][:, :], lhsT=mixT[i][:, c, :],
                           rhs=woutT[:, c, n * 512:(n + 1) * 512], start=(c == 0), stop=(c == 7))
                for n in range(2):
                    V("tensor_tensor", [f"ws:pb{n}", f"ws:xin{i}"], [f"ws:xin{i}"], out=xin[i][:, n * 512:(n + 1) * 512],
                      in0=pb[n][:, :], in1=xin[i][:, n * 512:(n + 1) * 512], op=ALU.add)
                P.dma("gpsimd", x1s[r0 + tt * 128:r0 + (tt + 1) * 128, :], xin[i][:], r=[f"ws:xin{i}"],
                      w=[f"x1s{seq}_{tt}"], sem=f"x1w{i}")

        if stop_after == 'p1':
            o = P.dma("sync", out[0:S, :], x1s[0:S, :], r=[f'x1s0_{i}' for i in range(16)], sem="stopout")
            P.emit(final_wait_ops=[o])
            return nc
        P.fence()
        cv = Carver(ws[:])
        AT = cv.take([128, 256], BF16)
        xnT = cv.take([8, 256], BF16)
        qT2 = cv.take([16, 256], BF16)
        x1t = [cv.take([1024], F32) for _ in range(2)]
        xnb = [cv.take([1024], BF16) for _ in range(2)]
        wqt = [cv.take([8, 128], BF16) for _ in range(2)]
        scores = cv.take([16, 128], F32)
        vals = cv.take([16, 16], F32)
        idxu = cv.take([16, 16], U32)
        idxf = cv.take([16, 16], F32)
        cand = cv.take([8, 256], F32)
        best = cv.take([8, 16], F32)
        fidx = cv.take([8, 16], U32)
        r1u = cv.take([8, 16], U32)
        r2u = cv.take([8, 16], U32)
        r1f = cv.take([8, 16], F32)
        r2f = cv.take([8, 16], F32)
        oht = cv.take([8, 16, 16], F32)
        oht2 = cv.take([8, 16, 16], F32)
        i1f = cv.take([8, 16], F32)
        i2f = cv.take([8, 16], F32)
        gf = cv.take([8, 16], F32)
        ex = cv.take([8, 16], F32)
        i1T = cv.take([256], BF16)
        i2T = cv.take([256], BF16)
        gT = cv.take([256], BF16)
        TGK = 16
        OH1 = [cv.take([TGK, 128], BF16) for _ in range(2)]
        E1w = [cv.take([TGK, 128], BF16) for _ in range(2)]
        OH2 = [cv.take([TGK, 128], BF16) for _ in range(2)]
        wu = [cv.take([8, 128], BF16) for _ in range(4)]
        wv = [cv.take([1024], BF16) for _ in range(4)]
        gl = [cv.take([256], BF16) for _ in range(2)]
        GT = [cv.take([256], BF16) for _ in range(2)]
        outsb = [cv.take([1024], F32)] * 2
        print("phase2 ws used", cv.off, "of", cv.cap)

        out_ops = []
        nblk = NTOK // 256
        wring = 0
        hring = 0
        for b in range(nblk):
            R0 = b * 256
            seq = R0 // S
            for tt in range(2):
                gtt = (R0 % S) // 128 + tt
                P.dma("sync", x1t[tt][:], x1s[R0 + tt * 128:R0 + (tt + 1) * 128, :], r=[f"x1s{seq}_{gtt}"],
                      w=[f"ws:x1t{tt}"], sem=f"x1t{tt}")
                ss, ssk = stcol()
                A([f"ws:x1t{tt}"], [f"ws:xnb{tt}", ssk], out=xnb[tt][:], in_=x1t[tt][:], func=AF.Square, accum_out=ss)
                rs, rsk = stcol()
                rstd_from_ss(ss, ssk, 1, 1.0 / D, rs, rsk)
                V("tensor_scalar", [f"ws:x1t{tt}", rsk], [f"ws:xnb{tt}"], out=xnb[tt][:], in0=x1t[tt][:], scalar1=rs,
                  scalar2=None, op0=ALU.mult)
                pbk = 6 + tt
                for c in range(8):
                    TR([f"ws:xnb{tt}", "identb"], [f"ws:pb{pbk}"], out=pbb[pbk][:, c * 128:(c + 1) * 128],
                       in_=xnb[tt][:, c * 128:(c + 1) * 128], identity=identb[:])
                A([f"ws:pb{pbk}"], [f"ws:xnT{tt}"], out=xnT[:, :, tt * 128:(tt + 1) * 128],
                  in_=pbb[pbk][:].rearrange("p (c t) -> p c t", c=8), func=AF.Copy)
            xnK = ["ws:xnT0", "ws:xnT1"]
            if b == 0 and stop_here("p2F", x1t[0][:], ["ws:x1t0"] + xnK):
                return nc
            for hp in range(16):
                wi = hp % 2
                P.dma("sync", wqt[wi][:], wqb_d[hp], r=["wqb_d"], w=[f"ws:wqt{wi}"], sem=f"wqt{wi}")
                hs = hp % 2
                hv = pb[4 + hs][:, 0:256]
                hk = f"ws:ph{hs}"
                for c in range(8):
                    MM(xnK + [f"ws:wqt{wi}"], [hk], out=hv, lhsT=wqt[wi][:, c, :], rhs=xnT[:, c, :], start=(c == 0),
                       stop=(c == 7))
                if hp % 2 == 0 or os.environ.get("KVAR") == "A":
                    A([hk], [f"ws:qT2_{hp}"], out=qT2[:, hp, :], in_=hv, func=AF.Copy)
                else:
                    V("tensor_copy", [hk], [f"ws:qT2_{hp}"], out=qT2[:, hp, :], in_=hv)
                if b == 0 and stop_here(f"p2G_{hp}", x1t[0][:], ["ws:x1t0", f"ws:qT2_{hp}"]):
                    return nc
            if b == 0 and stop_here("p2G0", x1t[0][:], ["ws:x1t0"] + [f"ws:qT2_{hp}" for hp in range(16)]):
                return nc
            for tt in range(2):
                for hp in range(16):
                    MM([f"ws:qT2_{hp}", f"keysT{hp}"], [f"ws:pb{hp // 4}"], out=pb[hp // 4][:, (hp % 4) * 128:(hp % 4 + 1) * 128],
                       lhsT=qT2[:, hp, tt * 128:(tt + 1) * 128], rhs=keysT[:, hp, :], start=True, stop=True)
                for q4 in range(4):
                    A([f"ws:pb{q4}"], [f"ws:sc{q4}"], out=scores[:, q4 * 4:(q4 + 1) * 4, :],
                      in_=pb[q4][:, :].rearrange("p (a n) -> p a n", a=4), func=AF.Copy)
                if b == 0 and tt == 0 and stop_here("p2G", scores[:].rearrange("p a n -> p (a n)")[:, 0:1024], [f"ws:sc{q}" for q in range(4)]):
                    return nc
                for hp in range(16):
                    sk = f"ws:sc{hp // 4}"
                    sc = scores[:, hp, :]
                    V("max", [sk], [f"ws:vals{hp}a"], out=vals[:, hp, 0:8], in_=sc)
                    V("max_index", [sk, f"ws:vals{hp}a"], [f"ws:idx{hp}a"], out=idxu[:, hp, 0:8], in_max=vals[:, hp, 0:8],
                      in_values=sc)
                    V("match_replace", [sk, f"ws:vals{hp}a"], [sk], out=sc, in_to_replace=vals[:, hp, 0:8], in_values=sc,
                      imm_value=-1e30)
                    V("max", [sk], [f"ws:vals{hp}b"], out=vals[:, hp, 8:16], in_=sc)
                    V("max_index", [sk, f"ws:vals{hp}b"], [f"ws:idx{hp}b"], out=idxu[:, hp, 8:16], in_max=vals[:, hp, 8:16],
                      in_values=sc)
                valK = [f"ws:vals{hp}{ab}" for hp in range(16) for ab in "ab"]
                idxK = [f"ws:idx{hp}{ab}" for hp in range(16) for ab in "ab"]
                V("tensor_copy", idxK, ["ws:idxf"], out=idxf[:], in_=idxu[:])
                v4 = vals[:].rearrange("p (h t) k -> p h t k", t=2)
                V("tensor_tensor", valK, ["ws:cand"], out=cand[:].rearrange("p h (a b) -> p h a b", a=16),
                  in0=v4[:, :, 0, :].unsqueeze(3).to_broadcast([128, 8, 16, 16]),
                  in1=v4[:, :, 1, :].unsqueeze(2).to_broadcast([128, 8, 16, 16]), op=ALU.add)
                for h in range(8):
                    ck = f"ws:cand{h}"
                    dep = ["ws:cand"]
                    V("max", dep, [f"ws:best{h}a"], out=best[:, h, 0:8], in_=cand[:, h, :])
                    V("max_index", dep + [f"ws:best{h}a"], [f"ws:fidx{h}a"], out=fidx[:, h, 0:8], in_max=best[:, h, 0:8],
                      in_values=cand[:, h, :])
                    V("match_replace", dep + [f"ws:best{h}a"], [ck], out=cand[:, h, :], in_to_replace=best[:, h, 0:8],
                      in_values=cand[:, h, :], imm_value=-1e30)
                    V("max", [ck], [f"ws:best{h}b"], out=best[:, h, 8:16], in_=cand[:, h, :])
                    V("max_index", [ck, f"ws:best{h}b"], [f"ws:fidx{h}b"], out=fidx[:, h, 8:16], in_max=best[:, h, 8:16],
                      in_values=cand[:, h, :])
                bestK = [f"ws:best{h}{ab}" for h in range(8) for ab in "ab"]
                fidxK = [f"ws:fidx{h}{ab}" for h in range(8) for ab in "ab"]
                candK = [f"ws:cand{h}" for h in range(8)]
                V("tensor_tensor", bestK, ["ws:ex"], out=ex[:], in0=best[:],
                  in1=best[:, :, 0:1].to_broadcast([128, 8, 16]), op=ALU.subtract)
                A(["ws:ex"], ["ws:ex2"], out=ex[:], in_=ex[:], func=AF.Exp)
                z8, z8k = stcol(8) if False else (None, None)
                zt, ztk = stat[:, 48:56], "statz"
                V("tensor_reduce", ["ws:ex2"], [ztk], out=zt, in_=ex[:], axis=AX.X, op=ALU.add)
                zr, zrk = stat[:, 56:64], "statzr"
                V("reciprocal", [ztk], [zrk], out=zr, in_=zt)
                V("tensor_tensor", ["ws:ex2", zrk], ["ws:gf"], out=gf[:], in0=ex[:],
                  in1=zr.unsqueeze(2).to_broadcast([128, 8, 16]), op=ALU.mult)
                V("tensor_single_scalar", fidxK, ["ws:r1u"], out=r1u[:], in_=fidx[:], scalar=4, op=ALU.logical_shift_right)
                V("tensor_single_scalar", fidxK, ["ws:r2u"], out=r2u[:], in_=fidx[:], scalar=15, op=ALU.bitwise_and)
                V("tensor_copy", ["ws:r1u"], ["ws:r1f"], out=r1f[:], in_=r1u[:])
                V("tensor_copy", ["ws:r2u"], ["ws:r2f"], out=r2f[:], in_=r2u[:])
                idx4 = idxf[:].rearrange("p (h t) k -> p h t k", t=2)
                io16 = iorow[:, 0:16].unsqueeze(1).unsqueeze(1).to_broadcast([128, 8, 16, 16])
                for (rf, rk, tsel, dst, dk) in ((r1f, "ws:r1f", 0, i1f, "ws:i1f"), (r2f, "ws:r2f", 1, i2f, "ws:i2f")):
                    V("tensor_tensor", [rk, "iorow"], ["ws:oht"], out=oht[:],
                      in0=rf[:].unsqueeze(3).to_broadcast([128, 8, 16, 16]), in1=io16, op=ALU.is_equal)
                    V("tensor_tensor", ["ws:oht", "ws:idxf"], ["ws:oht2"], out=oht2[:], in0=oht[:],
                      in1=idx4[:, :, tsel, :].unsqueeze(2).to_broadcast([128, 8, 16, 16]), op=ALU.mult)
                    V("tensor_reduce", ["ws:oht2"], [dk], out=dst[:], in_=oht2[:], axis=AX.X, op=ALU.add)
                if b == 0 and tt == 0 and stop_here("p2H", gf[:].rearrange("p h k -> p (h k)"), ["ws:gf", "ws:i1f", "ws:i2f"]):
                    return nc
                if b == 0 and tt == 0 and stop_here("p2H1", i1f[:].rearrange("p h k -> p (h k)"), ["ws:gf", "ws:i1f", "ws:i2f"]):
                    return nc
                if b == 0 and tt == 0 and stop_here("p2H2", i2f[:].rearrange("p h k -> p (h k)"), ["ws:gf", "ws:i1f", "ws:i2f"]):
                    return nc
                for (src, sk_, dstT, dk) in ((i1f, "ws:i1f", i1T, "ws:i1T"), (i2f, "ws:i2f", i2T, "ws:i2T"),
                                             (gf, "ws:gf", gT, "ws:gT")):
                    TR([sk_, "identf"], ["ws:pb7"], out=pb[7][:, 0:128], in_=src[:].rearrange("p h k -> p (h k)"),
                       identity=identf[:])
                    V("tensor_copy", ["ws:pb7"], [dk + str(tt)], out=dstT[:, tt * 128:(tt + 1) * 128], in_=pb[7][:, 0:128])
            io_b = iob[:].unsqueeze(1).to_broadcast([128, TGK, 128])
            for g in range(256 // TGK):
                sl = g % 2
                tt = (g * TGK) // 128
                tsl = slice(g * TGK, (g + 1) * TGK)
                V("tensor_tensor", ["iob", f"ws:i2T{tt}"], [f"ws:OH2{sl}"], out=OH2[sl][:], in0=io_b,
                  in1=i2T[:, tsl].unsqueeze(2).to_broadcast([128, TGK, 128]), op=ALU.is_equal)
                V("tensor_tensor", ["iob", f"ws:i1T{tt}"], [f"ws:OH1{sl}"], out=OH1[sl][:], in0=io_b,
                  in1=i1T[:, tsl].unsqueeze(2).to_broadcast([128, TGK, 128]), op=ALU.is_equal)
                V("tensor_tensor", [f"ws:OH1{sl}", f"ws:gT{tt}"], [f"ws:E1w{sl}"], out=E1w[sl][:], in0=OH1[sl][:],
                  in1=gT[:, tsl].unsqueeze(2).to_broadcast([128, TGK, 128]), op=ALU.mult)
                for k4 in range(TGK // 4):
                    pbk = 6 + (k4 % 2)
                    for kk in range(4):
                        k = k4 * 4 + kk
                        MM([f"ws:OH2{sl}", f"ws:E1w{sl}"], [f"ws:pb{pbk}"], out=pb[pbk][:, kk * 128:(kk + 1) * 128],
                           lhsT=OH2[sl][:, k, :], rhs=E1w[sl][:, k, :], start=True, stop=True)
                    t0 = g * TGK + k4 * 4
                    A([f"ws:pb{pbk}"], ["ws:AT"], out=AT[:, :, t0:t0 + 4].rearrange("p i t -> p t i"),
                      in_=pb[pbk][:, :].rearrange("p (t i) -> p t i", t=4), func=AF.Copy)
            if b == 0 and stop_here("p2J", x1t[0][:], ["ws:AT", "ws:x1t0"]):
                return nc
            for et in range(128):
                wi = wring % 4
                wring += 1
                P.dma("sync", wu[wi][:].rearrange("p c e -> p (c e)"), wuT_d[et], r=[f"wuT{et}"], w=[f"ws:wu{wi}"],
                      sem=f"wu{wi}")
                P.dma("sync", wv[wi][:], wvb_d[et], r=[f"wvb{et}"], w=[f"ws:wv{wi}"], sem=f"wv{wi}")
                hs = hring % 2
                hring += 1
                hv = pb[4 + hs][:, 0:256]
                hk = f"ws:ph{hs}"
                for c in range(8):
                    MM(xnK + [f"ws:wu{wi}"], [hk], out=hv, lhsT=wu[wi][:, c, :], rhs=xnT[:, c, :], start=(c == 0),
                       stop=(c == 7))
                gs = et % 2
                A([hk], [f"ws:gl{gs}"], out=gl[gs][:], in_=hv, func=AF.Gelu_apprx_tanh)
                V("tensor_tensor", [f"ws:gl{gs}", "ws:AT"], [f"ws:GT{gs}"], out=GT[gs][:], in0=gl[gs][:], in1=AT[:, et, :],
                  op=ALU.mult)
                for tt in range(2):
                    for dh in range(2):
                        MM([f"ws:GT{gs}", f"ws:wv{wi}"], [f"ws:pb{tt * 2 + dh}"], out=pb[tt * 2 + dh][:, :],
                           lhsT=GT[gs][:, tt * 128:(tt + 1) * 128], rhs=wv[wi][:, dh * 512:(dh + 1) * 512],
                           start=(et == 0), stop=(et == 127))
            if b == 0 and stop_here("p2K", x1t[0][:], ["ws:x1t0"] + [f"ws:pb{i}" for i in range(4)]):
                return nc
            for tt in range(2):
                for dh in range(2):
                    V("tensor_tensor", [f"ws:pb{tt * 2 + dh}", f"ws:x1t{tt}"], [f"ws:x1t{tt}"],
                      out=x1t[tt][:, dh * 512:(dh + 1) * 512], in0=pb[tt * 2 + dh][:, :],
                      in1=x1t[tt][:, dh * 512:(dh + 1) * 512], op=ALU.add)
                ss, ssk = stcol()
                A([f"ws:x1t{tt}"], ["ws:outsb", ssk], out=outsb[tt][:], in_=x1t[tt][:], func=AF.Square, accum_out=ss)
                rs, rsk = stcol()
                rstd_from_ss(ss, ssk, 1, 1.0 / D, rs, rsk)
                V("scalar_tensor_tensor", [f"ws:x1t{tt}", rsk, "gfin"], ["ws:outsb"], out=outsb[tt][:],
                  in0=x1t[tt][:], scalar=rs, in1=gfin[:], op0=ALU.mult, op1=ALU.mult)
                out_ops.append(P.dma("gpsimd", out[R0 + tt * 128:R0 + (tt + 1) * 128, :], outsb[tt][:],
                                     r=["ws:outsb"], sem="outw"))
        print("n ops", len(P.ops))
        P.emit(final_wait_ops=out_ops)
    return nc


def kernel(x, w_in, lam_q1, lam_k1, lam_q2, lam_k2, g_subln, w_s, b_s, g_gv, g_gout, w_out, g_mix, g_ffn,
           peer_wq, peer_keys, peer_wu, peer_wv, g_final):
    f = lambda a: np.ascontiguousarray(np.asarray(a, dtype=np.float32))
    x = f(x)
    B = x.shape[0]
    nseq = B // NCORES
    nc = build_nc(nseq)
    common = {
        "w_in": f(w_in[0]),
        "lamv": f(np.stack([np.asarray(lam_q1)[0], np.asarray(lam_k1)[0], np.asarray(lam_q2)[0], np.asarray(lam_k2)[0]])),
        "g_subln": f(g_subln[0]), "w_s": f(w_s[0]), "b_s": f(b_s[0]), "g_gv": f(g_gv[0]), "g_gout": f(g_gout[0]),
        "w_out": f(w_out[0]), "g_mix": f(g_mix[0]), "g_ffn": f(g_ffn[0]), "peer_wq": f(peer_wq[0]),
        "peer_keys": f(np.asarray(peer_keys)[0].reshape(16, 128, 128)), "peer_wu": f(peer_wu[0]),
        "peer_wv": f(peer_wv[0]), "g_final": f(g_final),
    }
    in_maps = []
    for c in range(NCORES):
        m = dict(common)
        m["x"] = x[c * nseq:(c + 1) * nseq].reshape(nseq * S, D)
        in_maps.append(m)
    res = run_bass_kernel_spmd(nc, in_maps, core_ids=list(range(NCORES)))
    outs = [np.asarray(r["out"]).reshape(nseq, S, D) for r in res.results]
    return np.concatenate(outs, axis=0).astype(np.float32)
```

```python
import numpy as np
from contextlib import ExitStack
import concourse.bass as bass
import concourse.mybir as mybir
from concourse.bass_utils import run_bass_kernel_spmd

F32 = mybir.dt.float32
BF16 = mybir.dt.bfloat16
U32 = mybir.dt.uint32
AF = mybir.ActivationFunctionType
ALU = mybir.AluOpType
AX = mybir.AxisListType

ENGS = ["sync", "scalar", "vector", "gpsimd", "tensor"]
EPS = 1e-6
NCORES = 8
D = 1024
S = 2048
NT = S // 128
NEG = -30000.0


class Prog:
    def __init__(self, nc):
        self.nc = nc
        self.ops = []
        self.last_w = {}
        self.readers = {}
        self.dma_sem_count = {}
        self.epoch = 0
        self.key_epoch = {}
        self.fence_ops = []
        self.last_eng_op = {}
        self.last_dma_op = {}

    def fence(self):
        self.epoch += 1
        self.fence_ops = list(self.last_eng_op.values()) + list(self.last_dma_op.values())

    def _add(self, eng, fn, r, w, dma_sem=None):
        idx = len(self.ops)
        pk = [k for k in r if k.startswith("ws:pb")]
        if pk:
            r = [k for k in r if not k.startswith("ws:pb")]
            w = list(w) + pk
        deps = set()
        for k in list(r) + list(w):
            if k.startswith("ws:") and self.key_epoch.get(k) != self.epoch:
                self.key_epoch[k] = self.epoch
                self.last_w.pop(k, None)
                self.readers.pop(k, None)
                deps.update(self.fence_ops)
        raw = set()
        for k in r:
            lw = self.last_w.get(k)
            if lw is not None:
                deps.add(lw)
                raw.add(lw)
        for k in w:
            lw = self.last_w.get(k)
            if lw is not None:
                deps.add(lw)
            deps.update(self.readers.get(k, ()))
        real = set()
        for d in deps:
            od = self.ops[d]
            if od["dma_sem"] is None and od["eng"] == eng and d not in raw:
                continue
            real.add(d)
        op = dict(eng=eng, fn=fn, deps=real, dma_sem=dma_sem, idx=idx)
        if dma_sem is not None:
            c = self.dma_sem_count.get(dma_sem, 0) + 16
            self.dma_sem_count[dma_sem] = c
            op["val"] = c
            self.last_dma_op[dma_sem] = idx
        else:
            self.last_eng_op[eng] = idx
        self.ops.append(op)
        for k in r:
            self.readers.setdefault(k, []).append(idx)
        for k in w:
            self.last_w[k] = idx
            self.readers[k] = []
        return idx

    def op(self, eng, fn, r=(), w=()):
        return self._add(eng, fn, r, w)

    def dma(self, eng, out, in_, r=(), w=(), sem="dma", **kw):
        def fn(e, out=out, in_=in_, kw=kw):
            return e.dma_start(out=out, in_=in_, **kw)
        return self._add(eng, fn, r, w, dma_sem=sem)

    def emit(self, final_wait_ops=()):
        nc = self.nc
        ops = self.ops
        needs_inc = [False] * len(ops)
        for o in ops:
            for d in o["deps"]:
                needs_inc[d] = True
        for d in final_wait_ops:
            needs_inc[d] = True
        cnt = {e: 0 for e in ENGS}
        for o in ops:
            if o["dma_sem"] is None:
                o["sem"] = "eng_" + o["eng"]
                if needs_inc[o["idx"]]:
                    cnt[o["eng"]] += 1
                    o["val"] = cnt[o["eng"]]
                else:
                    o["val"] = None
            else:
                o["sem"] = "dma_" + str(o["dma_sem"])
        semnames = ["eng_" + e for e in ENGS] + ["dma_" + str(k) for k in self.dma_sem_count]
        with ExitStack() as st:
            sems = {n: st.enter_context(nc.semaphore(n)) for n in semnames}
            block = st.enter_context(nc.Block())
            per_eng = {e: [o for o in ops if o["eng"] == e] for e in ENGS}

            def make(ename):
                def body(eng):
                    waited = {}
                    for o in per_eng[ename]:
                        for d in sorted(o["deps"]):
                            od = ops[d]
                            s, v = od["sem"], od["val"]
                            if waited.get(s, 0) >= v:
                                continue
                            eng.wait_ge(sems[s], v)
                            waited[s] = v
                        ins = o["fn"](eng)
                        if o["dma_sem"] is not None:
                            ins.then_inc(sems[o["sem"]], 16)
                        elif o["val"] is not None:
                            ins.then_inc(sems[o["sem"]], 1)
                    if ename == "sync":
                        for d in final_wait_ops:
                            od = ops[d]
                            if waited.get(od["sem"], 0) >= od["val"]:
                                continue
                            eng.wait_ge(sems[od["sem"]], od["val"])
                            waited[od["sem"]] = od["val"]
                return body

            block.sync(make("sync"))
            block.scalar(make("scalar"))
            block.vector(make("vector"))
            block.gpsimd(make("gpsimd"))
            block.tensor(make("tensor"))


class Carver:
    def __init__(self, base):
        self.base = base
        self.off = 0
        self.cap = base.shape[1]

    def take(self, shape, dtype):
        esz = 2 if dtype == BF16 else 4
        n = int(np.prod(shape)) * esz // 2
        n_al = (n + 15) // 16 * 16
        assert self.off + n_al <= self.cap, ("workspace overflow", self.off, n_al, self.cap)
        v = self.base[:, self.off:self.off + n]
        self.off += n_al
        if dtype != BF16:
            v = v.bitcast(dtype)
        if len(shape) == 2:
            v = v.rearrange("p (a b) -> p a b", a=shape[0])
        elif len(shape) == 3:
            v = v.rearrange("p (a b c) -> p a b c", a=shape[0], b=shape[1])
        return v


def build_nc(nseq, stop_after=None):
    import os
    stop_after = os.environ.get('KSTOP', stop_after)
    NTOK = nseq * S
    nc = bass.Bass("TRN2", target_bir_lowering=False)
    dt = lambda n, s, d=F32, kind="ExternalInput": nc.dram_tensor(n, s, d, kind=kind).ap()
    x = dt("x", [NTOK, D])
    w_in = dt("w_in", [D, 2560])
    lamv = dt("lamv", [4, 64])
    g_subln = dt("g_subln", [128])
    w_s = dt("w_s", [4, 128, 128])
    b_s = dt("b_s", [4, 128])
    g_gv = dt("g_gv", [512])
    g_gout = dt("g_gout", [512])
    w_out = dt("w_out", [D, D])
    g_mix = dt("g_mix", [D])
    g_ffn = dt("g_ffn", [D])
    peer_wq = dt("peer_wq", [D, 2048])
    peer_keys = dt("peer_keys", [16, 128, 128])
    peer_wu = dt("peer_wu", [16384, D])
    peer_wv = dt("peer_wv", [16384, D])
    g_final = dt("g_final", [D])
    out = dt("out", [NTOK, D], F32, "ExternalOutput")
    wuT_d = dt("wuT_d", [128, 128, 1024], BF16, "Internal")
    wvb_d = dt("wvb_d", [128, 128, 1024], BF16, "Internal")
    wqb_d = dt("wqb_d", [16, 128, 8, 128], BF16, "Internal")
    x1s = dt("x1s", [NTOK, D], F32, "Internal")

    WS_ELEMS = (212800 - 21600) // 2 // 16 * 16
    with ExitStack() as st:
        sb = lambda n, s, d: st.enter_context(nc.sbuf_tensor(n, s, d))
        ws = sb("ws", [128, WS_ELEMS], BF16)
        iotf = sb("iotf", [128, 128], F32)
        iorow = sb("iorow", [128, 128], F32)
        iob = sb("iob", [128, 128], BF16)
        identb = sb("identb", [128, 128], BF16)
        identf = sb("identf", [128, 128], F32)
        trilm = sb("trilm", [128, 128], F32)
        MdT = sb("MdT", [128, 4, 128], F32)
        bcol = sb("bcol", [128, 4, 16], F32)
        lamt = sb("lamt", [128, 4, 64], F32)
        lamw = sb("lamw", [128, 8], F32)
        ggv = sb("ggv", [128, 512], F32)
        ggo = sb("ggo", [128, 512], F32)
        gsub = sb("gsub", [128, 128], F32)
        gfin = sb("gfin", [128, 1024], F32)
        gmc = sb("gmc", [128, 8], F32)
        gfc = sb("gfc", [128, 8], F32)
        gfcb = sb("gfcb", [128, 8], BF16)
        bsT = sb("bsT", [128, 4], F32)
        wsT = sb("wsT", [128, 4, 128], BF16)
        keysT = sb("keysT", [128, 16, 128], BF16)
        stat = sb("stat", [128, 64], F32)
        pb = [st.enter_context(nc.psum_tensor(f"ws:pb{i}", [128, 512], F32)) for i in range(8)]
        pbb = [p[:].bitcast(BF16) for p in pb]

        P = Prog(nc)
        V = lambda name, r, w, **kw: P.op("vector", lambda e: getattr(e, name)(**kw), r, w)
        A = lambda r, w, **kw: P.op("scalar", lambda e: e.activation(**kw), r, w)
        G = lambda name, r, w, **kw: P.op("gpsimd", lambda e: getattr(e, name)(**kw), r, w)
        MM = lambda r, w, **kw: P.op("tensor", lambda e: e.matmul(**kw), r, w)
        TR = lambda r, w, **kw: P.op("tensor", lambda e: e.transpose(**kw), r, w)

        statn = [0]

        def stcol(n=1):
            c = statn[0] % 12 * 4
            statn[0] += 1
            return stat[:, c:c + n], f"stat{c}"

        def rstd_from_ss(ss_ap, ss_key, n, inv_n, out_ap, out_key):
            t1, k1 = stcol(n)
            V("tensor_scalar", [ss_key], [k1], out=t1, in0=ss_ap, scalar1=inv_n, scalar2=EPS,
              op0=ALU.mult, op1=ALU.add)
            t2, k2 = stcol(n)
            A([k1], [k2], out=t2, in_=t1, func=AF.Sqrt)
            V("reciprocal", [k2], [out_key], out=out_ap, in_=t2)


        def stop_here(tag, src_ap, rkeys):
            if stop_after != tag:
                return False
            o = P.dma("sync", out[0:128, 0:src_ap.shape[1]], src_ap, r=rkeys, sem="stopout")
            print("STOP at", tag, "n ops", len(P.ops))
            P.emit(final_wait_ops=[o])
            return True
        G("iota", [], ["iotf"], out=iotf[:], pattern=[[1, 128]], base=0, channel_multiplier=-1,
          allow_small_or_imprecise_dtypes=True)
        G("iota", [], ["iorow"], out=iorow[:], pattern=[[1, 128]], base=0, channel_multiplier=0,
          allow_small_or_imprecise_dtypes=True)
        V("tensor_copy", ["iorow"], ["iob"], out=iob[:], in_=iorow[:])
        V("tensor_single_scalar", ["iotf"], ["identb"], out=identb[:], in_=iotf[:], scalar=0.0, op=ALU.is_equal)
        V("tensor_single_scalar", ["iotf"], ["identf"], out=identf[:], in_=iotf[:], scalar=0.0, op=ALU.is_equal)
        V("tensor_single_scalar", ["iotf"], ["trilm"], out=trilm[:], in_=iotf[:], scalar=0.0, op=ALU.is_le)
        absd = MdT[:, 3, :]
        A(["iotf"], ["MdT3"], out=absd, in_=iotf[:], func=AF.Abs)
        V("tensor_tensor", ["MdT3", "iorow"], ["MdT3"], out=absd, in0=iorow[:], in1=absd, op=ALU.subtract)
        slopes = [2.0 ** (-2.0 * (h + 1)) for h in range(4)]
        for h in range(4):
            V("tensor_scalar", ["MdT3"], [f"MdT{h}"], out=MdT[:, h, :], in0=absd, scalar1=slopes[h], scalar2=None,
              op0=ALU.mult)
        for h in range(4):
            V("memset", [], [f"MdT{h}"], ap=MdT[64:128, h, 0:64], constant=NEG)
        G("iota", [], ["bcol3"], out=bcol[:, 3, :], pattern=[[-128, 16]], base=0, channel_multiplier=1,
          allow_small_or_imprecise_dtypes=True)
        for h in range(4):
            V("tensor_scalar", ["bcol3"], [f"bcol{h}"], out=bcol[:, h, :], in0=bcol[:, 3, :], scalar1=slopes[h],
              scalar2=None, op0=ALU.mult)
        P.dma("sync", lamt[:].rearrange("p a b -> p (a b)"), lamv.rearrange("a b -> (a b)").partition_broadcast(128),
              w=["lamt"], sem="c0")
        V("tensor_tensor", ["lamt"], ["lamp"], out=lamt[:, 0, :], in0=lamt[:, 0, :], in1=lamt[:, 1, :], op=ALU.mult)
        V("tensor_tensor", ["lamt"], ["lamp2"], out=lamt[:, 2, :], in0=lamt[:, 2, :], in1=lamt[:, 3, :], op=ALU.mult)
        V("tensor_reduce", ["lamp"], ["lw0"], out=lamw[:, 0:1], in_=lamt[:, 0, :], axis=AX.X, op=ALU.add)
        V("tensor_reduce", ["lamp2"], ["lw1"], out=lamw[:, 1:2], in_=lamt[:, 2, :], axis=AX.X, op=ALU.add)
        A(["lw0", "lw1"], ["lw23"], out=lamw[:, 2:4], in_=lamw[:, 0:2], func=AF.Exp)
        V("tensor_tensor", ["lw23"], ["lw4"], out=lamw[:, 4:5], in0=lamw[:, 3:4], in1=lamw[:, 2:3], op=ALU.subtract)
        V("tensor_scalar", ["lw4"], ["neglam"], out=lamw[:, 5:6], in0=lamw[:, 4:5], scalar1=-0.2, scalar2=None,
          op0=ALU.add)
        neglam = lamw[:, 5:6]
        P.dma("sync", ggv[:], g_gv.partition_broadcast(128), w=["ggv"], sem="c1")
        P.dma("sync", ggo[:], g_gout.partition_broadcast(128), w=["ggo"], sem="c2")
        P.dma("sync", gsub[:], g_subln.partition_broadcast(128), w=["gsubraw"], sem="c3")
        V("tensor_scalar", ["gsubraw"], ["gsub"], out=gsub[:], in0=gsub[:], scalar1=0.8, scalar2=None, op0=ALU.mult)
        P.dma("sync", gfin[:], g_final.partition_broadcast(128), w=["gfin"], sem="c4")
        P.dma("sync", gmc[:], g_mix.rearrange("(c p) -> p c", p=128), w=["gmc"], sem="c5",
              allow_slow_non_contiguous=True)
        P.dma("sync", gfc[:], g_ffn.rearrange("(c p) -> p c", p=128), w=["gfc"], sem="c6",
              allow_slow_non_contiguous=True)
        V("tensor_copy", ["gfc"], ["gfcb"], out=gfcb[:], in_=gfc[:])
        P.dma("sync", bsT[:], b_s.rearrange("g t -> t g"), w=["bsT"], sem="c7", allow_slow_non_contiguous=True)

        if stop_here('const', gfin[:], ['gfin','ggv','ggo','gsub','gmc','gfc','gfcb','bsT','neglam']):
            return nc
        cv = Carver(ws[:])
        f32s = [cv.take([1024], F32) for _ in range(4)]
        b16s = [cv.take([1024], BF16) for _ in range(4)]
        b16t = [cv.take([8, 128], BF16) for _ in range(2)]
        ri = [0]

        def ring(n):
            i = ri[0] % n
            ri[0] += 1
            return i

        for g in range(4):
            i = ring(4)
            P.dma("sync", f32s[i][:, 0:128], w_s[g], w=[f"ws:f32s{i}"], sem=f"f32s{i}")
            V("tensor_tensor", [f"ws:f32s{i}", "trilm"], [f"ws:f32s{i}"], out=f32s[i][:, 0:128], in0=f32s[i][:, 0:128],
              in1=trilm[:], op=ALU.mult)
            TR([f"ws:f32s{i}", "identf"], ["ws:pb6"], out=pb[6][:, 0:128], in_=f32s[i][:, 0:128], identity=identf[:])
            V("tensor_copy", ["ws:pb6"], [f"wsT{g}"], out=wsT[:, g, :], in_=pb[6][:, 0:128])
        for hp in range(16):
            i = ring(4)
            P.dma("sync", f32s[i][:, 0:128], peer_keys[hp], w=[f"ws:f32s{i}"], sem=f"f32s{i}")
            TR([f"ws:f32s{i}", "identf"], ["ws:pb6"], out=pb[6][:, 0:128], in_=f32s[i][:, 0:128], identity=identf[:])
            V("tensor_copy", ["ws:pb6"], [f"keysT{hp}"], out=keysT[:, hp, :], in_=pb[6][:, 0:128])
        for c in range(8):
            for half in range(2):
                i = ring(4)
                P.dma("sync", f32s[i][:], peer_wq[c * 128:(c + 1) * 128, half * 1024:(half + 1) * 1024],
                      w=[f"ws:f32s{i}"], sem=f"f32s{i}")
                V("tensor_scalar", [f"ws:f32s{i}", "gfc"], [f"ws:b16s{i}"], out=b16s[i][:], in0=f32s[i][:],
                  scalar1=gfc[:, c:c + 1], scalar2=None, op0=ALU.mult)
                P.dma("gpsimd", wqb_d[half * 8:(half + 1) * 8, :, c, :].rearrange("h p n -> p h n"),
                      b16s[i][:].rearrange("p (h n) -> p h n", h=8), r=[f"ws:b16s{i}"], w=["wqb_d"],
                      sem=f"b16w{i}")
        if stop_here('p0a', gfin[:], ['gfin','wqb_d'] + [f'keysT{i}' for i in range(16)]):
            return nc
        n_et = 128
        for et in range(n_et):
            i = ring(4)
            P.dma("sync", f32s[i][:], peer_wu[et * 128:(et + 1) * 128, :], w=[f"ws:f32s{i}"], sem=f"f32s{i}")
            A([f"ws:f32s{i}"], [f"ws:b16s{i}"], out=b16s[i][:], in_=f32s[i][:], func=AF.Copy)
            pbk = 6 + (et % 2)
            for c in range(8):
                TR([f"ws:b16s{i}", "identb"], [f"ws:pb{pbk}"], out=pbb[pbk][:, c * 128:(c + 1) * 128],
                   in_=b16s[i][:, c * 128:(c + 1) * 128], identity=identb[:])
            j = et % 2
            V("tensor_tensor", [f"ws:pb{pbk}", "gfcb"], [f"ws:b16t{j}"], out=b16t[j][:],
              in0=pbb[pbk][:].rearrange("p (c e) -> p c e", c=8), in1=gfcb[:].unsqueeze(2).to_broadcast([128, 8, 128]),
              op=ALU.mult)
            P.dma("gpsimd", wuT_d[et], b16t[j][:].rearrange("p c e -> p (c e)"), r=[f"ws:b16t{j}"], w=[f"wuT{et}"],
                  sem=f"b16tw{j}")
            i = ring(4)
            P.dma("sync", f32s[i][:], peer_wv[et * 128:(et + 1) * 128, :], w=[f"ws:f32s{i}"], sem=f"f32s{i}")
            V("tensor_copy", [f"ws:f32s{i}"], [f"ws:b16s{i}"], out=b16s[i][:], in_=f32s[i][:])
            P.dma("gpsimd", wvb_d[et], b16s[i][:], r=[f"ws:b16s{i}"], w=[f"wvb{et}"], sem=f"b16w{i}")

        if stop_here('p0', gfin[:], ['gfin'] + [f'wuT{i}' for i in range(128)] + [f'wvb{i}' for i in range(128)]):
            return nc
        P.fence()
        cv = Carver(ws[:])
        winT = cv.take([8, 2560], BF16)
        woutT = cv.take([8, 1024], BF16)
        hT = cv.take([8, S], BF16)
        qkT = [[cv.take([S], BF16) for _ in range(2)] for _ in range(2)]
        Vaug = cv.take([NT, 4, 130], BF16)
        mix = cv.take([NT, 1024], BF16)
        xin = [cv.take([1024], F32) for _ in range(2)]
        hb = [cv.take([1024], BF16) for _ in range(2)]
        ug = [cv.take([512], F32) for _ in range(2)]
        gvg = [cv.take([512], F32) for _ in range(2)]
        gvn = [cv.take([512], BF16) for _ in range(2)]
        t5a = [cv.take([512], F32) for _ in range(2)]
        pT = [cv.take([128], BF16) for _ in range(4)]
        dtmp = [cv.take([128], F32) for _ in range(2)]
        osb = [cv.take([128], F32) for _ in range(2)]
        osq = [cv.take([128], BF16) for _ in range(2)]
        mixT = [cv.take([8, 128], BF16) for _ in range(2)]
        print("phase1 ws used", cv.off, "of", cv.cap)

        ri[0] = 0
        for c in range(8):
            for (a, b) in ((0, 1024), (1024, 2048), (2048, 2560)):
                i = ring(2)
                P.dma("sync", xin[i][:, 0:b - a], w_in[c * 128:(c + 1) * 128, a:b], w=[f"ws:xin{i}"], sem=f"xin{i}")
                V("tensor_scalar", [f"ws:xin{i}", "gmc"], [f"ws:winT{c}"], out=winT[:, c, a:b], in0=xin[i][:, 0:b - a],
                  scalar1=gmc[:, c:c + 1], scalar2=None, op0=ALU.mult)
        for c in range(8):
            i = ring(2)
            P.dma("sync", xin[i][:], w_out[c * 128:(c + 1) * 128, :], w=[f"ws:xin{i}"], sem=f"xin{i}")
            A([f"ws:xin{i}"], [f"ws:woutT{c}"], out=woutT[:, c, :], in_=xin[i][:], func=AF.Copy)
        winK = [f"ws:winT{c}" for c in range(8)]
        woutK = [f"ws:woutT{c}" for c in range(8)]
        V("memset", [], ["ws:vones"], ap=Vaug[:, :, :, 128:130], constant=1.0)

        for seq in range(nseq):
            r0 = seq * S
            for tt in range(NT):
                i = tt % 2
                P.dma("sync", xin[i][:], x[r0 + tt * 128:r0 + (tt + 1) * 128, :], w=[f"ws:xin{i}"], sem=f"xin{i}")
                ss, ssk = stcol()
                A([f"ws:xin{i}"], [f"ws:hb{i}", ssk], out=hb[i][:], in_=xin[i][:], func=AF.Square, accum_out=ss)
                rs, rsk = stcol()
                rstd_from_ss(ss, ssk, 1, 1.0 / D, rs, rsk)
                V("tensor_scalar", [f"ws:xin{i}", rsk], [f"ws:hb{i}"], out=hb[i][:], in0=xin[i][:], scalar1=rs,
                  scalar2=None, op0=ALU.mult)
                pbk = 6 + i
                for c in range(8):
                    TR([f"ws:hb{i}", "identb"], [f"ws:pb{pbk}"], out=pbb[pbk][:, c * 128:(c + 1) * 128],
                       in_=hb[i][:, c * 128:(c + 1) * 128], identity=identb[:])
                A([f"ws:pb{pbk}"], [f"ws:hT{tt}"], out=hT[:, :, tt * 128:(tt + 1) * 128],
                  in_=pbb[pbk][:].rearrange("p (c t) -> p c t", c=8), func=AF.Copy)
            for tt in range(NT):
                i = tt % 2
                for n in range(3):
                    for c in range(8):
                        MM([f"ws:hT{tt}", winK[c]], [f"ws:pb{n}"], out=pb[n][:, :], lhsT=hT[:, c, tt * 128:(tt + 1) * 128],
                           rhs=winT[:, c, 1024 + n * 512:1024 + (n + 1) * 512], start=(c == 0), stop=(c == 7))
                V("tensor_copy", ["ws:pb0"], [f"ws:V{tt}"], out=Vaug[:, tt, :, 0:128],
                  in_=pb[0][:, :].rearrange("p (h e) -> p h e", h=4))
                A(["ws:pb1"], [f"ws:ug{i}"], out=ug[i][:], in_=pb[1][:, :], func=AF.Gelu_apprx_tanh)
                A(["ws:pb2"], [f"ws:gvg{i}"], out=gvg[i][:], in_=pb[2][:, :], func=AF.Gelu_apprx_tanh)
                V("tensor_tensor", [f"ws:gvg{i}"], [f"ws:t5a{i}"], out=t5a[i][:], in0=gvg[i][:], in1=gvg[i][:], op=ALU.mult)
                s4, s4k = stcol(4)
                V("tensor_reduce", [f"ws:t5a{i}"], [s4k], out=s4, in_=t5a[i][:].rearrange("p (g c) -> p g c", g=4),
                  axis=AX.X, op=ALU.add)
                r4, r4k = stcol(4)
                rstd_from_ss(s4, s4k, 4, 1.0 / 128, r4, r4k)
                V("tensor_tensor", [f"ws:gvg{i}", r4k], [f"ws:t5a{i}"], out=t5a[i][:].rearrange("p (g c) -> p g c", g=4),
                  in0=gvg[i][:].rearrange("p (g c) -> p g c", g=4), in1=r4.unsqueeze(2).to_broadcast([128, 4, 128]),
                  op=ALU.mult)
                V("tensor_tensor", [f"ws:t5a{i}", "ggv"], [f"ws:gvn{i}"], out=gvn[i][:], in0=t5a[i][:], in1=ggv[:], op=ALU.mult)
                for g in range(4):
                    MM([f"ws:gvn{i}", f"wsT{g}"], ["ws:pb3"], out=pb[3][:, g * 128:(g + 1) * 128], lhsT=wsT[:, g, :],
                       rhs=gvn[i][:, g * 128:(g + 1) * 128], start=True, stop=True)
                for g in range(4):
                    V("scalar_tensor_tensor", ["ws:pb3", "bsT", f"ws:ug{i}"], [f"ws:t5a{i}"],
                      out=t5a[i][:, g * 128:(g + 1) * 128], in0=pb[3][:, g * 128:(g + 1) * 128], scalar=bsT[:, g:g + 1],
                      in1=ug[i][:, g * 128:(g + 1) * 128], op0=ALU.add, op1=ALU.mult)
                V("tensor_tensor", [f"ws:t5a{i}"], [f"ws:gvg{i}"], out=gvg[i][:], in0=t5a[i][:], in1=t5a[i][:], op=ALU.mult)
                s4, s4k = stcol(4)
                V("tensor_reduce", [f"ws:gvg{i}"], [s4k], out=s4, in_=gvg[i][:].rearrange("p (g c) -> p g c", g=4),
                  axis=AX.X, op=ALU.add)
                r4, r4k = stcol(4)
                rstd_from_ss(s4, s4k, 4, 1.0 / 128, r4, r4k)
                V("tensor_tensor", [f"ws:t5a{i}", r4k], [f"ws:gvg{i}"], out=gvg[i][:].rearrange("p (g c) -> p g c", g=4),
                  in0=t5a[i][:].rearrange("p (g c) -> p g c", g=4), in1=r4.unsqueeze(2).to_broadcast([128, 4, 128]),
                  op=ALU.mult)
                V("tensor_tensor", [f"ws:gvg{i}", "ggo"], [f"ws:mixg{tt}"], out=mix[:, tt, 512:1024], in0=gvg[i][:],
                  in1=ggo[:], op=ALU.mult)
            pair = 0
            cntm = [0, 0]
            for h in range(4):
                sl = h % 2
                for which in range(2):
                    for tg in range(4):
                        pbk = 6 + (tg % 2)
                        for c in range(8):
                            MM([f"ws:hT{t}" for t in range(tg * 4, tg * 4 + 4)] + [winK[c]], [f"ws:pb{pbk}"],
                               out=pb[pbk][:, :], lhsT=winT[:, c, which * 512 + h * 128:which * 512 + (h + 1) * 128],
                               rhs=hT[:, c, tg * 512:(tg + 1) * 512], start=(c == 0), stop=(c == 7))
                        if tg % 2 == 0:
                            A([f"ws:pb{pbk}"], [f"ws:qk{sl}{which}_{tg}"], out=qkT[sl][which][:, tg * 512:(tg + 1) * 512],
                              in_=pb[pbk][:, :], func=AF.Copy)
                        else:
                            V("tensor_copy", [f"ws:pb{pbk}"], [f"ws:qk{sl}{which}_{tg}"],
                              out=qkT[sl][which][:, tg * 512:(tg + 1) * 512], in_=pb[pbk][:, :])
                qT_, kT_ = qkT[sl][0], qkT[sl][1]
                pairs = [(qt, m, j) for qt in range(NT) for m in range(2) for j in range(qt + 1)]
                LA = 4
                pbase = pair
                assign = []
                for (qt_, m_, j_) in pairs:
                    c_ = cntm[m_]
                    cntm[m_] += 1
                    assign.append((((0, 6), (1, 7))[m_][(c_ // 4) % 2], c_ % 4))
                pair += len(pairs)

                def rec_S(i2_, h=h, sl=sl, qT_=qT_, kT_=kT_, pbase=pbase, pairs=pairs, assign=assign):
                    qt, m, j = pairs[i2_]
                    sbank, sslot = assign[i2_]
                    sk = f"ws:pb{sbank}"
                    sview = pb[sbank][:, sslot * 128:(sslot + 1) * 128]
                    MM([f"ws:qk{sl}0_{qt // 4}", f"ws:qk{sl}1_{j // 4}"], [sk], out=sview,
                       lhsT=kT_[m * 64:(m + 1) * 64, j * 128:(j + 1) * 128],
                       rhs=qT_[m * 64:(m + 1) * 64, qt * 128:(qt + 1) * 128], start=True, stop=True)

                def rec_rest(i2_, h=h, pbase=pbase, pairs=pairs, assign=assign):
                    qt, m, j = pairs[i2_]
                    sbank, sslot = assign[i2_]
                    sk = f"ws:pb{sbank}"
                    sview = pb[sbank][:, sslot * 128:(sslot + 1) * 128]
                    ob = 2 + (qt % 2) * 2 + m
                    ps_ = (pbase + i2_) % 4
                    if j < qt:
                        A([sk, f"bcol{h}"], [f"ws:pT{ps_}"], out=pT[ps_][:], in_=sview, func=AF.Exp,
                          bias=bcol[:, h, qt - j:qt - j + 1], scale=0.125)
                    else:
                        dsl = m
                        V("scalar_tensor_tensor", [sk, f"MdT{h}"], [f"ws:dtmp{dsl}"], out=dtmp[dsl][:],
                          in0=sview, scalar=0.125, in1=MdT[:, h, :], op0=ALU.mult, op1=ALU.add)
                        A([f"ws:dtmp{dsl}"], [f"ws:pT{ps_}"], out=pT[ps_][:], in_=dtmp[dsl][:], func=AF.Exp)
                    MM([f"ws:pT{ps_}", f"ws:V{j}", "ws:vones"], [f"ws:pb{ob}"], out=pb[ob][:, 0:129],
                       lhsT=pT[ps_][:], rhs=Vaug[:, j, h, 0:129], start=(j == 0), stop=(j == qt))
                    if not (m == 1 and j == qt):
                        return
                    o1, o2 = 2 + (qt % 2) * 2, 2 + (qt % 2) * 2 + 1
                    osl = qt % 2
                    rz, rzk = stcol(2)
                    V("reciprocal", [f"ws:pb{o1}"], [rzk], out=rz[:, 0:1], in_=pb[o1][:, 128:129])
                    rz2, rz2k = stcol(2)
                    V("reciprocal", [f"ws:pb{o2}"], [rz2k], out=rz2[:, 0:1], in_=pb[o2][:, 128:129])
                    V("tensor_tensor", [rz2k, "neglam"], [rz2k], out=rz2[:, 1:2], in0=rz2[:, 0:1], in1=neglam,
                      op=ALU.mult)
                    V("tensor_scalar", [f"ws:pb{o1}", rzk], [f"ws:osb{osl}"], out=osb[osl][:], in0=pb[o1][:, 0:128],
                      scalar1=rz[:, 0:1], scalar2=None, op0=ALU.mult)
                    V("scalar_tensor_tensor", [f"ws:pb{o2}", rz2k, f"ws:osb{osl}"], [f"ws:osb{osl}"],
                      out=osb[osl][:], in0=pb[o2][:, 0:128], scalar=rz2[:, 1:2], in1=osb[osl][:], op0=ALU.mult,
                      op1=ALU.add)
                    sso, ssok = stcol()
                    A([f"ws:osb{osl}"], [f"ws:osq{osl}", ssok], out=osq[osl][:], in_=osb[osl][:], func=AF.Square,
                      accum_out=sso)
                    ro, rok = stcol()
                    rstd_from_ss(sso, ssok, 1, 1.0 / 128, ro, rok)
                    V("scalar_tensor_tensor", [f"ws:osb{osl}", rok, "gsub"], [f"ws:mixa{qt}_{h}"],
                      out=mix[:, qt, h * 128:(h + 1) * 128], in0=osb[osl][:], scalar=ro, in1=gsub[:], op0=ALU.mult,
                      op1=ALU.mult)

                for i2_ in range(min(LA, len(pairs))):
                    rec_S(i2_)
                for i2_ in range(len(pairs)):
                    if i2_ + LA < len(pairs):
                        rec_S(i2_ + LA)
                    rec_rest(i2_)
            for tt in range(NT):
                i = tt % 2
                pbk = 6 + i
                mk = [f"ws:mixa{tt}_{h}" for h in range(4)] + [f"ws:mixg{tt}"]
                for c in range(8):
                    TR(mk + ["identb"], [f"ws:pb{pbk}"], out=pbb[pbk][:, c * 128:(c + 1) * 128],
                       in_=mix[:, tt, c * 128:(c + 1) * 128], identity=identb[:])
                A([f"ws:pb{pbk}"], [f"ws:mixT{i}"], out=mixT[i][:], in_=pbb[pbk][:].rearrange("p (c t) -> p c t", c=8),
                  func=AF.Copy)
                P.dma("sync", xin[i][:], x[r0 + tt * 128:r0 + (tt + 1) * 128, :], w=[f"ws:xin{i}"], sem=f"xin{i}")
                for n in range(2):
                    for c in range(8):
                        MM([f"ws:mixT{i}", woutK[c]], [f"ws:pb{n}"], out=pb[n][:, :], lhsT=mixT[i][:, c, :],
                           rhs=woutT[:, c, n * 512:(n + 1) * 512], start=(c == 0), stop=(c == 7))
                for n in range(2):
                    V("tensor_tensor", [f"ws:pb{n}", f"ws:xin{i}"], [f"ws:xin{i}"], out=xin[i][:, n * 512:(n + 1) * 512],
                      in0=pb[n][:, :], in1=xin[i][:, n * 512:(n + 1) * 512], op=ALU.add)
                P.dma("gpsimd", x1s[r0 + tt * 128:r0 + (tt + 1) * 128, :], xin[i][:], r=[f"ws:xin{i}"],
                      w=[f"x1s{seq}_{tt}"], sem=f"x1w{i}")

        if stop_after == 'p1':
            o = P.dma("sync", out[0:S, :], x1s[0:S, :], r=[f'x1s0_{i}' for i in range(16)], sem="stopout")
            P.emit(final_wait_ops=[o])
            return nc
        P.fence()
        cv = Carver(ws[:])
        AT = cv.take([256, 128], BF16)
        xnT = cv.take([8, 256], BF16)
        qT2 = cv.take([16, 256], BF16)
        x1t = [cv.take([1024], F32) for _ in range(2)]
        xnb = [cv.take([1024], BF16) for _ in range(2)]
        wqt = [cv.take([8, 128], BF16) for _ in range(2)]
        scores = cv.take([16, 128], F32)
        vals = cv.take([16, 16], F32)
        idxu = cv.take([16, 16], U32)
        idxf = cv.take([16, 16], F32)
        cand = cv.take([8, 256], F32)
        best = cv.take([8, 16], F32)
        fidx = cv.take([8, 16], U32)
        r1u = cv.take([8, 16], U32)
        r2u = cv.take([8, 16], U32)
        r1f = cv.take([8, 16], F32)
        r2f = cv.take([8, 16], F32)
        oht = cv.take([8, 16, 16], F32)
        oht2 = cv.take([8, 16, 16], F32)
        i1f = cv.take([8, 16], F32)
        i2f = cv.take([8, 16], F32)
        gf = cv.take([8, 16], F32)
        ex = cv.take([8, 16], F32)
        i1T = cv.take([256], BF16)
        i2T = cv.take([256], BF16)
        gT = cv.take([256], BF16)
        TGK = 16
        OH1 = [cv.take([TGK, 128], BF16) for _ in range(2)]
        E1w = [cv.take([TGK, 128], BF16) for _ in range(2)]
        OH2 = [cv.take([TGK, 128], BF16) for _ in range(2)]
        wu = [cv.take([8, 128], BF16) for _ in range(4)]
        wv = [cv.take([1024], BF16) for _ in range(4)]
        gl = [cv.take([256], BF16) for _ in range(2)]
        GT = [cv.take([256], BF16) for _ in range(2)]
        outsb = [cv.take([1024], F32)] * 2
        print("phase2 ws used", cv.off, "of", cv.cap)

        out_ops = []
        nblk = NTOK // 256
        wring = 0
        hring = 0
        for b in range(nblk):
            R0 = b * 256
            seq = R0 // S
            for tt in range(2):
                gtt = (R0 % S) // 128 + tt
                P.dma("sync", x1t[tt][:], x1s[R0 + tt * 128:R0 + (tt + 1) * 128, :], r=[f"x1s{seq}_{gtt}"],
                      w=[f"ws:x1t{tt}"], sem=f"x1t{tt}")
                ss, ssk = stcol()
                A([f"ws:x1t{tt}"], [f"ws:xnb{tt}", ssk], out=xnb[tt][:], in_=x1t[tt][:], func=AF.Square, accum_out=ss)
                rs, rsk = stcol()
                rstd_from_ss(ss, ssk, 1, 1.0 / D, rs, rsk)
                V("tensor_scalar", [f"ws:x1t{tt}", rsk], [f"ws:xnb{tt}"], out=xnb[tt][:], in0=x1t[tt][:], scalar1=rs,
                  scalar2=None, op0=ALU.mult)
                pbk = 6 + tt
                for c in range(8):
                    TR([f"ws:xnb{tt}", "identb"], [f"ws:pb{pbk}"], out=pbb[pbk][:, c * 128:(c + 1) * 128],
                       in_=xnb[tt][:, c * 128:(c + 1) * 128], identity=identb[:])
                A([f"ws:pb{pbk}"], [f"ws:xnT{tt}"], out=xnT[:, :, tt * 128:(tt + 1) * 128],
                  in_=pbb[pbk][:].rearrange("p (c t) -> p c t", c=8), func=AF.Copy)
            xnK = ["ws:xnT0", "ws:xnT1"]
            if b == 0 and stop_here("p2F", x1t[0][:], ["ws:x1t0"] + xnK):
                return nc
            for hp in range(16):
                wi = hp % 2
                P.dma("sync", wqt[wi][:], wqb_d[hp], r=["wqb_d"], w=[f"ws:wqt{wi}"], sem=f"wqt{wi}")
                hs = hp % 2
                hv = pb[4 + hs][:, 0:256]
                hk = f"ws:pb{4 + hs}"
                for c in range(8):
                    MM(xnK + [f"ws:wqt{wi}"], [hk], out=hv, lhsT=wqt[wi][:, c, :], rhs=xnT[:, c, :], start=(c == 0),
                       stop=(c == 7))
                if hp % 2 == 0 or os.environ.get("KVAR") == "A":
                    A([hk], [f"ws:qT2_{hp}"], out=qT2[:, hp, :], in_=hv, func=AF.Copy)
                else:
                    V("tensor_copy", [hk], [f"ws:qT2_{hp}"], out=qT2[:, hp, :], in_=hv)
                if b == 0 and stop_here(f"p2G_{hp}", x1t[0][:], ["ws:x1t0", f"ws:qT2_{hp}"]):
                    return nc
            if b == 0 and stop_here("p2G0", x1t[0][:], ["ws:x1t0"] + [f"ws:qT2_{hp}" for hp in range(16)]):
                return nc
            for tt in range(2):
                for hp in range(16):
                    MM([f"ws:qT2_{hp}", f"keysT{hp}"], [f"ws:pb{hp // 4}"], out=pb[hp // 4][:, (hp % 4) * 128:(hp % 4 + 1) * 128],
                       lhsT=qT2[:, hp, tt * 128:(tt + 1) * 128], rhs=keysT[:, hp, :], start=True, stop=True)
                for q4 in range(4):
                    A([f"ws:pb{q4}"], [f"ws:sc{q4}"], out=scores[:, q4 * 4:(q4 + 1) * 4, :],
                      in_=pb[q4][:, :].rearrange("p (a n) -> p a n", a=4), func=AF.Copy)
                if b == 0 and tt == 0 and stop_here("p2G", scores[:].rearrange("p a n -> p (a n)")[:, 0:1024], [f"ws:sc{q}" for q in range(4)]):
                    return nc
                for hp in range(16):
                    sk = f"ws:sc{hp // 4}"
                    sc = scores[:, hp, :]
                    V("max", [sk], [f"ws:vals{hp}a"], out=vals[:, hp, 0:8], in_=sc)
                    V("max_index", [sk, f"ws:vals{hp}a"], [f"ws:idx{hp}a"], out=idxu[:, hp, 0:8], in_max=vals[:, hp, 0:8],
                      in_values=sc)
                    V("match_replace", [sk, f"ws:vals{hp}a"], [sk], out=sc, in_to_replace=vals[:, hp, 0:8], in_values=sc,
                      imm_value=-1e30)
                    V("max", [sk], [f"ws:vals{hp}b"], out=vals[:, hp, 8:16], in_=sc)
                    V("max_index", [sk, f"ws:vals{hp}b"], [f"ws:idx{hp}b"], out=idxu[:, hp, 8:16], in_max=vals[:, hp, 8:16],
                      in_values=sc)
                valK = [f"ws:vals{hp}{ab}" for hp in range(16) for ab in "ab"]
                idxK = [f"ws:idx{hp}{ab}" for hp in range(16) for ab in "ab"]
                V("tensor_copy", idxK, ["ws:idxf"], out=idxf[:], in_=idxu[:])
                v4 = vals[:].rearrange("p (h t) k -> p h t k", t=2)
                V("tensor_tensor", valK, ["ws:cand"], out=cand[:].rearrange("p h (a b) -> p h a b", a=16),
                  in0=v4[:, :, 0, :].unsqueeze(3).to_broadcast([128, 8, 16, 16]),
                  in1=v4[:, :, 1, :].unsqueeze(2).to_broadcast([128, 8, 16, 16]), op=ALU.add)
                for h in range(8):
                    ck = f"ws:cand{h}"
                    dep = ["ws:cand"]
                    V("max", dep, [f"ws:best{h}a"], out=best[:, h, 0:8], in_=cand[:, h, :])
                    V("max_index", dep + [f"ws:best{h}a"], [f"ws:fidx{h}a"], out=fidx[:, h, 0:8], in_max=best[:, h, 0:8],
                      in_values=cand[:, h, :])
                    V("match_replace", dep + [f"ws:best{h}a"], [ck], out=cand[:, h, :], in_to_replace=best[:, h, 0:8],
                      in_values=cand[:, h, :], imm_value=-1e30)
                    V("max", [ck], [f"ws:best{h}b"], out=best[:, h, 8:16], in_=cand[:, h, :])
                    V("max_index", [ck, f"ws:best{h}b"], [f"ws:fidx{h}b"], out=fidx[:, h, 8:16], in_max=best[:, h, 8:16],
                      in_values=cand[:, h, :])
                bestK = [f"ws:best{h}{ab}" for h in range(8) for ab in "ab"]
                fidxK = [f"ws:fidx{h}{ab}" for h in range(8) for ab in "ab"]
                candK = [f"ws:cand{h}" for h in range(8)]
                V("tensor_tensor", bestK, ["ws:ex"], out=ex[:], in0=best[:],
                  in1=best[:, :, 0:1].to_broadcast([128, 8, 16]), op=ALU.subtract)
                A(["ws:ex"], ["ws:ex2"], out=ex[:], in_=ex[:], func=AF.Exp)
                z8, z8k = stcol(8) if False else (None, None)
                zt, ztk = stat[:, 48:56], "statz"
                V("tensor_reduce", ["ws:ex2"], [ztk], out=zt, in_=ex[:], axis=AX.X, op=ALU.add)
                zr, zrk = stat[:, 56:64], "statzr"
                V("reciprocal", [ztk], [zrk], out=zr, in_=zt)
                V("tensor_tensor", ["ws:ex2", zrk], ["ws:gf"], out=gf[:], in0=ex[:],
                  in1=zr.unsqueeze(2).to_broadcast([128, 8, 16]), op=ALU.mult)
                V("tensor_single_scalar", fidxK, ["ws:r1u"], out=r1u[:], in_=fidx[:], scalar=4, op=ALU.logical_shift_right)
                V("tensor_single_scalar", fidxK, ["ws:r2u"], out=r2u[:], in_=fidx[:], scalar=15, op=ALU.bitwise_and)
                V("tensor_copy", ["ws:r1u"], ["ws:r1f"], out=r1f[:], in_=r1u[:])
                V("tensor_copy", ["ws:r2u"], ["ws:r2f"], out=r2f[:], in_=r2u[:])
                idx4 = idxf[:].rearrange("p (h t) k -> p h t k", t=2)
                io16 = iorow[:, 0:16].unsqueeze(1).unsqueeze(1).to_broadcast([128, 8, 16, 16])
                for (rf, rk, tsel, dst, dk) in ((r1f, "ws:r1f", 0, i1f, "ws:i1f"), (r2f, "ws:r2f", 1, i2f, "ws:i2f")):
                    V("tensor_tensor", [rk, "iorow"], ["ws:oht"], out=oht[:],
                      in0=rf[:].unsqueeze(3).to_broadcast([128, 8, 16, 16]), in1=io16, op=ALU.is_equal)
                    V("tensor_tensor", ["ws:oht", "ws:idxf"], ["ws:oht2"], out=oht2[:], in0=oht[:],
                      in1=idx4[:, :, tsel, :].unsqueeze(2).to_broadcast([128, 8, 16, 16]), op=ALU.mult)
                    V("tensor_reduce", ["ws:oht2"], [dk], out=dst[:], in_=oht2[:], axis=AX.X, op=ALU.add)
                if b == 0 and tt == 0 and stop_here("p2H", gf[:].rearrange("p h k -> p (h k)"), ["ws:gf", "ws:i1f", "ws:i2f"]):
                    return nc
                if b == 0 and tt == 0 and stop_here("p2H1", i1f[:].rearrange("p h k -> p (h k)"), ["ws:gf", "ws:i1f", "ws:i2f"]):
                    return nc
                if b == 0 and tt == 0 and stop_here("p2H2", i2f[:].rearrange("p h k -> p (h k)"), ["ws:gf", "ws:i1f", "ws:i2f"]):
                    return nc
                for (src, sk_, dstT, dk) in ((i1f, "ws:i1f", i1T, "ws:i1T"), (i2f, "ws:i2f", i2T, "ws:i2T"),
                                             (gf, "ws:gf", gT, "ws:gT")):
                    TR([sk_, "identf"], ["ws:pb7"], out=pb[7][:, 0:128], in_=src[:].rearrange("p h k -> p (h k)"),
                       identity=identf[:])
                    V("tensor_copy", ["ws:pb7"], [dk + str(tt)], out=dstT[:, tt * 128:(tt + 1) * 128], in_=pb[7][:, 0:128])
            io_b = iob[:].unsqueeze(1).to_broadcast([128, TGK, 128])
            for g in range(256 // TGK):
                sl = g % 2
                tt = (g * TGK) // 128
                tsl = slice(g * TGK, (g + 1) * TGK)
                V("tensor_tensor", ["iob", f"ws:i2T{tt}"], [f"ws:OH2{sl}"], out=OH2[sl][:], in0=io_b,
                  in1=i2T[:, tsl].unsqueeze(2).to_broadcast([128, TGK, 128]), op=ALU.is_equal)
                V("tensor_tensor", ["iob", f"ws:i1T{tt}"], [f"ws:OH1{sl}"], out=OH1[sl][:], in0=io_b,
                  in1=i1T[:, tsl].unsqueeze(2).to_broadcast([128, TGK, 128]), op=ALU.is_equal)
                G("tensor_tensor", [f"ws:OH1{sl}", f"ws:gT{tt}"], [f"ws:E1w{sl}"], out=E1w[sl][:], in0=OH1[sl][:],
                  in1=gT[:, tsl].unsqueeze(2).to_broadcast([128, TGK, 128]), op=ALU.mult)
                for k4 in range(TGK // 4):
                    pbk = 6 + (k4 % 2)
                    for kk in range(4):
                        k = k4 * 4 + kk
                        MM([f"ws:OH2{sl}", f"ws:E1w{sl}"], [f"ws:pb{pbk}"], out=pb[pbk][:, kk * 128:(kk + 1) * 128],
                           lhsT=OH2[sl][:, k, :], rhs=E1w[sl][:, k, :], start=True, stop=True)
                    t0 = g * TGK + k4 * 4
                    A([f"ws:pb{pbk}"], ["ws:AT"], out=AT[:, t0:t0 + 4, :],
                      in_=pb[pbk][:, :].rearrange("p (t i) -> p t i", t=4), func=AF.Copy)
            if b == 0 and stop_here("p2J", x1t[0][:], ["ws:AT", "ws:x1t0"]):
                return nc
            def rec_H(et, b=b):
                gi = b * 128 + et
                wi = gi % 4
                P.dma("sync", wu[wi][:].rearrange("p c e -> p (c e)"), wuT_d[et], r=[f"wuT{et}"], w=[f"ws:wu{wi}"],
                      sem=f"wu{wi}")
                P.dma("sync", wv[wi][:], wvb_d[et], r=[f"wvb{et}"], w=[f"ws:wv{wi}"], sem=f"wv{wi}")
                hs = gi % 2
                for c in range(8):
                    MM(xnK + [f"ws:wu{wi}"], [f"ws:pb{4 + hs}"], out=pb[4 + hs][:, 0:256], lhsT=wu[wi][:, c, :],
                       rhs=xnT[:, c, :], start=(c == 0), stop=(c == 7))

            def rec_rest(et, b=b):
                gi = b * 128 + et
                wi = gi % 4
                hs = gi % 2
                gs = gi % 2
                A([f"ws:pb{4 + hs}"], [f"ws:gl{gs}"], out=gl[gs][:], in_=pb[4 + hs][:, 0:256], func=AF.Gelu_apprx_tanh)
                V("tensor_tensor", [f"ws:gl{gs}", "ws:AT"], [f"ws:GT{gs}"], out=GT[gs][:], in0=gl[gs][:],
                  in1=AT[:, :, et], op=ALU.mult)
                for tt in range(2):
                    for dh in range(2):
                        MM([f"ws:GT{gs}", f"ws:wv{wi}"], [f"ws:pb{tt * 2 + dh}"], out=pb[tt * 2 + dh][:, :],
                           lhsT=GT[gs][:, tt * 128:(tt + 1) * 128], rhs=wv[wi][:, dh * 512:(dh + 1) * 512],
                           start=(et == 0), stop=(et == 127))

            rec_H(0)
            for et in range(128):
                if et + 1 < 128:
                    rec_H(et + 1)
                rec_rest(et)
            if b == 0 and stop_here("p2K", x1t[0][:], ["ws:x1t0"] + [f"ws:pb{i}" for i in range(4)]):
                return nc
            for tt in range(2):
                for dh in range(2):
                    V("tensor_tensor", [f"ws:pb{tt * 2 + dh}", f"ws:x1t{tt}"], [f"ws:x1t{tt}"],
                      out=x1t[tt][:, dh * 512:(dh + 1) * 512], in0=pb[tt * 2 + dh][:, :],
                      in1=x1t[tt][:, dh * 512:(dh + 1) * 512], op=ALU.add)
                ss, ssk = stcol()
                A([f"ws:x1t{tt}"], ["ws:outsb", ssk], out=outsb[tt][:], in_=x1t[tt][:], func=AF.Square, accum_out=ss)
                rs, rsk = stcol()
                rstd_from_ss(ss, ssk, 1, 1.0 / D, rs, rsk)
                V("scalar_tensor_tensor", [f"ws:x1t{tt}", rsk, "gfin"], ["ws:outsb"], out=outsb[tt][:],
                  in0=x1t[tt][:], scalar=rs, in1=gfin[:], op0=ALU.mult, op1=ALU.mult)
                out_ops.append(P.dma("gpsimd", out[R0 + tt * 128:R0 + (tt + 1) * 128, :], outsb[tt][:],
                                     r=["ws:outsb"], sem="outw"))
        print("n ops", len(P.ops))
        P.emit(final_wait_ops=out_ops)
    return nc


def kernel(x, w_in, lam_q1, lam_k1, lam_q2, lam_k2, g_subln, w_s, b_s, g_gv, g_gout, w_out, g_mix, g_ffn,
           peer_wq, peer_keys, peer_wu, peer_wv, g_final):
    f = lambda a: np.ascontiguousarray(np.asarray(a, dtype=np.float32))
    x = f(x)
    B = x.shape[0]
    nseq = B // NCORES
    nc = build_nc(nseq)
    common = {
        "w_in": f(w_in[0]),
        "lamv": f(np.stack([np.asarray(lam_q1)[0], np.asarray(lam_k1)[0], np.asarray(lam_q2)[0], np.asarray(lam_k2)[0]])),
        "g_subln": f(g_subln[0]), "w_s": f(w_s[0]), "b_s": f(b_s[0]), "g_gv": f(g_gv[0]), "g_gout": f(g_gout[0]),
        "w_out": f(w_out[0]), "g_mix": f(g_mix[0]), "g_ffn": f(g_ffn[0]), "peer_wq": f(peer_wq[0]),
        "peer_keys": f(np.asarray(peer_keys)[0].reshape(16, 128, 128)), "peer_wu": f(peer_wu[0]),
        "peer_wv": f(peer_wv[0]), "g_final": f(g_final),
    }
    in_maps = []
    for c in range(NCORES):
        m = dict(common)
        m["x"] = x[c * nseq:(c + 1) * nseq].reshape(nseq * S, D)
        in_maps.append(m)
    res = run_bass_kernel_spmd(nc, in_maps, core_ids=list(range(NCORES)))
    outs = [np.asarray(r["out"]).reshape(nseq, S, D) for r in res.results]
    return np.concatenate(outs, axis=0).astype(np.float32)
```

```python
import numpy as np
from contextlib import ExitStack
import concourse.bass as bass
import concourse.mybir as mybir
from concourse.bass_utils import run_bass_kernel_spmd

F32 = mybir.dt.float32
BF16 = mybir.dt.bfloat16
U32 = mybir.dt.uint32
AF = mybir.ActivationFunctionType
ALU = mybir.AluOpType
AX = mybir.AxisListType

ENGS = ["sync", "scalar", "vector", "gpsimd", "tensor"]
EPS = 1e-6
NCORES = 8
D = 1024
S = 2048
NT = S // 128
NEG = -30000.0


class Prog:
    def __init__(self, nc):
        self.nc = nc
        self.ops = []
        self.last_w = {}
        self.readers = {}
        self.dma_sem_count = {}
        self.epoch = 0
        self.key_epoch = {}
        self.fence_ops = []
        self.last_eng_op = {}
        self.last_dma_op = {}

    def fence(self):
        self.epoch += 1
        self.fence_ops = list(self.last_eng_op.values()) + list(self.last_dma_op.values())

    def _add(self, eng, fn, r, w, dma_sem=None):
        idx = len(self.ops)
        pk = [k for k in r if k.startswith("ws:pb")]
        if pk:
            r = [k for k in r if not k.startswith("ws:pb")]
            w = list(w) + pk
        deps = set()
        for k in list(r) + list(w):
            if k.startswith("ws:") and self.key_epoch.get(k) != self.epoch:
                self.key_epoch[k] = self.epoch
                self.last_w.pop(k, None)
                self.readers.pop(k, None)
                deps.update(self.fence_ops)
        raw = set()
        for k in r:
            lw = self.last_w.get(k)
            if lw is not None:
                deps.add(lw)
                raw.add(lw)
        for k in w:
            lw = self.last_w.get(k)
            if lw is not None:
                deps.add(lw)
            deps.update(self.readers.get(k, ()))
        real = set()
        for d in deps:
            od = self.ops[d]
            if od["dma_sem"] is None and od["eng"] == eng and d not in raw:
                continue
            real.add(d)
        op = dict(eng=eng, fn=fn, deps=real, dma_sem=dma_sem, idx=idx)
        if dma_sem is not None:
            c = self.dma_sem_count.get(dma_sem, 0) + 16
            self.dma_sem_count[dma_sem] = c
            op["val"] = c
            self.last_dma_op[dma_sem] = idx
        else:
            self.last_eng_op[eng] = idx
        self.ops.append(op)
        for k in r:
            self.readers.setdefault(k, []).append(idx)
        for k in w:
            self.last_w[k] = idx
            self.readers[k] = []
        return idx

    def op(self, eng, fn, r=(), w=()):
        return self._add(eng, fn, r, w)

    def dma(self, eng, out, in_, r=(), w=(), sem="dma", **kw):
        def fn(e, out=out, in_=in_, kw=kw):
            return e.dma_start(out=out, in_=in_, **kw)
        return self._add(eng, fn, r, w, dma_sem=sem)

    def emit(self, final_wait_ops=()):
        nc = self.nc
        ops = self.ops
        needs_inc = [False] * len(ops)
        for o in ops:
            for d in o["deps"]:
                needs_inc[d] = True
        for d in final_wait_ops:
            needs_inc[d] = True
        SEGN = 30000
        cnt = {e: 0 for e in ENGS}
        semnames = []
        for o in ops:
            if o["dma_sem"] is None:
                if needs_inc[o["idx"]]:
                    c = cnt[o["eng"]]
                    cnt[o["eng"]] += 1
                    o["sem"] = "eng_%s_%d" % (o["eng"], c // SEGN)
                    o["val"] = c % SEGN + 1
                    if o["sem"] not in semnames:
                        semnames.append(o["sem"])
                else:
                    o["sem"] = None
                    o["val"] = None
            else:
                o["sem"] = "dma_" + str(o["dma_sem"])
        semnames += ["dma_" + str(k) for k in self.dma_sem_count]
        with ExitStack() as st:
            sems = {n: st.enter_context(nc.semaphore(n)) for n in semnames}
            block = st.enter_context(nc.Block())
            per_eng = {e: [o for o in ops if o["eng"] == e] for e in ENGS}

            def make(ename):
                def body(eng):
                    waited = {}
                    for o in per_eng[ename]:
                        for d in sorted(o["deps"]):
                            od = ops[d]
                            s, v = od["sem"], od["val"]
                            if waited.get(s, 0) >= v:
                                continue
                            eng.wait_ge(sems[s], v)
                            waited[s] = v
                        ins = o["fn"](eng)
                        if o["dma_sem"] is not None:
                            ins.then_inc(sems[o["sem"]], 16)
                        elif o["val"] is not None:
                            ins.then_inc(sems[o["sem"]], 1)
                    if ename == "sync":
                        for d in final_wait_ops:
                            od = ops[d]
                            if waited.get(od["sem"], 0) >= od["val"]:
                                continue
                            eng.wait_ge(sems[od["sem"]], od["val"])
                            waited[od["sem"]] = od["val"]
                return body

            block.sync(make("sync"))
            block.scalar(make("scalar"))
            block.vector(make("vector"))
            block.gpsimd(make("gpsimd"))
            block.tensor(make("tensor"))


class Carver:
    def __init__(self, base):
        self.base = base
        self.off = 0
        self.cap = base.shape[1]

    def take(self, shape, dtype):
        esz = 2 if dtype == BF16 else 4
        n = int(np.prod(shape)) * esz // 2
        n_al = (n + 15) // 16 * 16
        assert self.off + n_al <= self.cap, ("workspace overflow", self.off, n_al, self.cap)
        v = self.base[:, self.off:self.off + n]
        self.off += n_al
        if dtype != BF16:
            v = v.bitcast(dtype)
        if len(shape) == 2:
            v = v.rearrange("p (a b) -> p a b", a=shape[0])
        elif len(shape) == 3:
            v = v.rearrange("p (a b c) -> p a b c", a=shape[0], b=shape[1])
        return v


def build_nc(nseq, stop_after=None):
    import os
    stop_after = os.environ.get('KSTOP', stop_after)
    NTOK = nseq * S
    nc = bass.Bass("TRN2", target_bir_lowering=False)
    dt = lambda n, s, d=F32, kind="ExternalInput": nc.dram_tensor(n, s, d, kind=kind).ap()
    x = dt("x", [NTOK, D])
    w_in = dt("w_in", [D, 2560])
    lamv = dt("lamv", [4, 64])
    g_subln = dt("g_subln", [128])
    w_s = dt("w_s", [4, 128, 128])
    b_s = dt("b_s", [4, 128])
    g_gv = dt("g_gv", [512])
    g_gout = dt("g_gout", [512])
    w_out = dt("w_out", [D, D])
    g_mix = dt("g_mix", [D])
    g_ffn = dt("g_ffn", [D])
    peer_wq = dt("peer_wq", [D, 2048])
    peer_keys = dt("peer_keys", [16, 128, 128])
    peer_wu = dt("peer_wu", [16384, D])
    peer_wv = dt("peer_wv", [16384, D])
    g_final = dt("g_final", [D])
    out = dt("out", [NTOK, D], F32, "ExternalOutput")
    wuT_d = dt("wuT_d", [128, 128, 1024], BF16, "Internal")
    wvb_d = dt("wvb_d", [128, 128, 1024], BF16, "Internal")
    wqb_d = dt("wqb_d", [16, 128, 8, 128], BF16, "Internal")
    x1s = dt("x1s", [NTOK, D], F32, "Internal")

    WS_ELEMS = (212800 - 21600) // 2 // 16 * 16
    with ExitStack() as st:
        sb = lambda n, s, d: st.enter_context(nc.sbuf_tensor(n, s, d))
        ws = sb("ws", [128, WS_ELEMS], BF16)
        iotf = sb("iotf", [128, 128], F32)
        iorow = sb("iorow", [128, 128], F32)
        iob = sb("iob", [128, 128], BF16)
        identb = sb("identb", [128, 128], BF16)
        identf = sb("identf", [128, 128], F32)
        trilm = sb("trilm", [128, 128], F32)
        MdT = sb("MdT", [128, 4, 128], F32)
        bcol = sb("bcol", [128, 4, 16], F32)
        lamt = sb("lamt", [128, 4, 64], F32)
        lamw = sb("lamw", [128, 8], F32)
        ggv = sb("ggv", [128, 512], F32)
        ggo = sb("ggo", [128, 512], F32)
        gsub = sb("gsub", [128, 128], F32)
        gfin = sb("gfin", [128, 1024], F32)
        gmc = sb("gmc", [128, 8], F32)
        gfc = sb("gfc", [128, 8], F32)
        gfcb = sb("gfcb", [128, 8], BF16)
        bsT = sb("bsT", [128, 4], F32)
        wsT = sb("wsT", [128, 4, 128], BF16)
        keysT = sb("keysT", [128, 16, 128], BF16)
        stat = sb("stat", [128, 64], F32)
        pb = [st.enter_context(nc.psum_tensor(f"ws:pb{i}", [128, 512], F32)) for i in range(8)]
        pbb = [p[:].bitcast(BF16) for p in pb]

        P = Prog(nc)
        V = lambda name, r, w, **kw: P.op("vector", lambda e: getattr(e, name)(**kw), r, w)
        A = lambda r, w, **kw: P.op("scalar", lambda e: e.activation(**kw), r, w)
        G = lambda name, r, w, **kw: P.op("gpsimd", lambda e: getattr(e, name)(**kw), r, w)
        MM = lambda r, w, **kw: P.op("tensor", lambda e: e.matmul(**kw), r, w)
        TR = lambda r, w, **kw: P.op("tensor", lambda e: e.transpose(**kw), r, w)

        statn = [0]

        def stcol(n=1):
            c = statn[0] % 12 * 4
            statn[0] += 1
            return stat[:, c:c + n], f"stat{c}"

        def rstd_from_ss(ss_ap, ss_key, n, inv_n, out_ap, out_key):
            t1, k1 = stcol(n)
            V("tensor_scalar", [ss_key], [k1], out=t1, in0=ss_ap, scalar1=inv_n, scalar2=EPS,
              op0=ALU.mult, op1=ALU.add)
            t2, k2 = stcol(n)
            A([k1], [k2], out=t2, in_=t1, func=AF.Sqrt)
            V("reciprocal", [k2], [out_key], out=out_ap, in_=t2)


        def stop_here(tag, src_ap, rkeys):
            if stop_after != tag:
                return False
            o = P.dma("sync", out[0:128, 0:src_ap.shape[1]], src_ap, r=rkeys, sem="stopout")
            print("STOP at", tag, "n ops", len(P.ops))
            P.emit(final_wait_ops=[o])
            return True
        G("iota", [], ["iotf"], out=iotf[:], pattern=[[1, 128]], base=0, channel_multiplier=-1,
          allow_small_or_imprecise_dtypes=True)
        G("iota", [], ["iorow"], out=iorow[:], pattern=[[1, 128]], base=0, channel_multiplier=0,
          allow_small_or_imprecise_dtypes=True)
        V("tensor_copy", ["iorow"], ["iob"], out=iob[:], in_=iorow[:])
        V("tensor_single_scalar", ["iotf"], ["identb"], out=identb[:], in_=iotf[:], scalar=0.0, op=ALU.is_equal)
        V("tensor_single_scalar", ["iotf"], ["identf"], out=identf[:], in_=iotf[:], scalar=0.0, op=ALU.is_equal)
        V("tensor_single_scalar", ["iotf"], ["trilm"], out=trilm[:], in_=iotf[:], scalar=0.0, op=ALU.is_le)
        absd = MdT[:, 3, :]
        A(["iotf"], ["MdT3"], out=absd, in_=iotf[:], func=AF.Abs)
        V("tensor_tensor", ["MdT3", "iorow"], ["MdT3"], out=absd, in0=iorow[:], in1=absd, op=ALU.subtract)
        slopes = [2.0 ** (-2.0 * (h + 1)) for h in range(4)]
        for h in range(4):
            V("tensor_scalar", ["MdT3"], [f"MdT{h}"], out=MdT[:, h, :], in0=absd, scalar1=slopes[h], scalar2=None,
              op0=ALU.mult)
        for h in range(4):
            V("memset", [], [f"MdT{h}"], ap=MdT[64:128, h, 0:64], constant=NEG)
        G("iota", [], ["bcol3"], out=bcol[:, 3, :], pattern=[[-128, 16]], base=0, channel_multiplier=1,
          allow_small_or_imprecise_dtypes=True)
        for h in range(4):
            V("tensor_scalar", ["bcol3"], [f"bcol{h}"], out=bcol[:, h, :], in0=bcol[:, 3, :], scalar1=slopes[h],
              scalar2=None, op0=ALU.mult)
        P.dma("sync", lamt[:].rearrange("p a b -> p (a b)"), lamv.rearrange("a b -> (a b)").partition_broadcast(128),
              w=["lamt"], sem="c0")
        V("tensor_tensor", ["lamt"], ["lamp"], out=lamt[:, 0, :], in0=lamt[:, 0, :], in1=lamt[:, 1, :], op=ALU.mult)
        V("tensor_tensor", ["lamt"], ["lamp2"], out=lamt[:, 2, :], in0=lamt[:, 2, :], in1=lamt[:, 3, :], op=ALU.mult)
        V("tensor_reduce", ["lamp"], ["lw0"], out=lamw[:, 0:1], in_=lamt[:, 0, :], axis=AX.X, op=ALU.add)
        V("tensor_reduce", ["lamp2"], ["lw1"], out=lamw[:, 1:2], in_=lamt[:, 2, :], axis=AX.X, op=ALU.add)
        A(["lw0", "lw1"], ["lw23"], out=lamw[:, 2:4], in_=lamw[:, 0:2], func=AF.Exp)
        V("tensor_tensor", ["lw23"], ["lw4"], out=lamw[:, 4:5], in0=lamw[:, 3:4], in1=lamw[:, 2:3], op=ALU.subtract)
        V("tensor_scalar", ["lw4"], ["neglam"], out=lamw[:, 5:6], in0=lamw[:, 4:5], scalar1=-0.2, scalar2=None,
          op0=ALU.add)
        neglam = lamw[:, 5:6]
        P.dma("sync", ggv[:], g_gv.partition_broadcast(128), w=["ggv"], sem="c1")
        P.dma("sync", ggo[:], g_gout.partition_broadcast(128), w=["ggo"], sem="c2")
        P.dma("sync", gsub[:], g_subln.partition_broadcast(128), w=["gsubraw"], sem="c3")
        V("tensor_scalar", ["gsubraw"], ["gsub"], out=gsub[:], in0=gsub[:], scalar1=0.8, scalar2=None, op0=ALU.mult)
        P.dma("sync", gfin[:], g_final.partition_broadcast(128), w=["gfin"], sem="c4")
        P.dma("sync", gmc[:], g_mix.rearrange("(c p) -> p c", p=128), w=["gmc"], sem="c5",
              allow_slow_non_contiguous=True)
        P.dma("sync", gfc[:], g_ffn.rearrange("(c p) -> p c", p=128), w=["gfc"], sem="c6",
              allow_slow_non_contiguous=True)
        V("tensor_copy", ["gfc"], ["gfcb"], out=gfcb[:], in_=gfc[:])
        P.dma("sync", bsT[:], b_s.rearrange("g t -> t g"), w=["bsT"], sem="c7", allow_slow_non_contiguous=True)

        if stop_here('const', gfin[:], ['gfin','ggv','ggo','gsub','gmc','gfc','gfcb','bsT','neglam']):
            return nc
        cv = Carver(ws[:])
        f32s = [cv.take([1024], F32) for _ in range(4)]
        b16s = [cv.take([1024], BF16) for _ in range(4)]
        b16t = [cv.take([8, 128], BF16) for _ in range(2)]
        ri = [0]

        def ring(n):
            i = ri[0] % n
            ri[0] += 1
            return i

        for g in range(4):
            i = ring(4)
            P.dma("sync", f32s[i][:, 0:128], w_s[g], w=[f"ws:f32s{i}"], sem=f"f32s{i}")
            V("tensor_tensor", [f"ws:f32s{i}", "trilm"], [f"ws:f32s{i}"], out=f32s[i][:, 0:128], in0=f32s[i][:, 0:128],
              in1=trilm[:], op=ALU.mult)
            TR([f"ws:f32s{i}", "identf"], ["ws:pb6"], out=pb[6][:, 0:128], in_=f32s[i][:, 0:128], identity=identf[:])
            V("tensor_copy", ["ws:pb6"], [f"wsT{g}"], out=wsT[:, g, :], in_=pb[6][:, 0:128])
        for hp in range(16):
            i = ring(4)
            P.dma("sync", f32s[i][:, 0:128], peer_keys[hp], w=[f"ws:f32s{i}"], sem=f"f32s{i}")
            TR([f"ws:f32s{i}", "identf"], ["ws:pb6"], out=pb[6][:, 0:128], in_=f32s[i][:, 0:128], identity=identf[:])
            V("tensor_copy", ["ws:pb6"], [f"keysT{hp}"], out=keysT[:, hp, :], in_=pb[6][:, 0:128])
        for c in range(8):
            for half in range(2):
                i = ring(4)
                P.dma("sync", f32s[i][:], peer_wq[c * 128:(c + 1) * 128, half * 1024:(half + 1) * 1024],
                      w=[f"ws:f32s{i}"], sem=f"f32s{i}")
                V("tensor_scalar", [f"ws:f32s{i}", "gfc"], [f"ws:b16s{i}"], out=b16s[i][:], in0=f32s[i][:],
                  scalar1=gfc[:, c:c + 1], scalar2=None, op0=ALU.mult)
                P.dma("gpsimd", wqb_d[half * 8:(half + 1) * 8, :, c, :].rearrange("h p n -> p h n"),
                      b16s[i][:].rearrange("p (h n) -> p h n", h=8), r=[f"ws:b16s{i}"], w=["wqb_d"],
                      sem=f"b16w{i}")
        if stop_here('p0a', gfin[:], ['gfin','wqb_d'] + [f'keysT{i}' for i in range(16)]):
            return nc
        n_et = 128
        for et in range(n_et):
            i = ring(4)
            P.dma("sync", f32s[i][:], peer_wu[et * 128:(et + 1) * 128, :], w=[f"ws:f32s{i}"], sem=f"f32s{i}")
            A([f"ws:f32s{i}"], [f"ws:b16s{i}"], out=b16s[i][:], in_=f32s[i][:], func=AF.Copy)
            pbk = 6 + (et % 2)
            for c in range(8):
                TR([f"ws:b16s{i}", "identb"], [f"ws:pb{pbk}"], out=pbb[pbk][:, c * 128:(c + 1) * 128],
                   in_=b16s[i][:, c * 128:(c + 1) * 128], identity=identb[:])
            j = et % 2
            V("tensor_tensor", [f"ws:pb{pbk}", "gfcb"], [f"ws:b16t{j}"], out=b16t[j][:],
              in0=pbb[pbk][:].rearrange("p (c e) -> p c e", c=8), in1=gfcb[:].unsqueeze(2).to_broadcast([128, 8, 128]),
              op=ALU.mult)
            P.dma("gpsimd", wuT_d[et], b16t[j][:].rearrange("p c e -> p (c e)"), r=[f"ws:b16t{j}"], w=[f"wuT{et}"],
                  sem=f"b16tw{j}")
            i = ring(4)
            P.dma("sync", f32s[i][:], peer_wv[et * 128:(et + 1) * 128, :], w=[f"ws:f32s{i}"], sem=f"f32s{i}")
            V("tensor_copy", [f"ws:f32s{i}"], [f"ws:b16s{i}"], out=b16s[i][:], in_=f32s[i][:])
            P.dma("gpsimd", wvb_d[et], b16s[i][:], r=[f"ws:b16s{i}"], w=[f"wvb{et}"], sem=f"b16w{i}")

        if stop_here('p0', gfin[:], ['gfin'] + [f'wuT{i}' for i in range(128)] + [f'wvb{i}' for i in range(128)]):
            return nc
        P.fence()
        cv = Carver(ws[:])
        winT = cv.take([8, 2560], BF16)
        woutT = cv.take([8, 1024], BF16)
        hT = cv.take([8, S], BF16)
        qkT = [[cv.take([S], BF16) for _ in range(2)] for _ in range(2)]
        Vaug = cv.take([NT, 4, 130], BF16)
        mix = cv.take([NT, 1024], BF16)
        xin = [cv.take([1024], F32) for _ in range(2)]
        hb = [cv.take([1024], BF16) for _ in range(2)]
        ug = [cv.take([512], F32) for _ in range(2)]
        gvg = [cv.take([512], F32) for _ in range(2)]
        gvn = [cv.take([512], BF16) for _ in range(2)]
        t5a = [cv.take([512], F32) for _ in range(2)]
        pT = [cv.take([128], BF16) for _ in range(4)]
        dtmp = [cv.take([128], F32) for _ in range(2)]
        osb = [cv.take([128], F32) for _ in range(2)]
        osq = [cv.take([128], BF16) for _ in range(2)]
        mixT = [cv.take([8, 128], BF16) for _ in range(2)]
        print("phase1 ws used", cv.off, "of", cv.cap)

        ri[0] = 0
        for c in range(8):
            for (a, b) in ((0, 1024), (1024, 2048), (2048, 2560)):
                i = ring(2)
                P.dma("sync", xin[i][:, 0:b - a], w_in[c * 128:(c + 1) * 128, a:b], w=[f"ws:xin{i}"], sem=f"xin{i}")
                V("tensor_scalar", [f"ws:xin{i}", "gmc"], [f"ws:winT{c}"], out=winT[:, c, a:b], in0=xin[i][:, 0:b - a],
                  scalar1=gmc[:, c:c + 1], scalar2=None, op0=ALU.mult)
        for c in range(8):
            i = ring(2)
            P.dma("sync", xin[i][:], w_out[c * 128:(c + 1) * 128, :], w=[f"ws:xin{i}"], sem=f"xin{i}")
            A([f"ws:xin{i}"], [f"ws:woutT{c}"], out=woutT[:, c, :], in_=xin[i][:], func=AF.Copy)
        winK = [f"ws:winT{c}" for c in range(8)]
        woutK = [f"ws:woutT{c}" for c in range(8)]
        V("memset", [], ["ws:vones"], ap=Vaug[:, :, :, 128:130], constant=1.0)

        for seq in range(nseq):
            r0 = seq * S
            for tt in range(NT):
                i = tt % 2
                P.dma("sync", xin[i][:], x[r0 + tt * 128:r0 + (tt + 1) * 128, :], w=[f"ws:xin{i}"], sem=f"xin{i}")
                ss, ssk = stcol()
                A([f"ws:xin{i}"], [f"ws:hb{i}", ssk], out=hb[i][:], in_=xin[i][:], func=AF.Square, accum_out=ss)
                rs, rsk = stcol()
                rstd_from_ss(ss, ssk, 1, 1.0 / D, rs, rsk)
                V("tensor_scalar", [f"ws:xin{i}", rsk], [f"ws:hb{i}"], out=hb[i][:], in0=xin[i][:], scalar1=rs,
                  scalar2=None, op0=ALU.mult)
                pbk = 6 + i
                for c in range(8):
                    TR([f"ws:hb{i}", "identb"], [f"ws:pb{pbk}"], out=pbb[pbk][:, c * 128:(c + 1) * 128],
                       in_=hb[i][:, c * 128:(c + 1) * 128], identity=identb[:])
                A([f"ws:pb{pbk}"], [f"ws:hT{tt}"], out=hT[:, :, tt * 128:(tt + 1) * 128],
                  in_=pbb[pbk][:].rearrange("p (c t) -> p c t", c=8), func=AF.Copy)
            for tt in range(NT):
                i = tt % 2
                for n in range(3):
                    for c in range(8):
                        MM([f"ws:hT{tt}", winK[c]], [f"ws:pb{n}"], out=pb[n][:, :], lhsT=hT[:, c, tt * 128:(tt + 1) * 128],
                           rhs=winT[:, c, 1024 + n * 512:1024 + (n + 1) * 512], start=(c == 0), stop=(c == 7))
                V("tensor_copy", ["ws:pb0"], [f"ws:V{tt}"], out=Vaug[:, tt, :, 0:128],
                  in_=pb[0][:, :].rearrange("p (h e) -> p h e", h=4))
                A(["ws:pb1"], [f"ws:ug{i}"], out=ug[i][:], in_=pb[1][:, :], func=AF.Gelu_apprx_tanh)
                A(["ws:pb2"], [f"ws:gvg{i}"], out=gvg[i][:], in_=pb[2][:, :], func=AF.Gelu_apprx_tanh)
                V("tensor_tensor", [f"ws:gvg{i}"], [f"ws:t5a{i}"], out=t5a[i][:], in0=gvg[i][:], in1=gvg[i][:], op=ALU.mult)
                s4, s4k = stcol(4)
                V("tensor_reduce", [f"ws:t5a{i}"], [s4k], out=s4, in_=t5a[i][:].rearrange("p (g c) -> p g c", g=4),
                  axis=AX.X, op=ALU.add)
                r4, r4k = stcol(4)
                rstd_from_ss(s4, s4k, 4, 1.0 / 128, r4, r4k)
                V("tensor_tensor", [f"ws:gvg{i}", r4k], [f"ws:t5a{i}"], out=t5a[i][:].rearrange("p (g c) -> p g c", g=4),
                  in0=gvg[i][:].rearrange("p (g c) -> p g c", g=4), in1=r4.unsqueeze(2).to_broadcast([128, 4, 128]),
                  op=ALU.mult)
                V("tensor_tensor", [f"ws:t5a{i}", "ggv"], [f"ws:gvn{i}"], out=gvn[i][:], in0=t5a[i][:], in1=ggv[:], op=ALU.mult)
                for g in range(4):
                    MM([f"ws:gvn{i}", f"wsT{g}"], ["ws:pb3"], out=pb[3][:, g * 128:(g + 1) * 128], lhsT=wsT[:, g, :],
                       rhs=gvn[i][:, g * 128:(g + 1) * 128], start=True, stop=True)
                for g in range(4):
                    V("scalar_tensor_tensor", ["ws:pb3", "bsT", f"ws:ug{i}"], [f"ws:t5a{i}"],
                      out=t5a[i][:, g * 128:(g + 1) * 128], in0=pb[3][:, g * 128:(g + 1) * 128], scalar=bsT[:, g:g + 1],
                      in1=ug[i][:, g * 128:(g + 1) * 128], op0=ALU.add, op1=ALU.mult)
                V("tensor_tensor", [f"ws:t5a{i}"], [f"ws:gvg{i}"], out=gvg[i][:], in0=t5a[i][:], in1=t5a[i][:], op=ALU.mult)
                s4, s4k = stcol(4)
                V("tensor_reduce", [f"ws:gvg{i}"], [s4k], out=s4, in_=gvg[i][:].rearrange("p (g c) -> p g c", g=4),
                  axis=AX.X, op=ALU.add)
                r4, r4k = stcol(4)
                rstd_from_ss(s4, s4k, 4, 1.0 / 128, r4, r4k)
                V("tensor_tensor", [f"ws:t5a{i}", r4k], [f"ws:gvg{i}"], out=gvg[i][:].rearrange("p (g c) -> p g c", g=4),
                  in0=t5a[i][:].rearrange("p (g c) -> p g c", g=4), in1=r4.unsqueeze(2).to_broadcast([128, 4, 128]),
                  op=ALU.mult)
                V("tensor_tensor", [f"ws:gvg{i}", "ggo"], [f"ws:mixg{tt}"], out=mix[:, tt, 512:1024], in0=gvg[i][:],
                  in1=ggo[:], op=ALU.mult)
            pair = 0
            cntm = [0, 0]
            for h in range(4):
                sl = h % 2
                for which in range(2):
                    for tg in range(4):
                        pbk = 6 + (tg % 2)
                        for c in range(8):
                            MM([f"ws:hT{t}" for t in range(tg * 4, tg * 4 + 4)] + [winK[c]], [f"ws:pb{pbk}"],
                               out=pb[pbk][:, :], lhsT=winT[:, c, which * 512 + h * 128:which * 512 + (h + 1) * 128],
                               rhs=hT[:, c, tg * 512:(tg + 1) * 512], start=(c == 0), stop=(c == 7))
                        if tg % 2 == 0:
                            A([f"ws:pb{pbk}"], [f"ws:qk{sl}{which}_{tg}"], out=qkT[sl][which][:, tg * 512:(tg + 1) * 512],
                              in_=pb[pbk][:, :], func=AF.Copy)
                        else:
                            V("tensor_copy", [f"ws:pb{pbk}"], [f"ws:qk{sl}{which}_{tg}"],
                              out=qkT[sl][which][:, tg * 512:(tg + 1) * 512], in_=pb[pbk][:, :])
                qT_, kT_ = qkT[sl][0], qkT[sl][1]
                pairs = [(qt, m, j) for qt in range(NT) for m in range(2) for j in range(qt + 1)]
                LA = 4
                pbase = pair
                assign = []
                for (qt_, m_, j_) in pairs:
                    c_ = cntm[m_]
                    cntm[m_] += 1
                    assign.append((((0, 6), (1, 7))[m_][(c_ // 4) % 2], c_ % 4))
                pair += len(pairs)

                def rec_S(i2_, h=h, sl=sl, qT_=qT_, kT_=kT_, pbase=pbase, pairs=pairs, assign=assign):
                    qt, m, j = pairs[i2_]
                    sbank, sslot = assign[i2_]
                    sk = f"ws:pb{sbank}"
                    sview = pb[sbank][:, sslot * 128:(sslot + 1) * 128]
                    MM([f"ws:qk{sl}0_{qt // 4}", f"ws:qk{sl}1_{j // 4}"], [sk], out=sview,
                       lhsT=kT_[m * 64:(m + 1) * 64, j * 128:(j + 1) * 128],
                       rhs=qT_[m * 64:(m + 1) * 64, qt * 128:(qt + 1) * 128], start=True, stop=True)

                def rec_rest(i2_, h=h, pbase=pbase, pairs=pairs, assign=assign):
                    qt, m, j = pairs[i2_]
                    sbank, sslot = assign[i2_]
                    sk = f"ws:pb{sbank}"
                    sview = pb[sbank][:, sslot * 128:(sslot + 1) * 128]
                    ob = 2 + (qt % 2) * 2 + m
                    ps_ = (pbase + i2_) % 4
                    if j < qt:
                        A([sk, f"bcol{h}"], [f"ws:pT{ps_}"], out=pT[ps_][:], in_=sview, func=AF.Exp,
                          bias=bcol[:, h, qt - j:qt - j + 1], scale=0.125)
                    else:
                        dsl = m
                        V("scalar_tensor_tensor", [sk, f"MdT{h}"], [f"ws:dtmp{dsl}"], out=dtmp[dsl][:],
                          in0=sview, scalar=0.125, in1=MdT[:, h, :], op0=ALU.mult, op1=ALU.add)
                        A([f"ws:dtmp{dsl}"], [f"ws:pT{ps_}"], out=pT[ps_][:], in_=dtmp[dsl][:], func=AF.Exp)
                    MM([f"ws:pT{ps_}", f"ws:V{j}", "ws:vones"], [f"ws:pb{ob}"], out=pb[ob][:, 0:129],
                       lhsT=pT[ps_][:], rhs=Vaug[:, j, h, 0:129], start=(j == 0), stop=(j == qt))
                    if not (m == 1 and j == qt):
                        return
                    o1, o2 = 2 + (qt % 2) * 2, 2 + (qt % 2) * 2 + 1
                    osl = qt % 2
                    rz, rzk = stcol(2)
                    V("reciprocal", [f"ws:pb{o1}"], [rzk], out=rz[:, 0:1], in_=pb[o1][:, 128:129])
                    rz2, rz2k = stcol(2)
                    V("reciprocal", [f"ws:pb{o2}"], [rz2k], out=rz2[:, 0:1], in_=pb[o2][:, 128:129])
                    V("tensor_tensor", [rz2k, "neglam"], [rz2k], out=rz2[:, 1:2], in0=rz2[:, 0:1], in1=neglam,
                      op=ALU.mult)
                    V("tensor_scalar", [f"ws:pb{o1}", rzk], [f"ws:osb{osl}"], out=osb[osl][:], in0=pb[o1][:, 0:128],
                      scalar1=rz[:, 0:1], scalar2=None, op0=ALU.mult)
                    V("scalar_tensor_tensor", [f"ws:pb{o2}", rz2k, f"ws:osb{osl}"], [f"ws:osb{osl}"],
                      out=osb[osl][:], in0=pb[o2][:, 0:128], scalar=rz2[:, 1:2], in1=osb[osl][:], op0=ALU.mult,
                      op1=ALU.add)
                    sso, ssok = stcol()
                    A([f"ws:osb{osl}"], [f"ws:osq{osl}", ssok], out=osq[osl][:], in_=osb[osl][:], func=AF.Square,
                      accum_out=sso)
                    ro, rok = stcol()
                    rstd_from_ss(sso, ssok, 1, 1.0 / 128, ro, rok)
                    V("scalar_tensor_tensor", [f"ws:osb{osl}", rok, "gsub"], [f"ws:mixa{qt}_{h}"],
                      out=mix[:, qt, h * 128:(h + 1) * 128], in0=osb[osl][:], scalar=ro, in1=gsub[:], op0=ALU.mult,
                      op1=ALU.mult)

                for i2_ in range(min(LA, len(pairs))):
                    rec_S(i2_)
                for i2_ in range(len(pairs)):
                    if i2_ + LA < len(pairs):
                        rec_S(i2_ + LA)
                    rec_rest(i2_)
            for tt in range(NT):
                i = tt % 2
                pbk = 6 + i
                mk = [f"ws:mixa{tt}_{h}" for h in range(4)] + [f"ws:mixg{tt}"]
                for c in range(8):
                    TR(mk + ["identb"], [f"ws:pb{pbk}"], out=pbb[pbk][:, c * 128:(c + 1) * 128],
                       in_=mix[:, tt, c * 128:(c + 1) * 128], identity=identb[:])
                A([f"ws:pb{pbk}"], [f"ws:mixT{i}"], out=mixT[i][:], in_=pbb[pbk][:].rearrange("p (c t) -> p c t", c=8),
                  func=AF.Copy)
                P.dma("sync", xin[i][:], x[r0 + tt * 128:r0 + (tt + 1) * 128, :], w=[f"ws:xin{i}"], sem=f"xin{i}")
                for n in range(2):
                    for c in range(8):
                        MM([f"ws:mixT{i}", woutK[c]], [f"ws:pb{n}"], out=pb[n][:, :], lhsT=mixT[i][:, c, :],
                           rhs=woutT[:, c, n * 512:(n + 1) * 512], start=(c == 0), stop=(c == 7))
                for n in range(2):
                    V("tensor_tensor", [f"ws:pb{n}", f"ws:xin{i}"], [f"ws:xin{i}"], out=xin[i][:, n * 512:(n + 1) * 512],
                      in0=pb[n][:, :], in1=xin[i][:, n * 512:(n + 1) * 512], op=ALU.add)
                P.dma("gpsimd", x1s[r0 + tt * 128:r0 + (tt + 1) * 128, :], xin[i][:], r=[f"ws:xin{i}"],
                      w=[f"x1s{seq}_{tt}"], sem=f"x1w{i}")

        if stop_after == 'p1':
            o = P.dma("sync", out[0:S, :], x1s[0:S, :], r=[f'x1s0_{i}' for i in range(16)], sem="stopout")
            P.emit(final_wait_ops=[o])
            return nc
        P.fence()
        cv = Carver(ws[:])
        AT = cv.take([256, 128], BF16)
        xnT = [cv.take([8, 256], BF16) for _ in range(2)]
        qT2 = cv.take([16, 256], BF16)
        x1t = [[cv.take([1024], F32) for _ in range(2)] for _ in range(2)]
        xnb = [cv.take([1024], BF16) for _ in range(2)]
        wqt = [cv.take([8, 128], BF16) for _ in range(2)]
        scores = cv.take([16, 128], F32)
        vals = cv.take([16, 16], F32)
        idxu = cv.take([16, 16], U32)
        idxf = cv.take([16, 16], F32)
        cand = cv.take([8, 256], F32)
        best = cv.take([8, 16], F32)
        fidx = cv.take([8, 16], U32)
        r1u = cv.take([8, 16], U32)
        r2u = cv.take([8, 16], U32)
        r1f = cv.take([8, 16], F32)
        r2f = cv.take([8, 16], F32)
        oht = cv.take([4, 16, 16], F32)
        oht2 = cv.take([4, 16, 16], F32)
        i1f = cv.take([8, 16], F32)
        i2f = cv.take([8, 16], F32)
        gf = cv.take([8, 16], F32)
        ex = cv.take([8, 16], F32)
        i1T = [cv.take([256], BF16) for _ in range(2)]
        i2T = [cv.take([256], BF16) for _ in range(2)]
        gT = [cv.take([256], BF16) for _ in range(2)]
        TGK = 16
        OH1 = [cv.take([TGK, 128], BF16) for _ in range(2)]
        E1w = [cv.take([TGK, 128], BF16) for _ in range(2)]
        OH2 = [cv.take([TGK, 128], BF16) for _ in range(2)]
        wu = [cv.take([8, 128], BF16) for _ in range(4)]
        wv = [cv.take([1024], BF16) for _ in range(4)]
        gl = [cv.take([256], BF16) for _ in range(2)]
        GT = [cv.take([256], BF16) for _ in range(2)]
        outsb = cv.take([1024], F32)
        print("phase2 ws used", cv.off, "of", cv.cap)

        out_ops = []
        nblk = NTOK // 256

        def prep_gen(b):
            p = b % 2
            R0 = b * 256
            seq = R0 // S
            for tt in range(2):
                gtt = (R0 % S) // 128 + tt
                xk = f"ws:x1t{p}{tt}"
                P.dma("sync", x1t[p][tt][:], x1s[R0 + tt * 128:R0 + (tt + 1) * 128, :], r=[f"x1s{seq}_{gtt}"],
                      w=[xk], sem=f"x1t{p}{tt}")
                ss, ssk = stcol()
                A([xk], [f"ws:xnb{tt}", ssk], out=xnb[tt][:], in_=x1t[p][tt][:], func=AF.Square, accum_out=ss)
                rs, rsk = stcol()
                rstd_from_ss(ss, ssk, 1, 1.0 / D, rs, rsk)
                V("tensor_scalar", [xk, rsk], [f"ws:xnb{tt}"], out=xnb[tt][:], in0=x1t[p][tt][:], scalar1=rs,
                  scalar2=None, op0=ALU.mult)
                yield
                pbk = 6 + tt
                for c in range(8):
                    TR([f"ws:xnb{tt}", "identb"], [f"ws:pb{pbk}"], out=pbb[pbk][:, c * 128:(c + 1) * 128],
                       in_=xnb[tt][:, c * 128:(c + 1) * 128], identity=identb[:])
                A([f"ws:pb{pbk}"], [f"ws:xnT{p}{tt}"], out=xnT[p][:, :, tt * 128:(tt + 1) * 128],
                  in_=pbb[pbk][:].rearrange("p (c t) -> p c t", c=8), func=AF.Copy)
                yield
            xnK = [f"ws:xnT{p}0", f"ws:xnT{p}1"]
            for hp in range(16):
                wi = hp % 2
                P.dma("sync", wqt[wi][:], wqb_d[hp], r=["wqb_d"], w=[f"ws:wqt{wi}"], sem=f"wqt{wi}")
                hs = hp % 2
                hv = pb[6 + hs][:, 0:256]
                hk = f"ws:pb{6 + hs}"
                for c in range(8):
                    MM(xnK + [f"ws:wqt{wi}"], [hk], out=hv, lhsT=wqt[wi][:, c, :], rhs=xnT[p][:, c, :], start=(c == 0),
                       stop=(c == 7))
                if hp % 2 == 0:
                    A([hk], [f"ws:qT2_{hp}"], out=qT2[:, hp, :], in_=hv, func=AF.Copy)
                else:
                    V("tensor_copy", [hk], [f"ws:qT2_{hp}"], out=qT2[:, hp, :], in_=hv)
                yield
            for tt in range(2):
                for q4 in range(4):
                    pbk = 6 + (q4 % 2)
                    for hq in range(4):
                        hp = q4 * 4 + hq
                        MM([f"ws:qT2_{hp}", f"keysT{hp}"], [f"ws:pb{pbk}"], out=pb[pbk][:, hq * 128:(hq + 1) * 128],
                           lhsT=qT2[:, hp, tt * 128:(tt + 1) * 128], rhs=keysT[:, hp, :], start=True, stop=True)
                    A([f"ws:pb{pbk}"], [f"ws:sc{q4}"], out=scores[:, q4 * 4:(q4 + 1) * 4, :],
                      in_=pb[pbk][:, :].rearrange("p (a n) -> p a n", a=4), func=AF.Copy)
                    yield
                for hp in range(16):
                    sk = f"ws:sc{hp // 4}"
                    sc = scores[:, hp, :]
                    V("max", [sk], [f"ws:vals{hp}a"], out=vals[:, hp, 0:8], in_=sc)
                    V("max_index", [sk, f"ws:vals{hp}a"], [f"ws:idx{hp}a"], out=idxu[:, hp, 0:8], in_max=vals[:, hp, 0:8],
                      in_values=sc)
                    V("match_replace", [sk, f"ws:vals{hp}a"], [sk], out=sc, in_to_replace=vals[:, hp, 0:8], in_values=sc,
                      imm_value=-1e30)
                    V("max", [sk], [f"ws:vals{hp}b"], out=vals[:, hp, 8:16], in_=sc)
                    V("max_index", [sk, f"ws:vals{hp}b"], [f"ws:idx{hp}b"], out=idxu[:, hp, 8:16], in_max=vals[:, hp, 8:16],
                      in_values=sc)
                    yield
                valK = [f"ws:vals{hp}{ab}" for hp in range(16) for ab in "ab"]
                idxK = [f"ws:idx{hp}{ab}" for hp in range(16) for ab in "ab"]
                V("tensor_copy", idxK, ["ws:idxf"], out=idxf[:], in_=idxu[:])
                v4 = vals[:].rearrange("p (h t) k -> p h t k", t=2)
                V("tensor_tensor", valK, ["ws:cand"], out=cand[:].rearrange("p h (a b) -> p h a b", a=16),
                  in0=v4[:, :, 0, :].unsqueeze(3).to_broadcast([128, 8, 16, 16]),
                  in1=v4[:, :, 1, :].unsqueeze(2).to_broadcast([128, 8, 16, 16]), op=ALU.add)
                yield
                for h in range(8):
                    ck = f"ws:cand{h}"
                    dep = ["ws:cand"]
                    V("max", dep, [f"ws:best{h}a"], out=best[:, h, 0:8], in_=cand[:, h, :])
                    V("max_index", dep + [f"ws:best{h}a"], [f"ws:fidx{h}a"], out=fidx[:, h, 0:8], in_max=best[:, h, 0:8],
                      in_values=cand[:, h, :])
                    V("match_replace", dep + [f"ws:best{h}a"], [ck], out=cand[:, h, :], in_to_replace=best[:, h, 0:8],
                      in_values=cand[:, h, :], imm_value=-1e30)
                    V("max", [ck], [f"ws:best{h}b"], out=best[:, h, 8:16], in_=cand[:, h, :])
                    V("max_index", [ck, f"ws:best{h}b"], [f"ws:fidx{h}b"], out=fidx[:, h, 8:16], in_max=best[:, h, 8:16],
                      in_values=cand[:, h, :])
                    yield
                bestK = [f"ws:best{h}{ab}" for h in range(8) for ab in "ab"]
                fidxK = [f"ws:fidx{h}{ab}" for h in range(8) for ab in "ab"]
                V("tensor_tensor", bestK, ["ws:ex"], out=ex[:], in0=best[:],
                  in1=best[:, :, 0:1].to_broadcast([128, 8, 16]), op=ALU.subtract)
                A(["ws:ex"], ["ws:ex2"], out=ex[:], in_=ex[:], func=AF.Exp)
                zt, ztk = stat[:, 48:56], "statz"
                V("tensor_reduce", ["ws:ex2"], [ztk], out=zt, in_=ex[:], axis=AX.X, op=ALU.add)
                zr, zrk = stat[:, 56:64], "statzr"
                V("reciprocal", [ztk], [zrk], out=zr, in_=zt)
                V("tensor_tensor", ["ws:ex2", zrk], ["ws:gf"], out=gf[:], in0=ex[:],
                  in1=zr.unsqueeze(2).to_broadcast([128, 8, 16]), op=ALU.mult)
                yield
                V("tensor_single_scalar", fidxK, ["ws:r1u"], out=r1u[:], in_=fidx[:], scalar=4, op=ALU.logical_shift_right)
                V("tensor_single_scalar", fidxK, ["ws:r2u"], out=r2u[:], in_=fidx[:], scalar=15, op=ALU.bitwise_and)
                V("tensor_copy", ["ws:r1u"], ["ws:r1f"], out=r1f[:], in_=r1u[:])
                V("tensor_copy", ["ws:r2u"], ["ws:r2f"], out=r2f[:], in_=r2u[:])
                yield
                idx4 = idxf[:].rearrange("p (h t) k -> p h t k", t=2)
                io16 = iorow[:, 0:16].unsqueeze(1).unsqueeze(1).to_broadcast([128, 4, 16, 16])
                for (rf, rk, tsel, dst, dk) in ((r1f, "ws:r1f", 0, i1f, "ws:i1f"), (r2f, "ws:r2f", 1, i2f, "ws:i2f")):
                    for hh in range(2):
                        hsl = slice(hh * 4, hh * 4 + 4)
                        V("tensor_tensor", [rk, "iorow"], ["ws:oht"], out=oht[:],
                          in0=rf[:, hsl, :].unsqueeze(3).to_broadcast([128, 4, 16, 16]), in1=io16, op=ALU.is_equal)
                        V("tensor_tensor", ["ws:oht", "ws:idxf"], ["ws:oht2"], out=oht2[:], in0=oht[:],
                          in1=idx4[:, hsl, tsel, :].unsqueeze(2).to_broadcast([128, 4, 16, 16]), op=ALU.mult)
                        V("tensor_reduce", ["ws:oht2"], [dk + str(hh)], out=dst[:, hsl, :], in_=oht2[:], axis=AX.X,
                          op=ALU.add)
                        yield
                for (src, sks, dstT, dk) in ((i1f, ["ws:i1f0", "ws:i1f1"], i1T, "ws:i1T"),
                                             (i2f, ["ws:i2f0", "ws:i2f1"], i2T, "ws:i2T"), (gf, ["ws:gf"], gT, "ws:gT")):
                    TR(sks + ["identf"], ["ws:pb7"], out=pb[7][:, 0:128], in_=src[:].rearrange("p h k -> p (h k)"),
                       identity=identf[:])
                    V("tensor_copy", ["ws:pb7"], [f"{dk}{p}{tt}"], out=dstT[p][:, tt * 128:(tt + 1) * 128],
                      in_=pb[7][:, 0:128])
                    yield

        def build_AT(b):
            p = b % 2
            io_b = iob[:].unsqueeze(1).to_broadcast([128, TGK, 128])
            for g in range(256 // TGK):
                sl = g % 2
                tt = (g * TGK) // 128
                tsl = slice(g * TGK, (g + 1) * TGK)
                V("tensor_tensor", ["iob", f"ws:i2T{p}{tt}"], [f"ws:OH2{sl}"], out=OH2[sl][:], in0=io_b,
                  in1=i2T[p][:, tsl].unsqueeze(2).to_broadcast([128, TGK, 128]), op=ALU.is_equal)
                V("tensor_tensor", ["iob", f"ws:i1T{p}{tt}"], [f"ws:OH1{sl}"], out=OH1[sl][:], in0=io_b,
                  in1=i1T[p][:, tsl].unsqueeze(2).to_broadcast([128, TGK, 128]), op=ALU.is_equal)
                G("tensor_tensor", [f"ws:OH1{sl}", f"ws:gT{p}{tt}"], [f"ws:E1w{sl}"], out=E1w[sl][:], in0=OH1[sl][:],
                  in1=gT[p][:, tsl].unsqueeze(2).to_broadcast([128, TGK, 128]), op=ALU.mult)
                for k4 in range(TGK // 4):
                    pbk = 6 + (k4 % 2)
                    for kk in range(4):
                        k = k4 * 4 + kk
                        MM([f"ws:OH2{sl}", f"ws:E1w{sl}"], [f"ws:pb{pbk}"], out=pb[pbk][:, kk * 128:(kk + 1) * 128],
                           lhsT=OH2[sl][:, k, :], rhs=E1w[sl][:, k, :], start=True, stop=True)
                    t0 = g * TGK + k4 * 4
                    A([f"ws:pb{pbk}"], ["ws:AT"], out=AT[:, t0:t0 + 4, :],
                      in_=pb[pbk][:, :].rearrange("p (t i) -> p t i", t=4), func=AF.Copy)

        def sweep(b, gen):
            p = b % 2
            xnK = [f"ws:xnT{p}0", f"ws:xnT{p}1"]

            def rec_H(et):
                gi = b * 128 + et
                wi = gi % 4
                P.dma("sync", wu[wi][:].rearrange("p c e -> p (c e)"), wuT_d[et], r=[f"wuT{et}"], w=[f"ws:wu{wi}"],
                      sem=f"wu{wi}")
                P.dma("sync", wv[wi][:], wvb_d[et], r=[f"wvb{et}"], w=[f"ws:wv{wi}"], sem=f"wv{wi}")
                hs = gi % 2
                for c in range(8):
                    MM(xnK + [f"ws:wu{wi}"], [f"ws:pb{4 + hs}"], out=pb[4 + hs][:, 0:256], lhsT=wu[wi][:, c, :],
                       rhs=xnT[p][:, c, :], start=(c == 0), stop=(c == 7))

            def rec_rest(et):
                gi = b * 128 + et
                wi = gi % 4
                hs = gi % 2
                gs = gi % 2
                A([f"ws:pb{4 + hs}"], [f"ws:gl{gs}"], out=gl[gs][:], in_=pb[4 + hs][:, 0:256], func=AF.Gelu_apprx_tanh)
                V("tensor_tensor", [f"ws:gl{gs}", "ws:AT"], [f"ws:GT{gs}"], out=GT[gs][:], in0=gl[gs][:],
                  in1=AT[:, :, et], op=ALU.mult)
                for tt in range(2):
                    for dh in range(2):
                        MM([f"ws:GT{gs}", f"ws:wv{wi}"], [f"ws:pb{tt * 2 + dh}"], out=pb[tt * 2 + dh][:, :],
                           lhsT=GT[gs][:, tt * 128:(tt + 1) * 128], rhs=wv[wi][:, dh * 512:(dh + 1) * 512],
                           start=(et == 0), stop=(et == 127))

            rec_H(0)
            for et in range(128):
                if et + 1 < 128:
                    rec_H(et + 1)
                rec_rest(et)
                if gen is not None and et >= 2:
                    next(gen, None)
            if gen is not None:
                for _ in gen:
                    pass

        def finish(b):
            p = b % 2
            R0 = b * 256
            for tt in range(2):
                xk = f"ws:x1t{p}{tt}"
                for dh in range(2):
                    V("tensor_tensor", [f"ws:pb{tt * 2 + dh}", xk], [xk],
                      out=x1t[p][tt][:, dh * 512:(dh + 1) * 512], in0=pb[tt * 2 + dh][:, :],
                      in1=x1t[p][tt][:, dh * 512:(dh + 1) * 512], op=ALU.add)
                ss, ssk = stcol()
                A([xk], ["ws:outsb", ssk], out=outsb[:], in_=x1t[p][tt][:], func=AF.Square, accum_out=ss)
                rs, rsk = stcol()
                rstd_from_ss(ss, ssk, 1, 1.0 / D, rs, rsk)
                V("scalar_tensor_tensor", [xk, rsk, "gfin"], ["ws:outsb"], out=outsb[:],
                  in0=x1t[p][tt][:], scalar=rs, in1=gfin[:], op0=ALU.mult, op1=ALU.mult)
                out_ops.append(P.dma("gpsimd", out[R0 + tt * 128:R0 + (tt + 1) * 128, :], outsb[:],
                                     r=["ws:outsb"], sem="outw"))

        for _ in prep_gen(0):
            pass
        for b in range(nblk):
            build_AT(b)
            sweep(b, prep_gen(b + 1) if b + 1 < nblk else None)
            finish(b)
        print("n ops", len(P.ops))
        P.emit(final_wait_ops=out_ops)
    return nc


def kernel(x, w_in, lam_q1, lam_k1, lam_q2, lam_k2, g_subln, w_s, b_s, g_gv, g_gout, w_out, g_mix, g_ffn,
           peer_wq, peer_keys, peer_wu, peer_wv, g_final):
    f = lambda a: np.ascontiguousarray(np.asarray(a, dtype=np.float32))
    x = f(x)
    B = x.shape[0]
    nseq = B // NCORES
    nc = build_nc(nseq)
    common = {
        "w_in": f(w_in[0]),
        "lamv": f(np.stack([np.asarray(lam_q1)[0], np.asarray(lam_k1)[0], np.asarray(lam_q2)[0], np.asarray(lam_k2)[0]])),
        "g_subln": f(g_subln[0]), "w_s": f(w_s[0]), "b_s": f(b_s[0]), "g_gv": f(g_gv[0]), "g_gout": f(g_gout[0]),
        "w_out": f(w_out[0]), "g_mix": f(g_mix[0]), "g_ffn": f(g_ffn[0]), "peer_wq": f(peer_wq[0]),
        "peer_keys": f(np.asarray(peer_keys)[0].reshape(16, 128, 128)), "peer_wu": f(peer_wu[0]),
        "peer_wv": f(peer_wv[0]), "g_final": f(g_final),
    }
    in_maps = []
    for c in range(NCORES):
        m = dict(common)
        m["x"] = x[c * nseq:(c + 1) * nseq].reshape(nseq * S, D)
        in_maps.append(m)
    res = run_bass_kernel_spmd(nc, in_maps, core_ids=list(range(NCORES)))
    outs = [np.asarray(r["out"]).reshape(nseq, S, D) for r in res.results]
    return np.concatenate(outs, axis=0).astype(np.float32)
```

```python
import numpy as np
from contextlib import ExitStack
import concourse.bass as bass
import concourse.mybir as mybir
from concourse.bass_utils import run_bass_kernel_spmd

F32 = mybir.dt.float32
BF16 = mybir.dt.bfloat16
U32 = mybir.dt.uint32
AF = mybir.ActivationFunctionType
ALU = mybir.AluOpType
AX = mybir.AxisListType

ENGS = ["sync", "scalar", "vector", "gpsimd", "tensor"]
EPS = 1e-6
NCORES = 8
D = 1024
S = 2048
NT = S // 128
NEG = -30000.0


class Prog:
    def __init__(self, nc):
        self.nc = nc
        self.ops = []
        self.last_w = {}
        self.readers = {}
        self.dma_sem_count = {}
        self.epoch = 0
        self.key_epoch = {}
        self.fence_ops = []
        self.last_eng_op = {}
        self.last_dma_op = {}

    def fence(self):
        self.epoch += 1
        self.fence_ops = list(self.last_eng_op.values()) + list(self.last_dma_op.values())

    def _add(self, eng, fn, r, w, dma_sem=None):
        idx = len(self.ops)
        pk = [k for k in r if k.startswith("ws:pb")]
        if pk:
            r = [k for k in r if not k.startswith("ws:pb")]
            w = list(w) + pk
        deps = set()
        for k in list(r) + list(w):
            if k.startswith("ws:") and self.key_epoch.get(k) != self.epoch:
                self.key_epoch[k] = self.epoch
                self.last_w.pop(k, None)
                self.readers.pop(k, None)
                deps.update(self.fence_ops)
        raw = set()
        for k in r:
            lw = self.last_w.get(k)
            if lw is not None:
                deps.add(lw)
                raw.add(lw)
        for k in w:
            lw = self.last_w.get(k)
            if lw is not None:
                deps.add(lw)
            deps.update(self.readers.get(k, ()))
        real = set()
        for d in deps:
            od = self.ops[d]
            if od["dma_sem"] is None and od["eng"] == eng and d not in raw:
                continue
            real.add(d)
        op = dict(eng=eng, fn=fn, deps=real, dma_sem=dma_sem, idx=idx)
        if dma_sem is not None:
            c = self.dma_sem_count.get(dma_sem, 0) + 16
            self.dma_sem_count[dma_sem] = c
            op["val"] = c
            self.last_dma_op[dma_sem] = idx
        else:
            self.last_eng_op[eng] = idx
        self.ops.append(op)
        for k in r:
            self.readers.setdefault(k, []).append(idx)
        for k in w:
            self.last_w[k] = idx
            self.readers[k] = []
        return idx

    def op(self, eng, fn, r=(), w=()):
        return self._add(eng, fn, r, w)

    def dma(self, eng, out, in_, r=(), w=(), sem="dma", **kw):
        def fn(e, out=out, in_=in_, kw=kw):
            return e.dma_start(out=out, in_=in_, **kw)
        return self._add(eng, fn, r, w, dma_sem=sem)

    def emit(self, final_wait_ops=()):
        nc = self.nc
        ops = self.ops
        for o in ops:
            newest = {}
            for d in o["deps"]:
                od = ops[d]
                q = ("dma", od["dma_sem"]) if od["dma_sem"] is not None else ("eng", od["eng"])
                if q not in newest or d > newest[q]:
                    newest[q] = d
            o["deps"] = set(newest.values())
        needs_inc = [False] * len(ops)
        for o in ops:
            for d in o["deps"]:
                needs_inc[d] = True
        for d in final_wait_ops:
            needs_inc[d] = True
        SEGN = 30000
        cnt = {e: 0 for e in ENGS}
        semnames = []
        for o in ops:
            if o["dma_sem"] is None:
                if needs_inc[o["idx"]]:
                    c = cnt[o["eng"]]
                    cnt[o["eng"]] += 1
                    o["sem"] = "eng_%s_%d" % (o["eng"], c // SEGN)
                    o["val"] = c % SEGN + 1
                    if o["sem"] not in semnames:
                        semnames.append(o["sem"])
                else:
                    o["sem"] = None
                    o["val"] = None
            else:
                o["sem"] = "dma_" + str(o["dma_sem"])
        semnames += ["dma_" + str(k) for k in self.dma_sem_count]
        with ExitStack() as st:
            sems = {n: st.enter_context(nc.semaphore(n)) for n in semnames}
            block = st.enter_context(nc.Block())
            per_eng = {e: [o for o in ops if o["eng"] == e] for e in ENGS}

            def make(ename):
                def body(eng):
                    waited = {}
                    for o in per_eng[ename]:
                        for d in sorted(o["deps"]):
                            od = ops[d]
                            s, v = od["sem"], od["val"]
                            if waited.get(s, 0) >= v:
                                continue
                            eng.wait_ge(sems[s], v)
                            waited[s] = v
                        ins = o["fn"](eng)
                        if o["dma_sem"] is not None:
                            ins.then_inc(sems[o["sem"]], 16)
                        elif o["val"] is not None:
                            ins.then_inc(sems[o["sem"]], 1)
                    if ename == "sync":
                        for d in final_wait_ops:
                            od = ops[d]
                            if waited.get(od["sem"], 0) >= od["val"]:
                                continue
                            eng.wait_ge(sems[od["sem"]], od["val"])
                            waited[od["sem"]] = od["val"]
                return body

            block.sync(make("sync"))
            block.scalar(make("scalar"))
            block.vector(make("vector"))
            block.gpsimd(make("gpsimd"))
            block.tensor(make("tensor"))


class Carver:
    def __init__(self, base):
        self.base = base
        self.off = 0
        self.cap = base.shape[1]

    def take(self, shape, dtype):
        esz = 2 if dtype == BF16 else 4
        n = int(np.prod(shape)) * esz // 2
        n_al = (n + 15) // 16 * 16
        assert self.off + n_al <= self.cap, ("workspace overflow", self.off, n_al, self.cap)
        v = self.base[:, self.off:self.off + n]
        self.off += n_al
        if dtype != BF16:
            v = v.bitcast(dtype)
        if len(shape) == 2:
            v = v.rearrange("p (a b) -> p a b", a=shape[0])
        elif len(shape) == 3:
            v = v.rearrange("p (a b c) -> p a b c", a=shape[0], b=shape[1])
        return v


def build_nc(nseq, stop_after=None):
    import os
    stop_after = os.environ.get('KSTOP', stop_after)
    NTOK = nseq * S
    nc = bass.Bass("TRN2", target_bir_lowering=False)
    dt = lambda n, s, d=F32, kind="ExternalInput": nc.dram_tensor(n, s, d, kind=kind).ap()
    x = dt("x", [NTOK, D])
    w_in = dt("w_in", [D, 2560])
    lamv = dt("lamv", [4, 64])
    g_subln = dt("g_subln", [128])
    w_s = dt("w_s", [4, 128, 128])
    b_s = dt("b_s", [4, 128])
    g_gv = dt("g_gv", [512])
    g_gout = dt("g_gout", [512])
    w_out = dt("w_out", [D, D])
    g_mix = dt("g_mix", [D])
    g_ffn = dt("g_ffn", [D])
    peer_wq = dt("peer_wq", [D, 2048])
    peer_keys = dt("peer_keys", [16, 128, 128])
    peer_wu = dt("peer_wu", [16384, D])
    peer_wv = dt("peer_wv", [16384, D])
    g_final = dt("g_final", [D])
    out = dt("out", [NTOK, D], F32, "ExternalOutput")
    wuT_d = dt("wuT_d", [128, 128, 1024], BF16, "Internal")
    wvb_d = dt("wvb_d", [128, 128, 1024], BF16, "Internal")
    wqb_d = dt("wqb_d", [16, 128, 8, 128], BF16, "Internal")
    x1s = dt("x1s", [NTOK, D], F32, "Internal")

    WS_ELEMS = (212800 - 21600) // 2 // 16 * 16
    with ExitStack() as st:
        sb = lambda n, s, d: st.enter_context(nc.sbuf_tensor(n, s, d))
        ws = sb("ws", [128, WS_ELEMS], BF16)
        iotf = sb("iotf", [128, 128], F32)
        iorow = sb("iorow", [128, 128], F32)
        iob = sb("iob", [128, 128], BF16)
        identb = sb("identb", [128, 128], BF16)
        identf = sb("identf", [128, 128], F32)
        trilm = sb("trilm", [128, 128], F32)
        MdT = sb("MdT", [128, 4, 128], F32)
        bcol = sb("bcol", [128, 4, 16], F32)
        lamt = sb("lamt", [128, 4, 64], F32)
        lamw = sb("lamw", [128, 8], F32)
        ggv = sb("ggv", [128, 512], F32)
        ggo = sb("ggo", [128, 512], F32)
        gsub = sb("gsub", [128, 128], F32)
        gfin = sb("gfin", [128, 1024], F32)
        gmc = sb("gmc", [128, 8], F32)
        gfc = sb("gfc", [128, 8], F32)
        gfcb = sb("gfcb", [128, 8], BF16)
        bsT = sb("bsT", [128, 4], F32)
        wsT = sb("wsT", [128, 4, 128], BF16)
        keysT = sb("keysT", [128, 16, 128], BF16)
        stat = sb("stat", [128, 64], F32)
        pb = [st.enter_context(nc.psum_tensor(f"ws:pb{i}", [128, 512], F32)) for i in range(8)]
        pbb = [p[:].bitcast(BF16) for p in pb]

        P = Prog(nc)
        V = lambda name, r, w, **kw: P.op("vector", lambda e: getattr(e, name)(**kw), r, w)
        A = lambda r, w, **kw: P.op("scalar", lambda e: e.activation(**kw), r, w)
        G = lambda name, r, w, **kw: P.op("gpsimd", lambda e: getattr(e, name)(**kw), r, w)
        MM = lambda r, w, **kw: P.op("tensor", lambda e: e.matmul(**kw), r, w)
        TR = lambda r, w, **kw: P.op("tensor", lambda e: e.transpose(**kw), r, w)

        statn = [0]

        def stcol(n=1):
            c = statn[0] % 12 * 4
            statn[0] += 1
            return stat[:, c:c + n], f"stat{c}"

        def rstd_from_ss(ss_ap, ss_key, n, inv_n, out_ap, out_key):
            t1, k1 = stcol(n)
            V("tensor_scalar", [ss_key], [k1], out=t1, in0=ss_ap, scalar1=inv_n, scalar2=EPS,
              op0=ALU.mult, op1=ALU.add)
            t2, k2 = stcol(n)
            A([k1], [k2], out=t2, in_=t1, func=AF.Sqrt)
            V("reciprocal", [k2], [out_key], out=out_ap, in_=t2)


        def stop_here(tag, src_ap, rkeys):
            if stop_after != tag:
                return False
            o = P.dma("sync", out[0:128, 0:src_ap.shape[1]], src_ap, r=rkeys, sem="stopout")
            print("STOP at", tag, "n ops", len(P.ops))
            P.emit(final_wait_ops=[o])
            return True
        G("iota", [], ["iotf"], out=iotf[:], pattern=[[1, 128]], base=0, channel_multiplier=-1,
          allow_small_or_imprecise_dtypes=True)
        G("iota", [], ["iorow"], out=iorow[:], pattern=[[1, 128]], base=0, channel_multiplier=0,
          allow_small_or_imprecise_dtypes=True)
        V("tensor_copy", ["iorow"], ["iob"], out=iob[:], in_=iorow[:])
        V("tensor_single_scalar", ["iotf"], ["identb"], out=identb[:], in_=iotf[:], scalar=0.0, op=ALU.is_equal)
        V("tensor_single_scalar", ["iotf"], ["identf"], out=identf[:], in_=iotf[:], scalar=0.0, op=ALU.is_equal)
        V("tensor_single_scalar", ["iotf"], ["trilm"], out=trilm[:], in_=iotf[:], scalar=0.0, op=ALU.is_le)
        absd = MdT[:, 3, :]
        A(["iotf"], ["MdT3"], out=absd, in_=iotf[:], func=AF.Abs)
        V("tensor_tensor", ["MdT3", "iorow"], ["MdT3"], out=absd, in0=iorow[:], in1=absd, op=ALU.subtract)
        slopes = [2.0 ** (-2.0 * (h + 1)) for h in range(4)]
        for h in range(4):
            V("tensor_scalar", ["MdT3"], [f"MdT{h}"], out=MdT[:, h, :], in0=absd, scalar1=slopes[h], scalar2=None,
              op0=ALU.mult)
        for h in range(4):
            V("memset", [], [f"MdT{h}"], ap=MdT[64:128, h, 0:64], constant=NEG)
        G("iota", [], ["bcol3"], out=bcol[:, 3, :], pattern=[[-128, 16]], base=0, channel_multiplier=1,
          allow_small_or_imprecise_dtypes=True)
        for h in range(4):
            V("tensor_scalar", ["bcol3"], [f"bcol{h}"], out=bcol[:, h, :], in0=bcol[:, 3, :], scalar1=slopes[h],
              scalar2=None, op0=ALU.mult)
        P.dma("sync", lamt[:].rearrange("p a b -> p (a b)"), lamv.rearrange("a b -> (a b)").partition_broadcast(128),
              w=["lamt"], sem="c0")
        V("tensor_tensor", ["lamt"], ["lamp"], out=lamt[:, 0, :], in0=lamt[:, 0, :], in1=lamt[:, 1, :], op=ALU.mult)
        V("tensor_tensor", ["lamt"], ["lamp2"], out=lamt[:, 2, :], in0=lamt[:, 2, :], in1=lamt[:, 3, :], op=ALU.mult)
        V("tensor_reduce", ["lamp"], ["lw0"], out=lamw[:, 0:1], in_=lamt[:, 0, :], axis=AX.X, op=ALU.add)
        V("tensor_reduce", ["lamp2"], ["lw1"], out=lamw[:, 1:2], in_=lamt[:, 2, :], axis=AX.X, op=ALU.add)
        A(["lw0", "lw1"], ["lw23"], out=lamw[:, 2:4], in_=lamw[:, 0:2], func=AF.Exp)
        V("tensor_tensor", ["lw23"], ["lw4"], out=lamw[:, 4:5], in0=lamw[:, 3:4], in1=lamw[:, 2:3], op=ALU.subtract)
        V("tensor_scalar", ["lw4"], ["neglam"], out=lamw[:, 5:6], in0=lamw[:, 4:5], scalar1=-0.2, scalar2=None,
          op0=ALU.add)
        neglam = lamw[:, 5:6]
        P.dma("sync", ggv[:], g_gv.partition_broadcast(128), w=["ggv"], sem="c1")
        P.dma("sync", ggo[:], g_gout.partition_broadcast(128), w=["ggo"], sem="c2")
        P.dma("sync", gsub[:], g_subln.partition_broadcast(128), w=["gsubraw"], sem="c3")
        V("tensor_scalar", ["gsubraw"], ["gsub"], out=gsub[:], in0=gsub[:], scalar1=0.8, scalar2=None, op0=ALU.mult)
        P.dma("sync", gfin[:], g_final.partition_broadcast(128), w=["gfin"], sem="c4")
        P.dma("sync", gmc[:], g_mix.rearrange("(c p) -> p c", p=128), w=["gmc"], sem="c5",
              allow_slow_non_contiguous=True)
        P.dma("sync", gfc[:], g_ffn.rearrange("(c p) -> p c", p=128), w=["gfc"], sem="c6",
              allow_slow_non_contiguous=True)
        V("tensor_copy", ["gfc"], ["gfcb"], out=gfcb[:], in_=gfc[:])
        P.dma("sync", bsT[:], b_s.rearrange("g t -> t g"), w=["bsT"], sem="c7", allow_slow_non_contiguous=True)

        if stop_here('const', gfin[:], ['gfin','ggv','ggo','gsub','gmc','gfc','gfcb','bsT','neglam']):
            return nc
        cv = Carver(ws[:])
        f32s = [cv.take([1024], F32) for _ in range(4)]
        b16s = [cv.take([1024], BF16) for _ in range(4)]
        b16t = [cv.take([8, 128], BF16) for _ in range(2)]
        ri = [0]

        def ring(n):
            i = ri[0] % n
            ri[0] += 1
            return i

        for g in range(4):
            i = ring(4)
            P.dma("sync", f32s[i][:, 0:128], w_s[g], w=[f"ws:f32s{i}"], sem=f"f32s{i}")
            V("tensor_tensor", [f"ws:f32s{i}", "trilm"], [f"ws:f32s{i}"], out=f32s[i][:, 0:128], in0=f32s[i][:, 0:128],
              in1=trilm[:], op=ALU.mult)
            TR([f"ws:f32s{i}", "identf"], ["ws:pb6"], out=pb[6][:, 0:128], in_=f32s[i][:, 0:128], identity=identf[:])
            V("tensor_copy", ["ws:pb6"], [f"wsT{g}"], out=wsT[:, g, :], in_=pb[6][:, 0:128])
        for hp in range(16):
            i = ring(4)
            P.dma("sync", f32s[i][:, 0:128], peer_keys[hp], w=[f"ws:f32s{i}"], sem=f"f32s{i}")
            TR([f"ws:f32s{i}", "identf"], ["ws:pb6"], out=pb[6][:, 0:128], in_=f32s[i][:, 0:128], identity=identf[:])
            V("tensor_copy", ["ws:pb6"], [f"keysT{hp}"], out=keysT[:, hp, :], in_=pb[6][:, 0:128])
        for c in range(8):
            for half in range(2):
                i = ring(4)
                P.dma("sync", f32s[i][:], peer_wq[c * 128:(c + 1) * 128, half * 1024:(half + 1) * 1024],
                      w=[f"ws:f32s{i}"], sem=f"f32s{i}")
                V("tensor_scalar", [f"ws:f32s{i}", "gfc"], [f"ws:b16s{i}"], out=b16s[i][:], in0=f32s[i][:],
                  scalar1=gfc[:, c:c + 1], scalar2=None, op0=ALU.mult)
                P.dma("gpsimd", wqb_d[half * 8:(half + 1) * 8, :, c, :].rearrange("h p n -> p h n"),
                      b16s[i][:].rearrange("p (h n) -> p h n", h=8), r=[f"ws:b16s{i}"], w=["wqb_d"],
                      sem=f"b16w{i}")
        if stop_here('p0a', gfin[:], ['gfin','wqb_d'] + [f'keysT{i}' for i in range(16)]):
            return nc
        n_et = 128
        for et in range(n_et):
            i = ring(4)
            P.dma("sync", f32s[i][:], peer_wu[et * 128:(et + 1) * 128, :], w=[f"ws:f32s{i}"], sem=f"f32s{i}")
            A([f"ws:f32s{i}"], [f"ws:b16s{i}"], out=b16s[i][:], in_=f32s[i][:], func=AF.Copy)
            pbk = 6 + (et % 2)
            for c in range(8):
                TR([f"ws:b16s{i}", "identb"], [f"ws:pb{pbk}"], out=pbb[pbk][:, c * 128:(c + 1) * 128],
                   in_=b16s[i][:, c * 128:(c + 1) * 128], identity=identb[:])
            j = et % 2
            V("tensor_tensor", [f"ws:pb{pbk}", "gfcb"], [f"ws:b16t{j}"], out=b16t[j][:],
              in0=pbb[pbk][:].rearrange("p (c e) -> p c e", c=8), in1=gfcb[:].unsqueeze(2).to_broadcast([128, 8, 128]),
              op=ALU.mult)
            P.dma("gpsimd", wuT_d[et], b16t[j][:].rearrange("p c e -> p (c e)"), r=[f"ws:b16t{j}"], w=[f"wuT{et}"],
                  sem=f"b16tw{j}")
            i = ring(4)
            P.dma("sync", f32s[i][:], peer_wv[et * 128:(et + 1) * 128, :], w=[f"ws:f32s{i}"], sem=f"f32s{i}")
            V("tensor_copy", [f"ws:f32s{i}"], [f"ws:b16s{i}"], out=b16s[i][:], in_=f32s[i][:])
            P.dma("gpsimd", wvb_d[et], b16s[i][:], r=[f"ws:b16s{i}"], w=[f"wvb{et}"], sem=f"b16w{i}")

        if stop_here('p0', gfin[:], ['gfin'] + [f'wuT{i}' for i in range(128)] + [f'wvb{i}' for i in range(128)]):
            return nc
        P.fence()
        cv = Carver(ws[:])
        winT = cv.take([8, 2560], BF16)
        woutT = cv.take([8, 1024], BF16)
        hT = cv.take([8, S], BF16)
        qkT = [[cv.take([S], BF16) for _ in range(2)] for _ in range(2)]
        Vaug = cv.take([NT, 4, 130], BF16)
        mix = cv.take([NT, 1024], BF16)
        xin = [cv.take([1024], F32) for _ in range(2)]
        hb = [cv.take([1024], BF16) for _ in range(2)]
        ug = [cv.take([512], F32) for _ in range(2)]
        gvg = [cv.take([512], F32) for _ in range(2)]
        gvn = [cv.take([512], BF16) for _ in range(2)]
        t5a = [cv.take([512], F32) for _ in range(2)]
        pT = [cv.take([128], BF16) for _ in range(4)]
        dtmp = [cv.take([128], F32) for _ in range(2)]
        osb = [cv.take([128], F32) for _ in range(2)]
        osq = [cv.take([128], BF16) for _ in range(2)]
        mixT = [cv.take([8, 128], BF16) for _ in range(2)]
        print("phase1 ws used", cv.off, "of", cv.cap)

        ri[0] = 0
        for c in range(8):
            for (a, b) in ((0, 1024), (1024, 2048), (2048, 2560)):
                i = ring(2)
                P.dma("sync", xin[i][:, 0:b - a], w_in[c * 128:(c + 1) * 128, a:b], w=[f"ws:xin{i}"], sem=f"xin{i}")
                V("tensor_scalar", [f"ws:xin{i}", "gmc"], [f"ws:winT{c}"], out=winT[:, c, a:b], in0=xin[i][:, 0:b - a],
                  scalar1=gmc[:, c:c + 1], scalar2=None, op0=ALU.mult)
        for c in range(8):
            i = ring(2)
            P.dma("sync", xin[i][:], w_out[c * 128:(c + 1) * 128, :], w=[f"ws:xin{i}"], sem=f"xin{i}")
            A([f"ws:xin{i}"], [f"ws:woutT{c}"], out=woutT[:, c, :], in_=xin[i][:], func=AF.Copy)
        winK = [f"ws:winT{c}" for c in range(8)]
        woutK = [f"ws:woutT{c}" for c in range(8)]
        V("memset", [], ["ws:vones"], ap=Vaug[:, :, :, 128:130], constant=1.0)

        for seq in range(nseq):
            r0 = seq * S
            for tt in range(NT):
                i = tt % 2
                P.dma("sync", xin[i][:], x[r0 + tt * 128:r0 + (tt + 1) * 128, :], w=[f"ws:xin{i}"], sem=f"xin{i}")
                ss, ssk = stcol()
                A([f"ws:xin{i}"], [f"ws:hb{i}", ssk], out=hb[i][:], in_=xin[i][:], func=AF.Square, accum_out=ss)
                rs, rsk = stcol()
                rstd_from_ss(ss, ssk, 1, 1.0 / D, rs, rsk)
                V("tensor_scalar", [f"ws:xin{i}", rsk], [f"ws:hb{i}"], out=hb[i][:], in0=xin[i][:], scalar1=rs,
                  scalar2=None, op0=ALU.mult)
                pbk = 6 + i
                for c in range(8):
                    TR([f"ws:hb{i}", "identb"], [f"ws:pb{pbk}"], out=pbb[pbk][:, c * 128:(c + 1) * 128],
                       in_=hb[i][:, c * 128:(c + 1) * 128], identity=identb[:])
                A([f"ws:pb{pbk}"], [f"ws:hT{tt}"], out=hT[:, :, tt * 128:(tt + 1) * 128],
                  in_=pbb[pbk][:].rearrange("p (c t) -> p c t", c=8), func=AF.Copy)
            for tt in range(NT):
                i = tt % 2
                for n in range(3):
                    for c in range(8):
                        MM([f"ws:hT{tt}", winK[c]], [f"ws:pb{n}"], out=pb[n][:, :], lhsT=hT[:, c, tt * 128:(tt + 1) * 128],
                           rhs=winT[:, c, 1024 + n * 512:1024 + (n + 1) * 512], start=(c == 0), stop=(c == 7))
                V("tensor_copy", ["ws:pb0"], [f"ws:V{tt}"], out=Vaug[:, tt, :, 0:128],
                  in_=pb[0][:, :].rearrange("p (h e) -> p h e", h=4))
                A(["ws:pb1"], [f"ws:ug{i}"], out=ug[i][:], in_=pb[1][:, :], func=AF.Gelu_apprx_tanh)
                A(["ws:pb2"], [f"ws:gvg{i}"], out=gvg[i][:], in_=pb[2][:, :], func=AF.Gelu_apprx_tanh)
                V("tensor_tensor", [f"ws:gvg{i}"], [f"ws:t5a{i}"], out=t5a[i][:], in0=gvg[i][:], in1=gvg[i][:], op=ALU.mult)
                s4, s4k = stcol(4)
                V("tensor_reduce", [f"ws:t5a{i}"], [s4k], out=s4, in_=t5a[i][:].rearrange("p (g c) -> p g c", g=4),
                  axis=AX.X, op=ALU.add)
                r4, r4k = stcol(4)
                rstd_from_ss(s4, s4k, 4, 1.0 / 128, r4, r4k)
                V("tensor_tensor", [f"ws:gvg{i}", r4k], [f"ws:t5a{i}"], out=t5a[i][:].rearrange("p (g c) -> p g c", g=4),
                  in0=gvg[i][:].rearrange("p (g c) -> p g c", g=4), in1=r4.unsqueeze(2).to_broadcast([128, 4, 128]),
                  op=ALU.mult)
                V("tensor_tensor", [f"ws:t5a{i}", "ggv"], [f"ws:gvn{i}"], out=gvn[i][:], in0=t5a[i][:], in1=ggv[:], op=ALU.mult)
                for g in range(4):
                    MM([f"ws:gvn{i}", f"wsT{g}"], ["ws:pb3"], out=pb[3][:, g * 128:(g + 1) * 128], lhsT=wsT[:, g, :],
                       rhs=gvn[i][:, g * 128:(g + 1) * 128], start=True, stop=True)
                for g in range(4):
                    V("scalar_tensor_tensor", ["ws:pb3", "bsT", f"ws:ug{i}"], [f"ws:t5a{i}"],
                      out=t5a[i][:, g * 128:(g + 1) * 128], in0=pb[3][:, g * 128:(g + 1) * 128], scalar=bsT[:, g:g + 1],
                      in1=ug[i][:, g * 128:(g + 1) * 128], op0=ALU.add, op1=ALU.mult)
                V("tensor_tensor", [f"ws:t5a{i}"], [f"ws:gvg{i}"], out=gvg[i][:], in0=t5a[i][:], in1=t5a[i][:], op=ALU.mult)
                s4, s4k = stcol(4)
                V("tensor_reduce", [f"ws:gvg{i}"], [s4k], out=s4, in_=gvg[i][:].rearrange("p (g c) -> p g c", g=4),
                  axis=AX.X, op=ALU.add)
                r4, r4k = stcol(4)
                rstd_from_ss(s4, s4k, 4, 1.0 / 128, r4, r4k)
                V("tensor_tensor", [f"ws:t5a{i}", r4k], [f"ws:gvg{i}"], out=gvg[i][:].rearrange("p (g c) -> p g c", g=4),
                  in0=t5a[i][:].rearrange("p (g c) -> p g c", g=4), in1=r4.unsqueeze(2).to_broadcast([128, 4, 128]),
                  op=ALU.mult)
                V("tensor_tensor", [f"ws:gvg{i}", "ggo"], [f"ws:mixg{tt}"], out=mix[:, tt, 512:1024], in0=gvg[i][:],
                  in1=ggo[:], op=ALU.mult)
            pair = 0
            cntm = [0, 0]
            for h in range(4):
                sl = h % 2
                for which in range(2):
                    for tg in range(4):
                        pbk = 6 + (tg % 2)
                        for c in range(8):
                            MM([f"ws:hT{t}" for t in range(tg * 4, tg * 4 + 4)] + [winK[c]], [f"ws:pb{pbk}"],
                               out=pb[pbk][:, :], lhsT=winT[:, c, which * 512 + h * 128:which * 512 + (h + 1) * 128],
                               rhs=hT[:, c, tg * 512:(tg + 1) * 512], start=(c == 0), stop=(c == 7))
                        if tg % 2 == 0:
                            A([f"ws:pb{pbk}"], [f"ws:qk{sl}{which}_{tg}"], out=qkT[sl][which][:, tg * 512:(tg + 1) * 512],
                              in_=pb[pbk][:, :], func=AF.Copy)
                        else:
                            V("tensor_copy", [f"ws:pb{pbk}"], [f"ws:qk{sl}{which}_{tg}"],
                              out=qkT[sl][which][:, tg * 512:(tg + 1) * 512], in_=pb[pbk][:, :])
                qT_, kT_ = qkT[sl][0], qkT[sl][1]
                pairs = [(qt, m, j) for qt in range(NT) for m in range(2) for j in range(qt + 1)]
                LA = 4
                pbase = pair
                assign = []
                for (qt_, m_, j_) in pairs:
                    c_ = cntm[m_]
                    cntm[m_] += 1
                    assign.append((((0, 6), (1, 7))[m_][(c_ // 4) % 2], c_ % 4))
                pair += len(pairs)

                def rec_S(i2_, h=h, sl=sl, qT_=qT_, kT_=kT_, pbase=pbase, pairs=pairs, assign=assign):
                    qt, m, j = pairs[i2_]
                    sbank, sslot = assign[i2_]
                    sk = f"ws:pb{sbank}"
                    sview = pb[sbank][:, sslot * 128:(sslot + 1) * 128]
                    MM([f"ws:qk{sl}0_{qt // 4}", f"ws:qk{sl}1_{j // 4}"], [sk], out=sview,
                       lhsT=kT_[m * 64:(m + 1) * 64, j * 128:(j + 1) * 128],
                       rhs=qT_[m * 64:(m + 1) * 64, qt * 128:(qt + 1) * 128], start=True, stop=True)

                def rec_rest(i2_, h=h, pbase=pbase, pairs=pairs, assign=assign):
                    qt, m, j = pairs[i2_]
                    sbank, sslot = assign[i2_]
                    sk = f"ws:pb{sbank}"
                    sview = pb[sbank][:, sslot * 128:(sslot + 1) * 128]
                    ob = 2 + (qt % 2) * 2 + m
                    ps_ = (pbase + i2_) % 4
                    if j < qt:
                        A([sk, f"bcol{h}"], [f"ws:pT{ps_}"], out=pT[ps_][:], in_=sview, func=AF.Exp,
                          bias=bcol[:, h, qt - j:qt - j + 1], scale=0.125)
                    else:
                        dsl = m
                        V("scalar_tensor_tensor", [sk, f"MdT{h}"], [f"ws:dtmp{dsl}"], out=dtmp[dsl][:],
                          in0=sview, scalar=0.125, in1=MdT[:, h, :], op0=ALU.mult, op1=ALU.add)
                        A([f"ws:dtmp{dsl}"], [f"ws:pT{ps_}"], out=pT[ps_][:], in_=dtmp[dsl][:], func=AF.Exp)
                    MM([f"ws:pT{ps_}", f"ws:V{j}", "ws:vones"], [f"ws:pb{ob}"], out=pb[ob][:, 0:129],
                       lhsT=pT[ps_][:], rhs=Vaug[:, j, h, 0:129], start=(j == 0), stop=(j == qt))
                    if not (m == 1 and j == qt):
                        return
                    o1, o2 = 2 + (qt % 2) * 2, 2 + (qt % 2) * 2 + 1
                    osl = qt % 2
                    rz, rzk = stcol(2)
                    V("reciprocal", [f"ws:pb{o1}"], [rzk], out=rz[:, 0:1], in_=pb[o1][:, 128:129])
                    rz2, rz2k = stcol(2)
                    V("reciprocal", [f"ws:pb{o2}"], [rz2k], out=rz2[:, 0:1], in_=pb[o2][:, 128:129])
                    V("tensor_tensor", [rz2k, "neglam"], [rz2k], out=rz2[:, 1:2], in0=rz2[:, 0:1], in1=neglam,
                      op=ALU.mult)
                    V("tensor_scalar", [f"ws:pb{o1}", rzk], [f"ws:osb{osl}"], out=osb[osl][:], in0=pb[o1][:, 0:128],
                      scalar1=rz[:, 0:1], scalar2=None, op0=ALU.mult)
                    V("scalar_tensor_tensor", [f"ws:pb{o2}", rz2k, f"ws:osb{osl}"], [f"ws:osb{osl}"],
                      out=osb[osl][:], in0=pb[o2][:, 0:128], scalar=rz2[:, 1:2], in1=osb[osl][:], op0=ALU.mult,
                      op1=ALU.add)
                    sso, ssok = stcol()
                    A([f"ws:osb{osl}"], [f"ws:osq{osl}", ssok], out=osq[osl][:], in_=osb[osl][:], func=AF.Square,
                      accum_out=sso)
                    ro, rok = stcol()
                    rstd_from_ss(sso, ssok, 1, 1.0 / 128, ro, rok)
                    V("scalar_tensor_tensor", [f"ws:osb{osl}", rok, "gsub"], [f"ws:mixa{qt}_{h}"],
                      out=mix[:, qt, h * 128:(h + 1) * 128], in0=osb[osl][:], scalar=ro, in1=gsub[:], op0=ALU.mult,
                      op1=ALU.mult)

                for i2_ in range(min(LA, len(pairs))):
                    rec_S(i2_)
                for i2_ in range(len(pairs)):
                    if i2_ + LA < len(pairs):
                        rec_S(i2_ + LA)
                    rec_rest(i2_)
            for tt in range(NT):
                i = tt % 2
                pbk = 6 + i
                mk = [f"ws:mixa{tt}_{h}" for h in range(4)] + [f"ws:mixg{tt}"]
                for c in range(8):
                    TR(mk + ["identb"], [f"ws:pb{pbk}"], out=pbb[pbk][:, c * 128:(c + 1) * 128],
                       in_=mix[:, tt, c * 128:(c + 1) * 128], identity=identb[:])
                A([f"ws:pb{pbk}"], [f"ws:mixT{i}"], out=mixT[i][:], in_=pbb[pbk][:].rearrange("p (c t) -> p c t", c=8),
                  func=AF.Copy)
                P.dma("sync", xin[i][:], x[r0 + tt * 128:r0 + (tt + 1) * 128, :], w=[f"ws:xin{i}"], sem=f"xin{i}")
                for n in range(2):
                    for c in range(8):
                        MM([f"ws:mixT{i}", woutK[c]], [f"ws:pb{n}"], out=pb[n][:, :], lhsT=mixT[i][:, c, :],
                           rhs=woutT[:, c, n * 512:(n + 1) * 512], start=(c == 0), stop=(c == 7))
                for n in range(2):
                    V("tensor_tensor", [f"ws:pb{n}", f"ws:xin{i}"], [f"ws:xin{i}"], out=xin[i][:, n * 512:(n + 1) * 512],
                      in0=pb[n][:, :], in1=xin[i][:, n * 512:(n + 1) * 512], op=ALU.add)
                P.dma("gpsimd", x1s[r0 + tt * 128:r0 + (tt + 1) * 128, :], xin[i][:], r=[f"ws:xin{i}"],
                      w=[f"x1s{seq}_{tt}"], sem=f"x1w{i}")

        if stop_after == 'p1':
            o = P.dma("sync", out[0:S, :], x1s[0:S, :], r=[f'x1s0_{i}' for i in range(16)], sem="stopout")
            P.emit(final_wait_ops=[o])
            return nc
        P.fence()
        cv = Carver(ws[:])
        AT = cv.take([256, 128], BF16)
        xnT = [cv.take([8, 256], BF16) for _ in range(2)]
        qT2 = cv.take([16, 256], BF16)
        x1t = [[cv.take([1024], F32) for _ in range(2)] for _ in range(2)]
        xnb = [cv.take([1024], BF16) for _ in range(2)]
        wqt = [cv.take([8, 128], BF16) for _ in range(2)]
        scores = cv.take([16, 128], F32)
        vals = cv.take([16, 16], F32)
        idxu = cv.take([16, 16], U32)
        idxf = cv.take([16, 16], F32)
        cand = cv.take([8, 256], F32)
        best = cv.take([8, 16], F32)
        fidx = cv.take([8, 16], U32)
        r1u = cv.take([8, 16], U32)
        r2u = cv.take([8, 16], U32)
        r1f = cv.take([8, 16], F32)
        r2f = cv.take([8, 16], F32)
        oht = cv.take([4, 16, 16], F32)
        oht2 = cv.take([4, 16, 16], F32)
        i1f = cv.take([8, 16], F32)
        i2f = cv.take([8, 16], F32)
        gf = cv.take([8, 16], F32)
        ex = cv.take([8, 16], F32)
        i1T = [cv.take([256], BF16) for _ in range(2)]
        i2T = [cv.take([256], BF16) for _ in range(2)]
        gT = [cv.take([256], BF16) for _ in range(2)]
        TGK = 16
        OH1 = [cv.take([TGK, 128], BF16) for _ in range(2)]
        E1w = [cv.take([TGK, 128], BF16) for _ in range(2)]
        OH2 = [cv.take([TGK, 128], BF16) for _ in range(2)]
        wu = [cv.take([8, 128], BF16) for _ in range(4)]
        wv = [cv.take([1024], BF16) for _ in range(4)]
        gl = [cv.take([256], BF16) for _ in range(2)]
        GT = [cv.take([256], BF16) for _ in range(2)]
        outsb = cv.take([1024], F32)
        print("phase2 ws used", cv.off, "of", cv.cap)

        out_ops = []
        nblk = NTOK // 256

        def prep_gen(b):
            p = b % 2
            R0 = b * 256
            seq = R0 // S
            for tt in range(2):
                gtt = (R0 % S) // 128 + tt
                xk = f"ws:x1t{p}{tt}"
                P.dma("sync", x1t[p][tt][:], x1s[R0 + tt * 128:R0 + (tt + 1) * 128, :], r=[f"x1s{seq}_{gtt}"],
                      w=[xk], sem=f"x1t{p}{tt}")
                ss, ssk = stcol()
                A([xk], [f"ws:xnb{tt}", ssk], out=xnb[tt][:], in_=x1t[p][tt][:], func=AF.Square, accum_out=ss)
                rs, rsk = stcol()
                rstd_from_ss(ss, ssk, 1, 1.0 / D, rs, rsk)
                V("tensor_scalar", [xk, rsk], [f"ws:xnb{tt}"], out=xnb[tt][:], in0=x1t[p][tt][:], scalar1=rs,
                  scalar2=None, op0=ALU.mult)
                yield
                pbk = 6 + tt
                for c in range(8):
                    TR([f"ws:xnb{tt}", "identb"], [f"ws:pb{pbk}"], out=pbb[pbk][:, c * 128:(c + 1) * 128],
                       in_=xnb[tt][:, c * 128:(c + 1) * 128], identity=identb[:])
                A([f"ws:pb{pbk}"], [f"ws:xnT{p}{tt}"], out=xnT[p][:, :, tt * 128:(tt + 1) * 128],
                  in_=pbb[pbk][:].rearrange("p (c t) -> p c t", c=8), func=AF.Copy)
                yield
            xnK = [f"ws:xnT{p}0", f"ws:xnT{p}1"]
            for hp in range(16):
                wi = hp % 2
                P.dma("sync", wqt[wi][:], wqb_d[hp], r=["wqb_d"], w=[f"ws:wqt{wi}"], sem=f"wqt{wi}")
                hs = hp % 2
                hv = pb[6 + hs][:, 0:256]
                hk = f"ws:pb{6 + hs}"
                for c in range(8):
                    MM(xnK + [f"ws:wqt{wi}"], [hk], out=hv, lhsT=wqt[wi][:, c, :], rhs=xnT[p][:, c, :], start=(c == 0),
                       stop=(c == 7))
                if hp % 2 == 0:
                    A([hk], [f"ws:qT2_{hp}"], out=qT2[:, hp, :], in_=hv, func=AF.Copy)
                else:
                    V("tensor_copy", [hk], [f"ws:qT2_{hp}"], out=qT2[:, hp, :], in_=hv)
                yield
            for tt in range(2):
                for q4 in range(4):
                    pbk = 6 + (q4 % 2)
                    for hq in range(4):
                        hp = q4 * 4 + hq
                        MM([f"ws:qT2_{hp}", f"keysT{hp}"], [f"ws:pb{pbk}"], out=pb[pbk][:, hq * 128:(hq + 1) * 128],
                           lhsT=qT2[:, hp, tt * 128:(tt + 1) * 128], rhs=keysT[:, hp, :], start=True, stop=True)
                    A([f"ws:pb{pbk}"], [f"ws:sc{q4}"], out=scores[:, q4 * 4:(q4 + 1) * 4, :],
                      in_=pb[pbk][:, :].rearrange("p (a n) -> p a n", a=4), func=AF.Copy)
                    yield
                for hp in range(16):
                    sk = f"ws:sc{hp // 4}"
                    sc = scores[:, hp, :]
                    V("max", [sk], [f"ws:vals{hp}a"], out=vals[:, hp, 0:8], in_=sc)
                    V("max_index", [sk, f"ws:vals{hp}a"], [f"ws:idx{hp}a"], out=idxu[:, hp, 0:8], in_max=vals[:, hp, 0:8],
                      in_values=sc)
                    V("match_replace", [sk, f"ws:vals{hp}a"], [sk], out=sc, in_to_replace=vals[:, hp, 0:8], in_values=sc,
                      imm_value=-1e30)
                    V("max", [sk], [f"ws:vals{hp}b"], out=vals[:, hp, 8:16], in_=sc)
                    V("max_index", [sk, f"ws:vals{hp}b"], [f"ws:idx{hp}b"], out=idxu[:, hp, 8:16], in_max=vals[:, hp, 8:16],
                      in_values=sc)
                    yield
                valK = [f"ws:vals{hp}{ab}" for hp in range(16) for ab in "ab"]
                idxK = [f"ws:idx{hp}{ab}" for hp in range(16) for ab in "ab"]
                V("tensor_copy", idxK, ["ws:idxf"], out=idxf[:], in_=idxu[:])
                v4 = vals[:].rearrange("p (h t) k -> p h t k", t=2)
                V("tensor_tensor", valK, ["ws:cand"], out=cand[:].rearrange("p h (a b) -> p h a b", a=16),
                  in0=v4[:, :, 0, :].unsqueeze(3).to_broadcast([128, 8, 16, 16]),
                  in1=v4[:, :, 1, :].unsqueeze(2).to_broadcast([128, 8, 16, 16]), op=ALU.add)
                yield
                for h in range(8):
                    ck = f"ws:cand{h}"
                    dep = ["ws:cand"]
                    V("max", dep, [f"ws:best{h}a"], out=best[:, h, 0:8], in_=cand[:, h, :])
                    V("max_index", dep + [f"ws:best{h}a"], [f"ws:fidx{h}a"], out=fidx[:, h, 0:8], in_max=best[:, h, 0:8],
                      in_values=cand[:, h, :])
                    V("match_replace", dep + [f"ws:best{h}a"], [ck], out=cand[:, h, :], in_to_replace=best[:, h, 0:8],
                      in_values=cand[:, h, :], imm_value=-1e30)
                    V("max", [ck], [f"ws:best{h}b"], out=best[:, h, 8:16], in_=cand[:, h, :])
                    V("max_index", [ck, f"ws:best{h}b"], [f"ws:fidx{h}b"], out=fidx[:, h, 8:16], in_max=best[:, h, 8:16],
                      in_values=cand[:, h, :])
                    yield
                bestK = [f"ws:best{h}{ab}" for h in range(8) for ab in "ab"]
                fidxK = [f"ws:fidx{h}{ab}" for h in range(8) for ab in "ab"]
                V("tensor_tensor", bestK, ["ws:ex"], out=ex[:], in0=best[:],
                  in1=best[:, :, 0:1].to_broadcast([128, 8, 16]), op=ALU.subtract)
                A(["ws:ex"], ["ws:ex2"], out=ex[:], in_=ex[:], func=AF.Exp)
                zt, ztk = stat[:, 48:56], "statz"
                V("tensor_reduce", ["ws:ex2"], [ztk], out=zt, in_=ex[:], axis=AX.X, op=ALU.add)
                zr, zrk = stat[:, 56:64], "statzr"
                V("reciprocal", [ztk], [zrk], out=zr, in_=zt)
                V("tensor_tensor", ["ws:ex2", zrk], ["ws:gf"], out=gf[:], in0=ex[:],
                  in1=zr.unsqueeze(2).to_broadcast([128, 8, 16]), op=ALU.mult)
                yield
                V("tensor_single_scalar", fidxK, ["ws:r1u"], out=r1u[:], in_=fidx[:], scalar=4, op=ALU.logical_shift_right)
                V("tensor_single_scalar", fidxK, ["ws:r2u"], out=r2u[:], in_=fidx[:], scalar=15, op=ALU.bitwise_and)
                V("tensor_copy", ["ws:r1u"], ["ws:r1f"], out=r1f[:], in_=r1u[:])
                V("tensor_copy", ["ws:r2u"], ["ws:r2f"], out=r2f[:], in_=r2u[:])
                yield
                idx4 = idxf[:].rearrange("p (h t) k -> p h t k", t=2)
                io16 = iorow[:, 0:16].unsqueeze(1).unsqueeze(1).to_broadcast([128, 4, 16, 16])
                for (rf, rk, tsel, dst, dk) in ((r1f, "ws:r1f", 0, i1f, "ws:i1f"), (r2f, "ws:r2f", 1, i2f, "ws:i2f")):
                    for hh in range(2):
                        hsl = slice(hh * 4, hh * 4 + 4)
                        V("tensor_tensor", [rk, "iorow"], ["ws:oht"], out=oht[:],
                          in0=rf[:, hsl, :].unsqueeze(3).to_broadcast([128, 4, 16, 16]), in1=io16, op=ALU.is_equal)
                        V("tensor_tensor", ["ws:oht", "ws:idxf"], ["ws:oht2"], out=oht2[:], in0=oht[:],
                          in1=idx4[:, hsl, tsel, :].unsqueeze(2).to_broadcast([128, 4, 16, 16]), op=ALU.mult)
                        V("tensor_reduce", ["ws:oht2"], [dk + str(hh)], out=dst[:, hsl, :], in_=oht2[:], axis=AX.X,
                          op=ALU.add)
                        yield
                for (src, sks, dstT, dk) in ((i1f, ["ws:i1f0", "ws:i1f1"], i1T, "ws:i1T"),
                                             (i2f, ["ws:i2f0", "ws:i2f1"], i2T, "ws:i2T"), (gf, ["ws:gf"], gT, "ws:gT")):
                    TR(sks + ["identf"], ["ws:pb7"], out=pb[7][:, 0:128], in_=src[:].rearrange("p h k -> p (h k)"),
                       identity=identf[:])
                    V("tensor_copy", ["ws:pb7"], [f"{dk}{p}{tt}"], out=dstT[p][:, tt * 128:(tt + 1) * 128],
                      in_=pb[7][:, 0:128])
                    yield

        def build_AT(b):
            p = b % 2
            io_b = iob[:].unsqueeze(1).to_broadcast([128, TGK, 128])
            for g in range(256 // TGK):
                sl = g % 2
                tt = (g * TGK) // 128
                tsl = slice(g * TGK, (g + 1) * TGK)
                V("tensor_tensor", ["iob", f"ws:i2T{p}{tt}"], [f"ws:OH2{sl}"], out=OH2[sl][:], in0=io_b,
                  in1=i2T[p][:, tsl].unsqueeze(2).to_broadcast([128, TGK, 128]), op=ALU.is_equal)
                V("tensor_tensor", ["iob", f"ws:i1T{p}{tt}"], [f"ws:OH1{sl}"], out=OH1[sl][:], in0=io_b,
                  in1=i1T[p][:, tsl].unsqueeze(2).to_broadcast([128, TGK, 128]), op=ALU.is_equal)
                G("tensor_tensor", [f"ws:OH1{sl}", f"ws:gT{p}{tt}"], [f"ws:E1w{sl}"], out=E1w[sl][:], in0=OH1[sl][:],
                  in1=gT[p][:, tsl].unsqueeze(2).to_broadcast([128, TGK, 128]), op=ALU.mult)
                for k4 in range(TGK // 4):
                    pbk = 6 + (k4 % 2)
                    for kk in range(4):
                        k = k4 * 4 + kk
                        MM([f"ws:OH2{sl}", f"ws:E1w{sl}"], [f"ws:pb{pbk}"], out=pb[pbk][:, kk * 128:(kk + 1) * 128],
                           lhsT=OH2[sl][:, k, :], rhs=E1w[sl][:, k, :], start=True, stop=True)
                    t0 = g * TGK + k4 * 4
                    A([f"ws:pb{pbk}"], ["ws:AT"], out=AT[:, t0:t0 + 4, :],
                      in_=pb[pbk][:, :].rearrange("p (t i) -> p t i", t=4), func=AF.Copy)

        def sweep(b, gen):
            p = b % 2
            xnK = [f"ws:xnT{p}0", f"ws:xnT{p}1"]

            def rec_H(et):
                gi = b * 128 + et
                wi = gi % 4
                P.dma("sync", wu[wi][:].rearrange("p c e -> p (c e)"), wuT_d[et], r=[f"wuT{et}"], w=[f"ws:wu{wi}"],
                      sem=f"wu{wi}")
                P.dma("sync", wv[wi][:], wvb_d[et], r=[f"wvb{et}"], w=[f"ws:wv{wi}"], sem=f"wv{wi}")
                hs = gi % 2
                for c in range(8):
                    MM(xnK + [f"ws:wu{wi}"], [f"ws:pb{4 + hs}"], out=pb[4 + hs][:, 0:256], lhsT=wu[wi][:, c, :],
                       rhs=xnT[p][:, c, :], start=(c == 0), stop=(c == 7))

            def rec_rest(et):
                gi = b * 128 + et
                wi = gi % 4
                hs = gi % 2
                gs = gi % 2
                A([f"ws:pb{4 + hs}"], [f"ws:gl{gs}"], out=gl[gs][:], in_=pb[4 + hs][:, 0:256], func=AF.Gelu_apprx_tanh)
                V("tensor_tensor", [f"ws:gl{gs}", "ws:AT"], [f"ws:GT{gs}"], out=GT[gs][:], in0=gl[gs][:],
                  in1=AT[:, :, et], op=ALU.mult)
                for tt in range(2):
                    for dh in range(2):
                        MM([f"ws:GT{gs}", f"ws:wv{wi}"], [f"ws:pb{tt * 2 + dh}"], out=pb[tt * 2 + dh][:, :],
                           lhsT=GT[gs][:, tt * 128:(tt + 1) * 128], rhs=wv[wi][:, dh * 512:(dh + 1) * 512],
                           start=(et == 0), stop=(et == 127))

            rec_H(0)
            for et in range(128):
                if et + 1 < 128:
                    rec_H(et + 1)
                rec_rest(et)
                if gen is not None and et >= 2:
                    next(gen, None)
            if gen is not None:
                for _ in gen:
                    pass

        def finish(b):
            p = b % 2
            R0 = b * 256
            for tt in range(2):
                xk = f"ws:x1t{p}{tt}"
                for dh in range(2):
                    V("tensor_tensor", [f"ws:pb{tt * 2 + dh}", xk], [xk],
                      out=x1t[p][tt][:, dh * 512:(dh + 1) * 512], in0=pb[tt * 2 + dh][:, :],
                      in1=x1t[p][tt][:, dh * 512:(dh + 1) * 512], op=ALU.add)
                ss, ssk = stcol()
                A([xk], ["ws:outsb", ssk], out=outsb[:], in_=x1t[p][tt][:], func=AF.Square, accum_out=ss)
                rs, rsk = stcol()
                rstd_from_ss(ss, ssk, 1, 1.0 / D, rs, rsk)
                V("scalar_tensor_tensor", [xk, rsk, "gfin"], ["ws:outsb"], out=outsb[:],
                  in0=x1t[p][tt][:], scalar=rs, in1=gfin[:], op0=ALU.mult, op1=ALU.mult)
                out_ops.append(P.dma("gpsimd", out[R0 + tt * 128:R0 + (tt + 1) * 128, :], outsb[:],
                                     r=["ws:outsb"], sem="outw"))

        for _ in prep_gen(0):
            pass
        for b in range(nblk):
            build_AT(b)
            sweep(b, prep_gen(b + 1) if b + 1 < nblk else None)
            finish(b)
        print("n ops", len(P.ops))
        P.emit(final_wait_ops=out_ops)
    return nc


def kernel(x, w_in, lam_q1, lam_k1, lam_q2, lam_k2, g_subln, w_s, b_s, g_gv, g_gout, w_out, g_mix, g_ffn,
           peer_wq, peer_keys, peer_wu, peer_wv, g_final):
    f = lambda a: np.ascontiguousarray(np.asarray(a, dtype=np.float32))
    x = f(x)
    B = x.shape[0]
    nseq = B // NCORES
    nc = build_nc(nseq)
    common = {
        "w_in": f(w_in[0]),
        "lamv": f(np.stack([np.asarray(lam_q1)[0], np.asarray(lam_k1)[0], np.asarray(lam_q2)[0], np.asarray(lam_k2)[0]])),
        "g_subln": f(g_subln[0]), "w_s": f(w_s[0]), "b_s": f(b_s[0]), "g_gv": f(g_gv[0]), "g_gout": f(g_gout[0]),
        "w_out": f(w_out[0]), "g_mix": f(g_mix[0]), "g_ffn": f(g_ffn[0]), "peer_wq": f(peer_wq[0]),
        "peer_keys": f(np.asarray(peer_keys)[0].reshape(16, 128, 128)), "peer_wu": f(peer_wu[0]),
        "peer_wv": f(peer_wv[0]), "g_final": f(g_final),
    }
    in_maps = []
    for c in range(NCORES):
        m = dict(common)
        m["x"] = x[c * nseq:(c + 1) * nseq].reshape(nseq * S, D)
        in_maps.append(m)
    res = run_bass_kernel_spmd(nc, in_maps, core_ids=list(range(NCORES)))
    outs = [np.asarray(r["out"]).reshape(nseq, S, D) for r in res.results]
    return np.concatenate(outs, axis=0).astype(np.float32)
```

```python
import numpy as np
from contextlib import ExitStack
import concourse.bass as bass
import concourse.mybir as mybir
from concourse.bass_utils import run_bass_kernel_spmd

F32 = mybir.dt.float32
BF16 = mybir.dt.bfloat16
U32 = mybir.dt.uint32
AF = mybir.ActivationFunctionType
ALU = mybir.AluOpType
AX = mybir.AxisListType

ENGS = ["sync", "scalar", "vector", "gpsimd", "tensor"]
EPS = 1e-6
NCORES = 8
D = 1024
S = 2048
NT = S // 128
NEG = -30000.0


class Prog:
    def __init__(self, nc):
        self.nc = nc
        self.ops = []
        self.last_w = {}
        self.readers = {}
        self.dma_sem_count = {}
        self.epoch = 0
        self.key_epoch = {}
        self.fence_ops = []
        self.last_eng_op = {}
        self.last_dma_op = {}

    def fence(self):
        self.epoch += 1
        self.fence_ops = list(self.last_eng_op.values()) + list(self.last_dma_op.values())

    def _add(self, eng, fn, r, w, dma_sem=None):
        idx = len(self.ops)
        pk = [k for k in r if k.startswith("ws:pb")]
        if pk:
            r = [k for k in r if not k.startswith("ws:pb")]
            w = list(w) + pk
        deps = set()
        for k in list(r) + list(w):
            if k.startswith("ws:") and self.key_epoch.get(k) != self.epoch:
                self.key_epoch[k] = self.epoch
                self.last_w.pop(k, None)
                self.readers.pop(k, None)
                deps.update(self.fence_ops)
        raw = set()
        for k in r:
            lw = self.last_w.get(k)
            if lw is not None:
                deps.add(lw)
                raw.add(lw)
        for k in w:
            lw = self.last_w.get(k)
            if lw is not None:
                deps.add(lw)
            deps.update(self.readers.get(k, ()))
        real = set()
        for d in deps:
            od = self.ops[d]
            if od["dma_sem"] is None and od["eng"] == eng and d not in raw:
                continue
            real.add(d)
        op = dict(eng=eng, fn=fn, deps=real, dma_sem=dma_sem, idx=idx)
        if dma_sem is not None:
            c = self.dma_sem_count.get(dma_sem, 0) + 16
            self.dma_sem_count[dma_sem] = c
            op["val"] = c
            self.last_dma_op[dma_sem] = idx
        else:
            self.last_eng_op[eng] = idx
        self.ops.append(op)
        for k in r:
            self.readers.setdefault(k, []).append(idx)
        for k in w:
            self.last_w[k] = idx
            self.readers[k] = []
        return idx

    def op(self, eng, fn, r=(), w=()):
        return self._add(eng, fn, r, w)

    def dma(self, eng, out, in_, r=(), w=(), sem="dma", **kw):
        def fn(e, out=out, in_=in_, kw=kw):
            return e.dma_start(out=out, in_=in_, **kw)
        return self._add(eng, fn, r, w, dma_sem=sem)

    def emit(self, final_wait_ops=()):
        nc = self.nc
        ops = self.ops
        for o in ops:
            newest = {}
            for d in o["deps"]:
                od = ops[d]
                q = ("dma", od["dma_sem"]) if od["dma_sem"] is not None else ("eng", od["eng"])
                if q not in newest or d > newest[q]:
                    newest[q] = d
            o["deps"] = set(newest.values())
        needs_inc = [False] * len(ops)
        for o in ops:
            for d in o["deps"]:
                needs_inc[d] = True
        for d in final_wait_ops:
            needs_inc[d] = True
        SEGN = 30000
        cnt = {e: 0 for e in ENGS}
        semnames = []
        for o in ops:
            if o["dma_sem"] is None:
                if needs_inc[o["idx"]]:
                    c = cnt[o["eng"]]
                    cnt[o["eng"]] += 1
                    o["sem"] = "eng_%s_%d" % (o["eng"], c // SEGN)
                    o["val"] = c % SEGN + 1
                    if o["sem"] not in semnames:
                        semnames.append(o["sem"])
                else:
                    o["sem"] = None
                    o["val"] = None
            else:
                o["sem"] = "dma_" + str(o["dma_sem"])
        semnames += ["dma_" + str(k) for k in self.dma_sem_count]
        with ExitStack() as st:
            sems = {n: st.enter_context(nc.semaphore(n)) for n in semnames}
            block = st.enter_context(nc.Block())
            per_eng = {e: [o for o in ops if o["eng"] == e] for e in ENGS}

            def make(ename):
                def body(eng):
                    waited = {}
                    for o in per_eng[ename]:
                        for d in sorted(o["deps"]):
                            od = ops[d]
                            s, v = od["sem"], od["val"]
                            if waited.get(s, 0) >= v:
                                continue
                            eng.wait_ge(sems[s], v)
                            waited[s] = v
                        ins = o["fn"](eng)
                        if o["dma_sem"] is not None:
                            ins.then_inc(sems[o["sem"]], 16)
                        elif o["val"] is not None:
                            ins.then_inc(sems[o["sem"]], 1)
                    if ename == "sync":
                        for d in final_wait_ops:
                            od = ops[d]
                            if waited.get(od["sem"], 0) >= od["val"]:
                                continue
                            eng.wait_ge(sems[od["sem"]], od["val"])
                            waited[od["sem"]] = od["val"]
                return body

            block.sync(make("sync"))
            block.scalar(make("scalar"))
            block.vector(make("vector"))
            block.gpsimd(make("gpsimd"))
            block.tensor(make("tensor"))


class Carver:
    def __init__(self, base):
        self.base = base
        self.off = 0
        self.cap = base.shape[1]

    def take(self, shape, dtype):
        esz = 2 if dtype == BF16 else 4
        n = int(np.prod(shape)) * esz // 2
        n_al = (n + 15) // 16 * 16
        assert self.off + n_al <= self.cap, ("workspace overflow", self.off, n_al, self.cap)
        v = self.base[:, self.off:self.off + n]
        self.off += n_al
        if dtype != BF16:
            v = v.bitcast(dtype)
        if len(shape) == 2:
            v = v.rearrange("p (a b) -> p a b", a=shape[0])
        elif len(shape) == 3:
            v = v.rearrange("p (a b c) -> p a b c", a=shape[0], b=shape[1])
        return v


def build_nc(nseq, stop_after=None):
    import os
    stop_after = os.environ.get('KSTOP', stop_after)
    NTOK = nseq * S
    nc = bass.Bass("TRN2", target_bir_lowering=False)
    dt = lambda n, s, d=F32, kind="ExternalInput": nc.dram_tensor(n, s, d, kind=kind).ap()
    x = dt("x", [NTOK, D])
    w_in = dt("w_in", [D, 2560])
    lamv = dt("lamv", [4, 64])
    g_subln = dt("g_subln", [128])
    w_s = dt("w_s", [4, 128, 128])
    b_s = dt("b_s", [4, 128])
    g_gv = dt("g_gv", [512])
    g_gout = dt("g_gout", [512])
    w_out = dt("w_out", [D, D])
    g_mix = dt("g_mix", [D])
    g_ffn = dt("g_ffn", [D])
    peer_wq = dt("peer_wq", [D, 2048])
    peer_keys = dt("peer_keys", [16, 128, 128])
    peer_wu = dt("peer_wu", [16384, D])
    peer_wv = dt("peer_wv", [16384, D])
    g_final = dt("g_final", [D])
    out = dt("out", [NTOK, D], F32, "ExternalOutput")
    wuT_d = dt("wuT_d", [128, 128, 1024], BF16, "Internal")
    wvb_d = dt("wvb_d", [128, 128, 1024], BF16, "Internal")
    wqb_d = dt("wqb_d", [16, 128, 8, 128], BF16, "Internal")
    x1s = dt("x1s", [NTOK, D], F32, "Internal")

    WS_ELEMS = (212800 - 21600) // 2 // 16 * 16
    with ExitStack() as st:
        sb = lambda n, s, d: st.enter_context(nc.sbuf_tensor(n, s, d))
        ws = sb("ws", [128, WS_ELEMS], BF16)
        iotf = sb("iotf", [128, 128], F32)
        iorow = sb("iorow", [128, 128], F32)
        iob = sb("iob", [128, 128], BF16)
        identb = sb("identb", [128, 128], BF16)
        identf = sb("identf", [128, 128], F32)
        trilm = sb("trilm", [128, 128], F32)
        MdT = sb("MdT", [128, 4, 128], F32)
        bcol = sb("bcol", [128, 4, 16], F32)
        lamt = sb("lamt", [128, 4, 64], F32)
        lamw = sb("lamw", [128, 8], F32)
        ggv = sb("ggv", [128, 512], F32)
        ggo = sb("ggo", [128, 512], F32)
        gsub = sb("gsub", [128, 128], F32)
        gfin = sb("gfin", [128, 1024], F32)
        gmc = sb("gmc", [128, 8], F32)
        gfc = sb("gfc", [128, 8], F32)
        gfcb = sb("gfcb", [128, 8], BF16)
        bsT = sb("bsT", [128, 4], F32)
        wsT = sb("wsT", [128, 4, 128], BF16)
        keysT = sb("keysT", [128, 16, 128], BF16)
        epsc = sb("epsc", [128, 1], F32)
        stat = sb("stat", [128, 64], F32)
        pb = [st.enter_context(nc.psum_tensor(f"ws:pb{i}", [128, 512], F32)) for i in range(8)]
        pbb = [p[:].bitcast(BF16) for p in pb]

        P = Prog(nc)
        V = lambda name, r, w, **kw: P.op("vector", lambda e: getattr(e, name)(**kw), r, w)
        A = lambda r, w, **kw: P.op("scalar", lambda e: e.activation(**kw), r, w)
        G = lambda name, r, w, **kw: P.op("gpsimd", lambda e: getattr(e, name)(**kw), r, w)
        MM = lambda r, w, **kw: P.op("tensor", lambda e: e.matmul(**kw), r, w)
        TR = lambda r, w, **kw: P.op("tensor", lambda e: e.transpose(**kw), r, w)

        statn = [0]

        def stcol(n=1):
            c = statn[0] % 12 * 4
            statn[0] += 1
            return stat[:, c:c + n], f"stat{c}"

        def rstd_from_ss(ss_ap, ss_key, n, inv_n, out_ap, out_key):
            t1, k1 = stcol(n)
            A([ss_key, "epsc"], [k1], out=t1, in_=ss_ap, func=AF.Ln, scale=inv_n, bias=epsc[:, 0:1])
            A([k1], [out_key], out=out_ap, in_=t1, func=AF.Exp, scale=-0.5)


        def stop_here(tag, src_ap, rkeys):
            if stop_after != tag:
                return False
            o = P.dma("sync", out[0:128, 0:src_ap.shape[1]], src_ap, r=rkeys, sem="stopout")
            print("STOP at", tag, "n ops", len(P.ops))
            P.emit(final_wait_ops=[o])
            return True
        V("memset", [], ["epsc"], ap=epsc[:], constant=EPS)
        G("iota", [], ["iotf"], out=iotf[:], pattern=[[1, 128]], base=0, channel_multiplier=-1,
          allow_small_or_imprecise_dtypes=True)
        G("iota", [], ["iorow"], out=iorow[:], pattern=[[1, 128]], base=0, channel_multiplier=0,
          allow_small_or_imprecise_dtypes=True)
        V("tensor_copy", ["iorow"], ["iob"], out=iob[:], in_=iorow[:])
        V("tensor_single_scalar", ["iotf"], ["identb"], out=identb[:], in_=iotf[:], scalar=0.0, op=ALU.is_equal)
        V("tensor_single_scalar", ["iotf"], ["identf"], out=identf[:], in_=iotf[:], scalar=0.0, op=ALU.is_equal)
        V("tensor_single_scalar", ["iotf"], ["trilm"], out=trilm[:], in_=iotf[:], scalar=0.0, op=ALU.is_le)
        absd = MdT[:, 3, :]
        A(["iotf"], ["MdT3"], out=absd, in_=iotf[:], func=AF.Abs)
        V("tensor_tensor", ["MdT3", "iorow"], ["MdT3"], out=absd, in0=iorow[:], in1=absd, op=ALU.subtract)
        slopes = [2.0 ** (-2.0 * (h + 1)) for h in range(4)]
        for h in range(4):
            V("tensor_scalar", ["MdT3"], [f"MdT{h}"], out=MdT[:, h, :], in0=absd, scalar1=slopes[h], scalar2=None,
              op0=ALU.mult)
        for h in range(4):
            V("memset", [], [f"MdT{h}"], ap=MdT[64:128, h, 0:64], constant=NEG)
        G("iota", [], ["bcol3"], out=bcol[:, 3, :], pattern=[[-128, 16]], base=0, channel_multiplier=1,
          allow_small_or_imprecise_dtypes=True)
        for h in range(4):
            V("tensor_scalar", ["bcol3"], [f"bcol{h}"], out=bcol[:, h, :], in0=bcol[:, 3, :], scalar1=slopes[h],
              scalar2=None, op0=ALU.mult)
        P.dma("sync", lamt[:].rearrange("p a b -> p (a b)"), lamv.rearrange("a b -> (a b)").partition_broadcast(128),
              w=["lamt"], sem="c0")
        V("tensor_tensor", ["lamt"], ["lamp"], out=lamt[:, 0, :], in0=lamt[:, 0, :], in1=lamt[:, 1, :], op=ALU.mult)
        V("tensor_tensor", ["lamt"], ["lamp2"], out=lamt[:, 2, :], in0=lamt[:, 2, :], in1=lamt[:, 3, :], op=ALU.mult)
        V("tensor_reduce", ["lamp"], ["lw0"], out=lamw[:, 0:1], in_=lamt[:, 0, :], axis=AX.X, op=ALU.add)
        V("tensor_reduce", ["lamp2"], ["lw1"], out=lamw[:, 1:2], in_=lamt[:, 2, :], axis=AX.X, op=ALU.add)
        A(["lw0", "lw1"], ["lw23"], out=lamw[:, 2:4], in_=lamw[:, 0:2], func=AF.Exp)
        V("tensor_tensor", ["lw23"], ["lw4"], out=lamw[:, 4:5], in0=lamw[:, 3:4], in1=lamw[:, 2:3], op=ALU.subtract)
        V("tensor_scalar", ["lw4"], ["neglam"], out=lamw[:, 5:6], in0=lamw[:, 4:5], scalar1=-0.2, scalar2=None,
          op0=ALU.add)
        neglam = lamw[:, 5:6]
        P.dma("sync", ggv[:], g_gv.partition_broadcast(128), w=["ggv"], sem="c1")
        P.dma("sync", ggo[:], g_gout.partition_broadcast(128), w=["ggo"], sem="c2")
        P.dma("sync", gsub[:], g_subln.partition_broadcast(128), w=["gsubraw"], sem="c3")
        V("tensor_scalar", ["gsubraw"], ["gsub"], out=gsub[:], in0=gsub[:], scalar1=0.8, scalar2=None, op0=ALU.mult)
        P.dma("sync", gfin[:], g_final.partition_broadcast(128), w=["gfin"], sem="c4")
        P.dma("sync", gmc[:], g_mix.rearrange("(c p) -> p c", p=128), w=["gmc"], sem="c5",
              allow_slow_non_contiguous=True)
        P.dma("sync", gfc[:], g_ffn.rearrange("(c p) -> p c", p=128), w=["gfc"], sem="c6",
              allow_slow_non_contiguous=True)
        V("tensor_copy", ["gfc"], ["gfcb"], out=gfcb[:], in_=gfc[:])
        P.dma("sync", bsT[:], b_s.rearrange("g t -> t g"), w=["bsT"], sem="c7", allow_slow_non_contiguous=True)

        if stop_here('const', gfin[:], ['gfin','ggv','ggo','gsub','gmc','gfc','gfcb','bsT','neglam']):
            return nc
        cv = Carver(ws[:])
        f32s = [cv.take([1024], F32) for _ in range(4)]
        b16s = [cv.take([1024], BF16) for _ in range(4)]
        b16t = [cv.take([8, 128], BF16) for _ in range(2)]
        ri = [0]

        def ring(n):
            i = ri[0] % n
            ri[0] += 1
            return i

        for g in range(4):
            i = ring(4)
            P.dma("sync", f32s[i][:, 0:128], w_s[g], w=[f"ws:f32s{i}"], sem=f"f32s{i}")
            V("tensor_tensor", [f"ws:f32s{i}", "trilm"], [f"ws:f32s{i}"], out=f32s[i][:, 0:128], in0=f32s[i][:, 0:128],
              in1=trilm[:], op=ALU.mult)
            TR([f"ws:f32s{i}", "identf"], ["ws:pb6"], out=pb[6][:, 0:128], in_=f32s[i][:, 0:128], identity=identf[:])
            V("tensor_copy", ["ws:pb6"], [f"wsT{g}"], out=wsT[:, g, :], in_=pb[6][:, 0:128])
        for hp in range(16):
            i = ring(4)
            P.dma("sync", f32s[i][:, 0:128], peer_keys[hp], w=[f"ws:f32s{i}"], sem=f"f32s{i}")
            TR([f"ws:f32s{i}", "identf"], ["ws:pb6"], out=pb[6][:, 0:128], in_=f32s[i][:, 0:128], identity=identf[:])
            V("tensor_copy", ["ws:pb6"], [f"keysT{hp}"], out=keysT[:, hp, :], in_=pb[6][:, 0:128])
        for c in range(8):
            for half in range(2):
                i = ring(4)
                P.dma("sync", f32s[i][:], peer_wq[c * 128:(c + 1) * 128, half * 1024:(half + 1) * 1024],
                      w=[f"ws:f32s{i}"], sem=f"f32s{i}")
                V("tensor_scalar", [f"ws:f32s{i}", "gfc"], [f"ws:b16s{i}"], out=b16s[i][:], in0=f32s[i][:],
                  scalar1=gfc[:, c:c + 1], scalar2=None, op0=ALU.mult)
                P.dma("gpsimd", wqb_d[half * 8:(half + 1) * 8, :, c, :].rearrange("h p n -> p h n"),
                      b16s[i][:].rearrange("p (h n) -> p h n", h=8), r=[f"ws:b16s{i}"], w=["wqb_d"],
                      sem=f"b16w{i}")
        if stop_here('p0a', gfin[:], ['gfin','wqb_d'] + [f'keysT{i}' for i in range(16)]):
            return nc
        n_et = 128
        for et in range(n_et):
            i = ring(4)
            P.dma("sync", f32s[i][:], peer_wu[et * 128:(et + 1) * 128, :], w=[f"ws:f32s{i}"], sem=f"f32s{i}")
            A([f"ws:f32s{i}"], [f"ws:b16s{i}"], out=b16s[i][:], in_=f32s[i][:], func=AF.Copy)
            pbk = 6 + (et % 2)
            for c in range(8):
                TR([f"ws:b16s{i}", "identb"], [f"ws:pb{pbk}"], out=pbb[pbk][:, c * 128:(c + 1) * 128],
                   in_=b16s[i][:, c * 128:(c + 1) * 128], identity=identb[:])
            j = et % 2
            V("tensor_tensor", [f"ws:pb{pbk}", "gfcb"], [f"ws:b16t{j}"], out=b16t[j][:],
              in0=pbb[pbk][:].rearrange("p (c e) -> p c e", c=8), in1=gfcb[:].unsqueeze(2).to_broadcast([128, 8, 128]),
              op=ALU.mult)
            P.dma("gpsimd", wuT_d[et], b16t[j][:].rearrange("p c e -> p (c e)"), r=[f"ws:b16t{j}"], w=[f"wuT{et}"],
                  sem=f"b16tw{j}")
            i = ring(4)
            P.dma("sync", f32s[i][:], peer_wv[et * 128:(et + 1) * 128, :], w=[f"ws:f32s{i}"], sem=f"f32s{i}")
            V("tensor_copy", [f"ws:f32s{i}"], [f"ws:b16s{i}"], out=b16s[i][:], in_=f32s[i][:])
            P.dma("gpsimd", wvb_d[et], b16s[i][:], r=[f"ws:b16s{i}"], w=[f"wvb{et}"], sem=f"b16w{i}")

        if stop_here('p0', gfin[:], ['gfin'] + [f'wuT{i}' for i in range(128)] + [f'wvb{i}' for i in range(128)]):
            return nc
        P.fence()
        cv = Carver(ws[:])
        winT = cv.take([8, 2560], BF16)
        woutT = cv.take([8, 1024], BF16)
        hT = cv.take([8, S], BF16)
        qkT = [[cv.take([S], BF16) for _ in range(2)] for _ in range(2)]
        Vaug = cv.take([NT, 4, 130], BF16)
        mix = cv.take([NT, 1024], BF16)
        xin = [cv.take([1024], F32) for _ in range(2)]
        hb = [cv.take([1024], BF16) for _ in range(2)]
        ug = [cv.take([512], F32) for _ in range(2)]
        gvg = [cv.take([512], F32) for _ in range(2)]
        gvn = [cv.take([512], BF16) for _ in range(2)]
        t5a = [cv.take([512], F32) for _ in range(2)]
        pT = [cv.take([128], BF16) for _ in range(4)]
        dtmp = [cv.take([128], F32) for _ in range(2)]
        osb = [cv.take([128], F32) for _ in range(2)]
        osq = [cv.take([128], BF16) for _ in range(2)]
        mixT = [cv.take([8, 128], BF16) for _ in range(2)]
        print("phase1 ws used", cv.off, "of", cv.cap)

        ri[0] = 0
        for c in range(8):
            for (a, b) in ((0, 1024), (1024, 2048), (2048, 2560)):
                i = ring(2)
                P.dma("sync", xin[i][:, 0:b - a], w_in[c * 128:(c + 1) * 128, a:b], w=[f"ws:xin{i}"], sem=f"xin{i}")
                V("tensor_scalar", [f"ws:xin{i}", "gmc"], [f"ws:winT{c}"], out=winT[:, c, a:b], in0=xin[i][:, 0:b - a],
                  scalar1=gmc[:, c:c + 1], scalar2=None, op0=ALU.mult)
        for c in range(8):
            i = ring(2)
            P.dma("sync", xin[i][:], w_out[c * 128:(c + 1) * 128, :], w=[f"ws:xin{i}"], sem=f"xin{i}")
            A([f"ws:xin{i}"], [f"ws:woutT{c}"], out=woutT[:, c, :], in_=xin[i][:], func=AF.Copy)
        winK = [f"ws:winT{c}" for c in range(8)]
        woutK = [f"ws:woutT{c}" for c in range(8)]
        V("memset", [], ["ws:vones"], ap=Vaug[:, :, :, 128:130], constant=1.0)

        for seq in range(nseq):
            r0 = seq * S
            for tt in range(NT):
                i = tt % 2
                P.dma("sync", xin[i][:], x[r0 + tt * 128:r0 + (tt + 1) * 128, :], w=[f"ws:xin{i}"], sem=f"xin{i}")
                ss, ssk = stcol()
                A([f"ws:xin{i}"], [f"ws:hb{i}", ssk], out=hb[i][:], in_=xin[i][:], func=AF.Square, accum_out=ss)
                rs, rsk = stcol()
                rstd_from_ss(ss, ssk, 1, 1.0 / D, rs, rsk)
                V("tensor_scalar", [f"ws:xin{i}", rsk], [f"ws:hb{i}"], out=hb[i][:], in0=xin[i][:], scalar1=rs,
                  scalar2=None, op0=ALU.mult)
                pbk = 6 + i
                for c in range(8):
                    TR([f"ws:hb{i}", "identb"], [f"ws:pb{pbk}"], out=pbb[pbk][:, c * 128:(c + 1) * 128],
                       in_=hb[i][:, c * 128:(c + 1) * 128], identity=identb[:])
                A([f"ws:pb{pbk}"], [f"ws:hT{tt}"], out=hT[:, :, tt * 128:(tt + 1) * 128],
                  in_=pbb[pbk][:].rearrange("p (c t) -> p c t", c=8), func=AF.Copy)
            for tt in range(NT):
                i = tt % 2
                for n in range(3):
                    for c in range(8):
                        MM([f"ws:hT{tt}", winK[c]], [f"ws:pb{n}"], out=pb[n][:, :], lhsT=hT[:, c, tt * 128:(tt + 1) * 128],
                           rhs=winT[:, c, 1024 + n * 512:1024 + (n + 1) * 512], start=(c == 0), stop=(c == 7))
                V("tensor_copy", ["ws:pb0"], [f"ws:V{tt}"], out=Vaug[:, tt, :, 0:128],
                  in_=pb[0][:, :].rearrange("p (h e) -> p h e", h=4))
                A(["ws:pb1"], [f"ws:ug{i}"], out=ug[i][:], in_=pb[1][:, :], func=AF.Gelu_apprx_tanh)
                A(["ws:pb2"], [f"ws:gvg{i}"], out=gvg[i][:], in_=pb[2][:, :], func=AF.Gelu_apprx_tanh)
                V("tensor_tensor", [f"ws:gvg{i}"], [f"ws:t5a{i}"], out=t5a[i][:], in0=gvg[i][:], in1=gvg[i][:], op=ALU.mult)
                s4, s4k = stcol(4)
                V("tensor_reduce", [f"ws:t5a{i}"], [s4k], out=s4, in_=t5a[i][:].rearrange("p (g c) -> p g c", g=4),
                  axis=AX.X, op=ALU.add)
                r4, r4k = stcol(4)
                rstd_from_ss(s4, s4k, 4, 1.0 / 128, r4, r4k)
                V("tensor_tensor", [f"ws:gvg{i}", r4k], [f"ws:t5a{i}"], out=t5a[i][:].rearrange("p (g c) -> p g c", g=4),
                  in0=gvg[i][:].rearrange("p (g c) -> p g c", g=4), in1=r4.unsqueeze(2).to_broadcast([128, 4, 128]),
                  op=ALU.mult)
                V("tensor_tensor", [f"ws:t5a{i}", "ggv"], [f"ws:gvn{i}"], out=gvn[i][:], in0=t5a[i][:], in1=ggv[:], op=ALU.mult)
                for g in range(4):
                    MM([f"ws:gvn{i}", f"wsT{g}"], ["ws:pb3"], out=pb[3][:, g * 128:(g + 1) * 128], lhsT=wsT[:, g, :],
                       rhs=gvn[i][:, g * 128:(g + 1) * 128], start=True, stop=True)
                for g in range(4):
                    V("scalar_tensor_tensor", ["ws:pb3", "bsT", f"ws:ug{i}"], [f"ws:t5a{i}"],
                      out=t5a[i][:, g * 128:(g + 1) * 128], in0=pb[3][:, g * 128:(g + 1) * 128], scalar=bsT[:, g:g + 1],
                      in1=ug[i][:, g * 128:(g + 1) * 128], op0=ALU.add, op1=ALU.mult)
                V("tensor_tensor", [f"ws:t5a{i}"], [f"ws:gvg{i}"], out=gvg[i][:], in0=t5a[i][:], in1=t5a[i][:], op=ALU.mult)
                s4, s4k = stcol(4)
                V("tensor_reduce", [f"ws:gvg{i}"], [s4k], out=s4, in_=gvg[i][:].rearrange("p (g c) -> p g c", g=4),
                  axis=AX.X, op=ALU.add)
                r4, r4k = stcol(4)
                rstd_from_ss(s4, s4k, 4, 1.0 / 128, r4, r4k)
                V("tensor_tensor", [f"ws:t5a{i}", r4k], [f"ws:gvg{i}"], out=gvg[i][:].rearrange("p (g c) -> p g c", g=4),
                  in0=t5a[i][:].rearrange("p (g c) -> p g c", g=4), in1=r4.unsqueeze(2).to_broadcast([128, 4, 128]),
                  op=ALU.mult)
                V("tensor_tensor", [f"ws:gvg{i}", "ggo"], [f"ws:mixg{tt}"], out=mix[:, tt, 512:1024], in0=gvg[i][:],
                  in1=ggo[:], op=ALU.mult)
            pair = 0
            cntm = [0, 0]
            for h in range(4):
                sl = h % 2
                for which in range(2):
                    for tg in range(4):
                        pbk = 6 + (tg % 2)
                        for c in range(8):
                            MM([f"ws:hT{t}" for t in range(tg * 4, tg * 4 + 4)] + [winK[c]], [f"ws:pb{pbk}"],
                               out=pb[pbk][:, :], lhsT=winT[:, c, which * 512 + h * 128:which * 512 + (h + 1) * 128],
                               rhs=hT[:, c, tg * 512:(tg + 1) * 512], start=(c == 0), stop=(c == 7))
                        if tg % 2 == 0:
                            A([f"ws:pb{pbk}"], [f"ws:qk{sl}{which}_{tg}"], out=qkT[sl][which][:, tg * 512:(tg + 1) * 512],
                              in_=pb[pbk][:, :], func=AF.Copy)
                        else:
                            V("tensor_copy", [f"ws:pb{pbk}"], [f"ws:qk{sl}{which}_{tg}"],
                              out=qkT[sl][which][:, tg * 512:(tg + 1) * 512], in_=pb[pbk][:, :])
                qT_, kT_ = qkT[sl][0], qkT[sl][1]
                pairs = [(qt, m, j) for qt in range(NT) for m in range(2) for j in range(qt + 1)]
                LA = 4
                pbase = pair
                assign = []
                for (qt_, m_, j_) in pairs:
                    c_ = cntm[m_]
                    cntm[m_] += 1
                    assign.append((((0, 6), (1, 7))[m_][(c_ // 4) % 2], c_ % 4))
                pair += len(pairs)

                def rec_S(i2_, h=h, sl=sl, qT_=qT_, kT_=kT_, pbase=pbase, pairs=pairs, assign=assign):
                    qt, m, j = pairs[i2_]
                    sbank, sslot = assign[i2_]
                    sk = f"ws:pb{sbank}"
                    sview = pb[sbank][:, sslot * 128:(sslot + 1) * 128]
                    MM([f"ws:qk{sl}0_{qt // 4}", f"ws:qk{sl}1_{j // 4}"], [sk], out=sview,
                       lhsT=kT_[m * 64:(m + 1) * 64, j * 128:(j + 1) * 128],
                       rhs=qT_[m * 64:(m + 1) * 64, qt * 128:(qt + 1) * 128], start=True, stop=True)

                def rec_rest(i2_, h=h, pbase=pbase, pairs=pairs, assign=assign):
                    qt, m, j = pairs[i2_]
                    sbank, sslot = assign[i2_]
                    sk = f"ws:pb{sbank}"
                    sview = pb[sbank][:, sslot * 128:(sslot + 1) * 128]
                    ob = 2 + (qt % 2) * 2 + m
                    ps_ = (pbase + i2_) % 4
                    if j < qt:
                        A([sk, f"bcol{h}"], [f"ws:pT{ps_}"], out=pT[ps_][:], in_=sview, func=AF.Exp,
                          bias=bcol[:, h, qt - j:qt - j + 1], scale=0.125)
                    else:
                        dsl = m
                        V("scalar_tensor_tensor", [sk, f"MdT{h}"], [f"ws:dtmp{dsl}"], out=dtmp[dsl][:],
                          in0=sview, scalar=0.125, in1=MdT[:, h, :], op0=ALU.mult, op1=ALU.add)
                        A([f"ws:dtmp{dsl}"], [f"ws:pT{ps_}"], out=pT[ps_][:], in_=dtmp[dsl][:], func=AF.Exp)
                    MM([f"ws:pT{ps_}", f"ws:V{j}", "ws:vones"], [f"ws:pb{ob}"], out=pb[ob][:, 0:129],
                       lhsT=pT[ps_][:], rhs=Vaug[:, j, h, 0:129], start=(j == 0), stop=(j == qt))
                    if not (m == 1 and j == qt):
                        return
                    o1, o2 = 2 + (qt % 2) * 2, 2 + (qt % 2) * 2 + 1
                    osl = qt % 2
                    rz, rzk = stcol(2)
                    V("reciprocal", [f"ws:pb{o1}"], [rzk], out=rz[:, 0:1], in_=pb[o1][:, 128:129])
                    rz2, rz2k = stcol(2)
                    V("reciprocal", [f"ws:pb{o2}"], [rz2k], out=rz2[:, 0:1], in_=pb[o2][:, 128:129])
                    V("tensor_tensor", [rz2k, "neglam"], [rz2k], out=rz2[:, 1:2], in0=rz2[:, 0:1], in1=neglam,
                      op=ALU.mult)
                    V("tensor_scalar", [f"ws:pb{o1}", rzk], [f"ws:osb{osl}"], out=osb[osl][:], in0=pb[o1][:, 0:128],
                      scalar1=rz[:, 0:1], scalar2=None, op0=ALU.mult)
                    V("scalar_tensor_tensor", [f"ws:pb{o2}", rz2k, f"ws:osb{osl}"], [f"ws:osb{osl}"],
                      out=osb[osl][:], in0=pb[o2][:, 0:128], scalar=rz2[:, 1:2], in1=osb[osl][:], op0=ALU.mult,
                      op1=ALU.add)
                    sso, ssok = stcol()
                    A([f"ws:osb{osl}"], [f"ws:osq{osl}", ssok], out=osq[osl][:], in_=osb[osl][:], func=AF.Square,
                      accum_out=sso)
                    ro, rok = stcol()
                    rstd_from_ss(sso, ssok, 1, 1.0 / 128, ro, rok)
                    V("scalar_tensor_tensor", [f"ws:osb{osl}", rok, "gsub"], [f"ws:mixa{qt}_{h}"],
                      out=mix[:, qt, h * 128:(h + 1) * 128], in0=osb[osl][:], scalar=ro, in1=gsub[:], op0=ALU.mult,
                      op1=ALU.mult)

                for i2_ in range(min(LA, len(pairs))):
                    rec_S(i2_)
                for i2_ in range(len(pairs)):
                    if i2_ + LA < len(pairs):
                        rec_S(i2_ + LA)
                    rec_rest(i2_)
            for tt in range(NT):
                i = tt % 2
                pbk = 6 + i
                mk = [f"ws:mixa{tt}_{h}" for h in range(4)] + [f"ws:mixg{tt}"]
                for c in range(8):
                    TR(mk + ["identb"], [f"ws:pb{pbk}"], out=pbb[pbk][:, c * 128:(c + 1) * 128],
                       in_=mix[:, tt, c * 128:(c + 1) * 128], identity=identb[:])
                A([f"ws:pb{pbk}"], [f"ws:mixT{i}"], out=mixT[i][:], in_=pbb[pbk][:].rearrange("p (c t) -> p c t", c=8),
                  func=AF.Copy)
                P.dma("sync", xin[i][:], x[r0 + tt * 128:r0 + (tt + 1) * 128, :], w=[f"ws:xin{i}"], sem=f"xin{i}")
                for n in range(2):
                    for c in range(8):
                        MM([f"ws:mixT{i}", woutK[c]], [f"ws:pb{n}"], out=pb[n][:, :], lhsT=mixT[i][:, c, :],
                           rhs=woutT[:, c, n * 512:(n + 1) * 512], start=(c == 0), stop=(c == 7))
                for n in range(2):
                    V("tensor_tensor", [f"ws:pb{n}", f"ws:xin{i}"], [f"ws:xin{i}"], out=xin[i][:, n * 512:(n + 1) * 512],
                      in0=pb[n][:, :], in1=xin[i][:, n * 512:(n + 1) * 512], op=ALU.add)
                P.dma("gpsimd", x1s[r0 + tt * 128:r0 + (tt + 1) * 128, :], xin[i][:], r=[f"ws:xin{i}"],
                      w=[f"x1s{seq}_{tt}"], sem=f"x1w{i}")

        if stop_after == 'p1':
            o = P.dma("sync", out[0:S, :], x1s[0:S, :], r=[f'x1s0_{i}' for i in range(16)], sem="stopout")
            P.emit(final_wait_ops=[o])
            return nc
        P.fence()
        cv = Carver(ws[:])
        AT = cv.take([256, 128], BF16)
        xnT = [cv.take([8, 256], BF16) for _ in range(2)]
        qT2 = cv.take([16, 256], BF16)
        x1t = [[cv.take([1024], F32) for _ in range(2)] for _ in range(2)]
        xnb = [cv.take([1024], BF16) for _ in range(2)]
        wqt = [cv.take([8, 128], BF16) for _ in range(2)]
        scores = cv.take([16, 128], F32)
        vals = cv.take([16, 16], F32)
        idxu = cv.take([16, 16], U32)
        idxf = cv.take([16, 16], F32)
        cand = cv.take([8, 256], F32)
        best = cv.take([8, 16], F32)
        fidx = cv.take([8, 16], U32)
        r1u = cv.take([8, 16], U32)
        r2u = cv.take([8, 16], U32)
        r1f = cv.take([8, 16], F32)
        r2f = cv.take([8, 16], F32)
        oht = cv.take([4, 16, 16], F32)
        oht2 = cv.take([4, 16, 16], F32)
        i1f = cv.take([8, 16], F32)
        i2f = cv.take([8, 16], F32)
        gf = cv.take([8, 16], F32)
        ex = cv.take([8, 16], F32)
        i1T = [cv.take([256], BF16) for _ in range(2)]
        i2T = [cv.take([256], BF16) for _ in range(2)]
        gT = [cv.take([256], BF16) for _ in range(2)]
        TGK = 16
        OH1 = [cv.take([TGK, 128], BF16) for _ in range(2)]
        E1w = [cv.take([TGK, 128], BF16) for _ in range(2)]
        OH2 = [cv.take([TGK, 128], BF16) for _ in range(2)]
        wu = [cv.take([8, 128], BF16) for _ in range(4)]
        wv = [cv.take([1024], BF16) for _ in range(4)]
        gl = [cv.take([256], BF16) for _ in range(2)]
        GT = [cv.take([256], BF16) for _ in range(2)]
        outsb = cv.take([1024], F32)
        print("phase2 ws used", cv.off, "of", cv.cap)

        out_ops = []
        nblk = NTOK // 256

        def prep_gen(b):
            p = b % 2
            R0 = b * 256
            seq = R0 // S
            for tt in range(2):
                gtt = (R0 % S) // 128 + tt
                xk = f"ws:x1t{p}{tt}"
                P.dma("sync", x1t[p][tt][:], x1s[R0 + tt * 128:R0 + (tt + 1) * 128, :], r=[f"x1s{seq}_{gtt}"],
                      w=[xk], sem=f"x1t{p}{tt}")
                ss, ssk = stcol()
                A([xk], [f"ws:xnb{tt}", ssk], out=xnb[tt][:], in_=x1t[p][tt][:], func=AF.Square, accum_out=ss)
                rs, rsk = stcol()
                rstd_from_ss(ss, ssk, 1, 1.0 / D, rs, rsk)
                V("tensor_scalar", [xk, rsk], [f"ws:xnb{tt}"], out=xnb[tt][:], in0=x1t[p][tt][:], scalar1=rs,
                  scalar2=None, op0=ALU.mult)
                yield
                pbk = 6 + tt
                for c in range(8):
                    TR([f"ws:xnb{tt}", "identb"], [f"ws:pb{pbk}"], out=pbb[pbk][:, c * 128:(c + 1) * 128],
                       in_=xnb[tt][:, c * 128:(c + 1) * 128], identity=identb[:])
                A([f"ws:pb{pbk}"], [f"ws:xnT{p}{tt}"], out=xnT[p][:, :, tt * 128:(tt + 1) * 128],
                  in_=pbb[pbk][:].rearrange("p (c t) -> p c t", c=8), func=AF.Copy)
                yield
            xnK = [f"ws:xnT{p}0", f"ws:xnT{p}1"]
            for hp in range(16):
                wi = hp % 2
                P.dma("sync", wqt[wi][:], wqb_d[hp], r=["wqb_d"], w=[f"ws:wqt{wi}"], sem=f"wqt{wi}")
                hs = hp % 2
                hv = pb[6 + hs][:, 0:256]
                hk = f"ws:pb{6 + hs}"
                for c in range(8):
                    MM(xnK + [f"ws:wqt{wi}"], [hk], out=hv, lhsT=wqt[wi][:, c, :], rhs=xnT[p][:, c, :], start=(c == 0),
                       stop=(c == 7))
                if hp % 2 == 0:
                    A([hk], [f"ws:qT2_{hp}"], out=qT2[:, hp, :], in_=hv, func=AF.Copy)
                else:
                    V("tensor_copy", [hk], [f"ws:qT2_{hp}"], out=qT2[:, hp, :], in_=hv)
                yield
            for tt in range(2):
                for q4 in range(4):
                    pbk = 6 + (q4 % 2)
                    for hq in range(4):
                        hp = q4 * 4 + hq
                        MM([f"ws:qT2_{hp}", f"keysT{hp}"], [f"ws:pb{pbk}"], out=pb[pbk][:, hq * 128:(hq + 1) * 128],
                           lhsT=qT2[:, hp, tt * 128:(tt + 1) * 128], rhs=keysT[:, hp, :], start=True, stop=True)
                    A([f"ws:pb{pbk}"], [f"ws:sc{q4}"], out=scores[:, q4 * 4:(q4 + 1) * 4, :],
                      in_=pb[pbk][:, :].rearrange("p (a n) -> p a n", a=4), func=AF.Copy)
                    yield
                for hp in range(16):
                    sk = f"ws:sc{hp // 4}"
                    sc = scores[:, hp, :]
                    V("max", [sk], [f"ws:vals{hp}a"], out=vals[:, hp, 0:8], in_=sc)
                    V("max_index", [sk, f"ws:vals{hp}a"], [f"ws:idx{hp}a"], out=idxu[:, hp, 0:8], in_max=vals[:, hp, 0:8],
                      in_values=sc)
                    V("match_replace", [sk, f"ws:vals{hp}a"], [sk], out=sc, in_to_replace=vals[:, hp, 0:8], in_values=sc,
                      imm_value=-1e30)
                    V("max", [sk], [f"ws:vals{hp}b"], out=vals[:, hp, 8:16], in_=sc)
                    V("max_index", [sk, f"ws:vals{hp}b"], [f"ws:idx{hp}b"], out=idxu[:, hp, 8:16], in_max=vals[:, hp, 8:16],
                      in_values=sc)
                    yield
                valK = [f"ws:vals{hp}{ab}" for hp in range(16) for ab in "ab"]
                idxK = [f"ws:idx{hp}{ab}" for hp in range(16) for ab in "ab"]
                V("tensor_copy", idxK, ["ws:idxf"], out=idxf[:], in_=idxu[:])
                v4 = vals[:].rearrange("p (h t) k -> p h t k", t=2)
                V("tensor_tensor", valK, ["ws:cand"], out=cand[:].rearrange("p h (a b) -> p h a b", a=16),
                  in0=v4[:, :, 0, :].unsqueeze(3).to_broadcast([128, 8, 16, 16]),
                  in1=v4[:, :, 1, :].unsqueeze(2).to_broadcast([128, 8, 16, 16]), op=ALU.add)
                yield
                for h in range(8):
                    ck = f"ws:cand{h}"
                    dep = ["ws:cand"]
                    V("max", dep, [f"ws:best{h}a"], out=best[:, h, 0:8], in_=cand[:, h, :])
                    V("max_index", dep + [f"ws:best{h}a"], [f"ws:fidx{h}a"], out=fidx[:, h, 0:8], in_max=best[:, h, 0:8],
                      in_values=cand[:, h, :])
                    V("match_replace", dep + [f"ws:best{h}a"], [ck], out=cand[:, h, :], in_to_replace=best[:, h, 0:8],
                      in_values=cand[:, h, :], imm_value=-1e30)
                    V("max", [ck], [f"ws:best{h}b"], out=best[:, h, 8:16], in_=cand[:, h, :])
                    V("max_index", [ck, f"ws:best{h}b"], [f"ws:fidx{h}b"], out=fidx[:, h, 8:16], in_max=best[:, h, 8:16],
                      in_values=cand[:, h, :])
                    yield
                bestK = [f"ws:best{h}{ab}" for h in range(8) for ab in "ab"]
                fidxK = [f"ws:fidx{h}{ab}" for h in range(8) for ab in "ab"]
                V("tensor_tensor", bestK, ["ws:ex"], out=ex[:], in0=best[:],
                  in1=best[:, :, 0:1].to_broadcast([128, 8, 16]), op=ALU.subtract)
                A(["ws:ex"], ["ws:ex2"], out=ex[:], in_=ex[:], func=AF.Exp)
                zt, ztk = stat[:, 48:56], "statz"
                V("tensor_reduce", ["ws:ex2"], [ztk], out=zt, in_=ex[:], axis=AX.X, op=ALU.add)
                zr, zrk = stat[:, 56:64], "statzr"
                V("reciprocal", [ztk], [zrk], out=zr, in_=zt)
                V("tensor_tensor", ["ws:ex2", zrk], ["ws:gf"], out=gf[:], in0=ex[:],
                  in1=zr.unsqueeze(2).to_broadcast([128, 8, 16]), op=ALU.mult)
                yield
                V("tensor_single_scalar", fidxK, ["ws:r1u"], out=r1u[:], in_=fidx[:], scalar=4, op=ALU.logical_shift_right)
                V("tensor_single_scalar", fidxK, ["ws:r2u"], out=r2u[:], in_=fidx[:], scalar=15, op=ALU.bitwise_and)
                V("tensor_copy", ["ws:r1u"], ["ws:r1f"], out=r1f[:], in_=r1u[:])
                V("tensor_copy", ["ws:r2u"], ["ws:r2f"], out=r2f[:], in_=r2u[:])
                yield
                idx4 = idxf[:].rearrange("p (h t) k -> p h t k", t=2)
                io16 = iorow[:, 0:16].unsqueeze(1).unsqueeze(1).to_broadcast([128, 4, 16, 16])
                for (rf, rk, tsel, dst, dk) in ((r1f, "ws:r1f", 0, i1f, "ws:i1f"), (r2f, "ws:r2f", 1, i2f, "ws:i2f")):
                    for hh in range(2):
                        hsl = slice(hh * 4, hh * 4 + 4)
                        V("tensor_tensor", [rk, "iorow"], ["ws:oht"], out=oht[:],
                          in0=rf[:, hsl, :].unsqueeze(3).to_broadcast([128, 4, 16, 16]), in1=io16, op=ALU.is_equal)
                        V("tensor_tensor", ["ws:oht", "ws:idxf"], ["ws:oht2"], out=oht2[:], in0=oht[:],
                          in1=idx4[:, hsl, tsel, :].unsqueeze(2).to_broadcast([128, 4, 16, 16]), op=ALU.mult)
                        V("tensor_reduce", ["ws:oht2"], [dk + str(hh)], out=dst[:, hsl, :], in_=oht2[:], axis=AX.X,
                          op=ALU.add)
                        yield
                for (src, sks, dstT, dk) in ((i1f, ["ws:i1f0", "ws:i1f1"], i1T, "ws:i1T"),
                                             (i2f, ["ws:i2f0", "ws:i2f1"], i2T, "ws:i2T"), (gf, ["ws:gf"], gT, "ws:gT")):
                    TR(sks + ["identf"], ["ws:pb7"], out=pb[7][:, 0:128], in_=src[:].rearrange("p h k -> p (h k)"),
                       identity=identf[:])
                    V("tensor_copy", ["ws:pb7"], [f"{dk}{p}{tt}"], out=dstT[p][:, tt * 128:(tt + 1) * 128],
                      in_=pb[7][:, 0:128])
                    yield

        def build_AT(b):
            p = b % 2
            io_b = iob[:].unsqueeze(1).to_broadcast([128, TGK, 128])
            for g in range(256 // TGK):
                sl = g % 2
                tt = (g * TGK) // 128
                tsl = slice(g * TGK, (g + 1) * TGK)
                V("tensor_tensor", ["iob", f"ws:i2T{p}{tt}"], [f"ws:OH2{sl}"], out=OH2[sl][:], in0=io_b,
                  in1=i2T[p][:, tsl].unsqueeze(2).to_broadcast([128, TGK, 128]), op=ALU.is_equal)
                for k in range(TGK):
                    tk = g * TGK + k
                    V("tensor_scalar", ["iob", f"ws:i1T{p}{tt}", f"ws:gT{p}{tt}"], [f"ws:E1w{sl}"], out=E1w[sl][:, k, :],
                      in0=iob[:], scalar1=i1T[p][:, tk:tk + 1], scalar2=gT[p][:, tk:tk + 1], op0=ALU.is_equal,
                      op1=ALU.mult)
                for k4 in range(TGK // 4):
                    pbk = 6 + (k4 % 2)
                    for kk in range(4):
                        k = k4 * 4 + kk
                        MM([f"ws:OH2{sl}", f"ws:E1w{sl}"], [f"ws:pb{pbk}"], out=pb[pbk][:, kk * 128:(kk + 1) * 128],
                           lhsT=OH2[sl][:, k, :], rhs=E1w[sl][:, k, :], start=True, stop=True)
                    t0 = g * TGK + k4 * 4
                    A([f"ws:pb{pbk}"], ["ws:AT"], out=AT[:, t0:t0 + 4, :],
                      in_=pb[pbk][:, :].rearrange("p (t i) -> p t i", t=4), func=AF.Copy)

        def sweep(b, gen):
            p = b % 2
            xnK = [f"ws:xnT{p}0", f"ws:xnT{p}1"]

            def rec_H(et):
                gi = b * 128 + et
                wi = gi % 4
                P.dma("sync", wu[wi][:].rearrange("p c e -> p (c e)"), wuT_d[et], r=[f"wuT{et}"], w=[f"ws:wu{wi}"],
                      sem=f"wu{wi}")
                P.dma("sync", wv[wi][:], wvb_d[et], r=[f"wvb{et}"], w=[f"ws:wv{wi}"], sem=f"wv{wi}")
                hs = gi % 2
                for c in range(8):
                    MM(xnK + [f"ws:wu{wi}"], [f"ws:pb{4 + hs}"], out=pb[4 + hs][:, 0:256], lhsT=wu[wi][:, c, :],
                       rhs=xnT[p][:, c, :], start=(c == 0), stop=(c == 7))

            def rec_rest(et):
                gi = b * 128 + et
                wi = gi % 4
                hs = gi % 2
                gs = gi % 2
                A([f"ws:pb{4 + hs}"], [f"ws:gl{gs}"], out=gl[gs][:], in_=pb[4 + hs][:, 0:256], func=AF.Gelu_apprx_tanh)
                V("tensor_tensor", [f"ws:gl{gs}", "ws:AT"], [f"ws:GT{gs}"], out=GT[gs][:], in0=gl[gs][:],
                  in1=AT[:, :, et], op=ALU.mult)
                for tt in range(2):
                    for dh in range(2):
                        MM([f"ws:GT{gs}", f"ws:wv{wi}"], [f"ws:pb{tt * 2 + dh}"], out=pb[tt * 2 + dh][:, :],
                           lhsT=GT[gs][:, tt * 128:(tt + 1) * 128], rhs=wv[wi][:, dh * 512:(dh + 1) * 512],
                           start=(et == 0), stop=(et == 127))

            rec_H(0)
            for et in range(128):
                if et + 1 < 128:
                    rec_H(et + 1)
                rec_rest(et)
                if gen is not None and et >= 2:
                    next(gen, None)
            if gen is not None:
                for _ in gen:
                    pass

        def finish(b):
            p = b % 2
            R0 = b * 256
            for tt in range(2):
                xk = f"ws:x1t{p}{tt}"
                for dh in range(2):
                    V("tensor_tensor", [f"ws:pb{tt * 2 + dh}", xk], [xk],
                      out=x1t[p][tt][:, dh * 512:(dh + 1) * 512], in0=pb[tt * 2 + dh][:, :],
                      in1=x1t[p][tt][:, dh * 512:(dh + 1) * 512], op=ALU.add)
                ss, ssk = stcol()
                A([xk], ["ws:outsb", ssk], out=outsb[:], in_=x1t[p][tt][:], func=AF.Square, accum_out=ss)
                rs, rsk = stcol()
                rstd_from_ss(ss, ssk, 1, 1.0 / D, rs, rsk)
                V("scalar_tensor_tensor", [xk, rsk, "gfin"], ["ws:outsb"], out=outsb[:],
                  in0=x1t[p][tt][:], scalar=rs, in1=gfin[:], op0=ALU.mult, op1=ALU.mult)
                out_ops.append(P.dma("gpsimd", out[R0 + tt * 128:R0 + (tt + 1) * 128, :], outsb[:],
                                     r=["ws:outsb"], sem="outw"))

        for _ in prep_gen(0):
            pass
        for b in range(nblk):
            build_AT(b)
            sweep(b, prep_gen(b + 1) if b + 1 < nblk else None)
            finish(b)
        print("n ops", len(P.ops))
        P.emit(final_wait_ops=out_ops)
    return nc


def kernel(x, w_in, lam_q1, lam_k1, lam_q2, lam_k2, g_subln, w_s, b_s, g_gv, g_gout, w_out, g_mix, g_ffn,
           peer_wq, peer_keys, peer_wu, peer_wv, g_final):
    f = lambda a: np.ascontiguousarray(np.asarray(a, dtype=np.float32))
    x = f(x)
    B = x.shape[0]
    nseq = B // NCORES
    nc = build_nc(nseq)
    common = {
        "w_in": f(w_in[0]),
        "lamv": f(np.stack([np.asarray(lam_q1)[0], np.asarray(lam_k1)[0], np.asarray(lam_q2)[0], np.asarray(lam_k2)[0]])),
        "g_subln": f(g_subln[0]), "w_s": f(w_s[0]), "b_s": f(b_s[0]), "g_gv": f(g_gv[0]), "g_gout": f(g_gout[0]),
        "w_out": f(w_out[0]), "g_mix": f(g_mix[0]), "g_ffn": f(g_ffn[0]), "peer_wq": f(peer_wq[0]),
        "peer_keys": f(np.asarray(peer_keys)[0].reshape(16, 128, 128)), "peer_wu": f(peer_wu[0]),
        "peer_wv": f(peer_wv[0]), "g_final": f(g_final),
    }
    in_maps = []
    for c in range(NCORES):
        m = dict(common)
        m["x"] = x[c * nseq:(c + 1) * nseq].reshape(nseq * S, D)
        in_maps.append(m)
    res = run_bass_kernel_spmd(nc, in_maps, core_ids=list(range(NCORES)))
    outs = [np.asarray(r["out"]).reshape(nseq, S, D) for r in res.results]
    return np.concatenate(outs, axis=0).astype(np.float32)
```
